# Optimizing a Trainium2 kernel written in Bass

```python
import jax
import jax.numpy as jnp
from jax import lax
import numpy as np


D_MODEL = 1024
BATCH = 8
SEQ = 4096
DEPTH = 4

N_MIXERS = 2
EPS = 1e-6
HG_EXPAND = 128
HG_HEADS = D_MODEL // HG_EXPAND
HG_DK = HG_EXPAND
HG_DV = D_MODEL // HG_HEADS
HG_CHUNK = 64
MB_HEADS = 8
MB_HEAD_DIM = D_MODEL // MB_HEADS
MB_BLOCK = 256
MB_TOPK = 3
MB_QCHUNK = 16
ROPE_THETA = 10000.0
FFN_DIM = 3 * D_MODEL
CONV_WIDTH = 3
N_HGRN_LAYERS = len(range(0, DEPTH, N_MIXERS))
N_MOBA_LAYERS = DEPTH - N_HGRN_LAYERS

kernel_name = 'hgrn2_moba_convglu_hybrid'


def rms_norm(x, gain):
    xf = x.astype(jnp.float32)
    y = xf * lax.rsqrt(jnp.mean(xf * xf, axis=-1, keepdims=True) + EPS) * gain
    return y.astype(x.dtype)


def rope_tables(seq):
    inv = 1.0 / (ROPE_THETA ** (jnp.arange(0, MB_HEAD_DIM, 2, dtype=jnp.float32) / MB_HEAD_DIM))
    ang = jnp.arange(seq, dtype=jnp.float32)[:, None] * inv[None, :]
    ang = jnp.concatenate([ang, ang], axis=-1)
    return jnp.cos(ang), jnp.sin(ang)


def apply_rope(t, cos, sin):
    t1, t2 = jnp.split(t, 2, axis=-1)
    return t * cos + jnp.concatenate([-t2, t1], axis=-1) * sin


def hgrn2_mixer(h, w_in, lb, out_gain, w_out):
    bsz, seq, _ = h.shape
    nc = seq // HG_CHUNK
    hk, hv = HG_HEADS * HG_DK, HG_HEADS * HG_DV

    def heads(t, d):
        t = t.reshape(bsz, seq, HG_HEADS, d).transpose(0, 2, 1, 3)
        return t.reshape(bsz, HG_HEADS, nc, HG_CHUNK, d)

    q, fz, v, g = jnp.split(h @ w_in, [hk, 2 * hk, 2 * hk + hv], axis=-1)
    q = heads(q, HG_DK).astype(jnp.float32) * HG_DK ** -0.5
    fz = heads(fz, HG_DK).astype(jnp.float32)
    v = heads(v, HG_DV)
    lb = lb.astype(jnp.float32).reshape(1, HG_HEADS, 1, 1, HG_DK)
    log_f = jnp.logaddexp(jnp.log(lb), jnp.log1p(-lb) + jax.nn.log_sigmoid(fz))
    k = (1.0 - lb) * jax.nn.sigmoid(-fz)
    G = jnp.cumsum(log_f, axis=3)

    g_mid = G[:, :, :, HG_CHUNK // 2 - 1:HG_CHUNK // 2]
    q_rel = q * jnp.exp(G - g_mid)
    k_rel = k * jnp.exp(g_mid - G)
    causal = jnp.tril(jnp.ones((HG_CHUNK, HG_CHUNK), dtype=bool))
    att = jnp.where(causal, jnp.einsum('bhncd,bhnsd->bhncs', q_rel, k_rel), 0.0)
    o_intra = jnp.einsum('bhncs,bhnsv->bhncv', att, v)

    g_last = G[:, :, :, -1]
    q_in = q * jnp.exp(G)
    k_st = k * jnp.exp(g_last[:, :, :, None, :] - G)
    decay = jnp.exp(g_last)
    xs = tuple(jnp.moveaxis(t, 2, 0) for t in (q_in, k_st, v, decay))

    def step(state, inp):
        qi, ks, vc, dc = inp
        o = jnp.einsum('bhcd,bhdv->bhcv', qi, state)
        state = (dc[..., None] * state + jnp.einsum('bhcd,bhcv->bhdv', ks, vc)).astype(jnp.float32)
        return state, o

    s0 = jnp.zeros((bsz, HG_HEADS, HG_DK, HG_DV), jnp.float32)
    _, o_inter = lax.scan(step, s0, xs)
    o = (o_intra + jnp.moveaxis(o_inter, 0, 2)).reshape(bsz, HG_HEADS, seq, HG_DV)

    g = g.reshape(bsz, seq, HG_HEADS, HG_DV).transpose(0, 2, 1, 3).astype(jnp.float32)
    o = rms_norm(o, out_gain) * jax.nn.silu(g)
    o = o.transpose(0, 2, 1, 3).reshape(bsz, seq, hv).astype(h.dtype)
    return o @ w_out


def moba_mixer(h, w_qkv, q_gain, k_gain, w_out, cos, sin):
    bsz, seq, _ = h.shape
    qkv = (h @ w_qkv).reshape(bsz, seq, 3, MB_HEADS, MB_HEAD_DIM).transpose(2, 0, 3, 1, 4)
    q, k, v = qkv[0], qkv[1], qkv[2]
    q = apply_rope(rms_norm(q, q_gain), cos, sin) * MB_HEAD_DIM ** -0.5
    k = apply_rope(rms_norm(k, k_gain), cos, sin)

    n_blk = -(-seq // MB_BLOCK)
    pad = n_blk * MB_BLOCK - seq
    kb = jnp.pad(k, ((0, 0), (0, 0), (0, pad), (0, 0))).reshape(bsz, MB_HEADS, n_blk, MB_BLOCK, MB_HEAD_DIM)
    vb = jnp.pad(v, ((0, 0), (0, 0), (0, pad), (0, 0))).reshape(bsz, MB_HEADS, n_blk, MB_BLOCK, MB_HEAD_DIM)

    k_mean = jnp.mean(kb, axis=3)
    q_blk = jnp.arange(seq) // MB_BLOCK
    past = jnp.arange(n_blk)[None, :] < q_blk[:, None]
    gate = jnp.where(past, jnp.einsum('bhsd,bhnd->bhsn', q, k_mean), -jnp.inf)
    top = min(MB_TOPK, n_blk)
    _, sel = lax.top_k(gate, top)

    nq = seq // MB_QCHUNK
    q_c = q.reshape(bsz, MB_HEADS, nq, MB_QCHUNK, MB_HEAD_DIM).transpose(2, 0, 1, 3, 4)
    sel_c = sel.reshape(bsz, MB_HEADS, nq, MB_QCHUNK, top).transpose(2, 0, 1, 3, 4)
    b_idx = jnp.arange(bsz)[:, None, None, None]
    h_idx = jnp.arange(MB_HEADS)[None, :, None, None]

    def attend_chunk(args):
        qc, selc, c = args
        blk = (c * MB_QCHUNK) // MB_BLOCK
        k_sel = kb[b_idx, h_idx, selc]
        v_sel = vb[b_idx, h_idx, selc]
        k_own = lax.dynamic_index_in_dim(kb, blk, axis=2, keepdims=False)
        v_own = lax.dynamic_index_in_dim(vb, blk, axis=2, keepdims=False)
        ok_sel = (jnp.arange(top) < blk)[:, None]
        s_sel = jnp.where(ok_sel, jnp.einsum('bhqd,bhqnkd->bhqnk', qc, k_sel), -jnp.inf)
        s_sel = s_sel.reshape(bsz, MB_HEADS, MB_QCHUNK, top * MB_BLOCK)
        q_pos = c * MB_QCHUNK + jnp.arange(MB_QCHUNK)
        k_pos = blk * MB_BLOCK + jnp.arange(MB_BLOCK)
        s_own = jnp.where(k_pos[None, :] <= q_pos[:, None],
                          jnp.einsum('bhqd,bhkd->bhqk', qc, k_own), -jnp.inf)
        p = jax.nn.softmax(jnp.concatenate([s_sel, s_own], axis=-1).astype(jnp.float32), axis=-1)
        p_sel = p[..., :top * MB_BLOCK].reshape(bsz, MB_HEADS, MB_QCHUNK, top, MB_BLOCK)
        p_own = p[..., top * MB_BLOCK:]
        return (jnp.einsum('bhqnk,bhqnkd->bhqd', p_sel, v_sel)
                + jnp.einsum('bhqk,bhkd->bhqd', p_own, v_own))

    o = lax.map(attend_chunk, (q_c, sel_c, jnp.arange(nq)))
    o = o.transpose(1, 0, 3, 2, 4).reshape(bsz, seq, MB_HEADS * MB_HEAD_DIM).astype(h.dtype)
    return o @ w_out


def conv_glu_ffn(h, w_up, conv_w, conv_b, w_down):
    seq = h.shape[1]
    a, u = jnp.split(h @ w_up, 2, axis=-1)
    a_pad = jnp.pad(a, ((0, 0), (CONV_WIDTH - 1, 0), (0, 0)))
    a = sum(conv_w[j] * a_pad[:, j:j + seq] for j in range(CONV_WIDTH)) + conv_b
    return (jax.nn.silu(a) * u) @ w_down


def setup_inputs(seed: int = 0) -> dict:
    key = jax.random.key(seed)
    ks = jax.random.split(key, 16)

    def nrm(k, shape, scale):
        return jax.random.normal(k, shape, jnp.float32) * scale

    hk, hv = HG_HEADS * HG_DK, HG_HEADS * HG_DV
    mb_w = MB_HEADS * MB_HEAD_DIM
    return {
        'x': nrm(ks[0], (BATCH, SEQ, D_MODEL), 1.0),
        'attn_norm': 1.0 + nrm(ks[1], (DEPTH, D_MODEL), 0.02),
        'ffn_norm': 1.0 + nrm(ks[2], (DEPTH, D_MODEL), 0.02),
        'hgrn_w_in': nrm(ks[3], (N_HGRN_LAYERS, D_MODEL, 2 * hk + 2 * hv), D_MODEL ** -0.5),
        'hgrn_lb': nrm(ks[4], (N_HGRN_LAYERS, hk), 0.1),
        'hgrn_out_norm': 1.0 + nrm(ks[5], (N_HGRN_LAYERS, HG_DV), 0.02),
        'hgrn_w_out': nrm(ks[6], (N_HGRN_LAYERS, hv, D_MODEL), hv ** -0.5),
        'moba_w_qkv': nrm(ks[7], (N_MOBA_LAYERS, D_MODEL, 3 * mb_w), D_MODEL ** -0.5),
        'moba_q_norm': 1.0 + nrm(ks[8], (N_MOBA_LAYERS, MB_HEAD_DIM), 0.02),
        'moba_k_norm': 1.0 + nrm(ks[9], (N_MOBA_LAYERS, MB_HEAD_DIM), 0.02),
        'moba_w_out': nrm(ks[10], (N_MOBA_LAYERS, mb_w, D_MODEL), mb_w ** -0.5),
        'ffn_w_up': nrm(ks[11], (DEPTH, D_MODEL, 2 * FFN_DIM), D_MODEL ** -0.5),
        'ffn_conv_w': nrm(ks[12], (DEPTH, CONV_WIDTH, FFN_DIM), CONV_WIDTH ** -0.5),
        'ffn_conv_b': nrm(ks[13], (DEPTH, FFN_DIM), 0.01),
        'ffn_w_down': nrm(ks[14], (DEPTH, FFN_DIM, D_MODEL), FFN_DIM ** -0.5),
    }


def reference(x, attn_norm, ffn_norm, hgrn_w_in, hgrn_lb, hgrn_out_norm, hgrn_w_out,
              moba_w_qkv, moba_q_norm, moba_k_norm, moba_w_out,
              ffn_w_up, ffn_conv_w, ffn_conv_b, ffn_w_down):
    seq = x.shape[1]
    cos, sin = rope_tables(seq)
    lb_cum = jnp.cumsum(jax.nn.softmax(hgrn_lb.astype(jnp.float32), axis=0), axis=0)
    lower_bounds = lb_cum - lb_cum[0]
    for layer in range(DEPTH):
        slot = layer // N_MIXERS
        h = rms_norm(x, attn_norm[layer])
        if layer % N_MIXERS == 0:
            mix = hgrn2_mixer(h, hgrn_w_in[slot], lower_bounds[slot], hgrn_out_norm[slot], hgrn_w_out[slot])
        else:
            mix = moba_mixer(h, moba_w_qkv[slot], moba_q_norm[slot], moba_k_norm[slot], moba_w_out[slot], cos, sin)
        x = x + mix.astype(x.dtype)
        ffn = conv_glu_ffn(rms_norm(x, ffn_norm[layer]), ffn_w_up[layer], ffn_conv_w[layer],
                           ffn_conv_b[layer], ffn_w_down[layer])
        x = x + ffn.astype(x.dtype)
    return x
```

```python
import contextlib
import numpy as np
import concourse.bass as bass
import concourse.mybir as mybir
from concourse.bass_utils import run_bass_kernel_spmd

F32 = mybir.dt.float32
BF16 = mybir.dt.bfloat16
AF = mybir.ActivationFunctionType
ALU = mybir.AluOpType
AX = mybir.AxisListType

D = 1024
S = 4096
T = 512
NT = S // T
DEPTH = 4
FF = 3072
EPS = 1e-6
NEG = -30000.0


class Sem:
    __slots__ = ("h", "v")

    def __init__(self, h):
        self.h = h
        self.v = 0


class Buf:
    __slots__ = ("name", "w", "r")

    def __init__(self, name=""):
        self.name = name
        self.w = None
        self.r = {}


class Eng:
    def __init__(self, k, name, h, is_pe=False):
        self.k = k
        self.name = name
        self.h = h
        self.is_pe = is_pe
        self.sem = k.new_sem(name)
        self.dma_sems = []
        self.dma_i = 0
        self.waited = {}
        self.n_ops = 0
        self.n_waits = 0

    def next_dma_sem(self):
        if not self.dma_sems:
            n = 24 if self.name == "pool" else 16
            self.dma_sems = [self.k.new_sem(f"{self.name}_dma{i}") for i in range(n)]
        s = self.dma_sems[self.dma_i % len(self.dma_sems)]
        self.dma_i += 1
        return s


class K:
    def __init__(self, nc, es):
        self.nc = nc
        self.es = es
        self.sems = []
        self.pe = Eng(self, "pe", nc.tensor, is_pe=True)
        self.act = Eng(self, "act", nc.scalar)
        self.dve = Eng(self, "dve", nc.vector)
        self.pool = Eng(self, "pool", nc.gpsimd)
        self.sp = Eng(self, "sp", nc.sync)
        self.engs = [self.pe, self.act, self.dve, self.pool, self.sp]

    def new_sem(self, name):
        h = self.es.enter_context(self.nc.semaphore(f"s_{name}_{len(self.sems)}"))
        s = Sem(h)
        self.sems.append(s)
        return s

    def op(self, eng, fn, reads=(), writes=(), dma=False, after=None):
        deps = {}
        if after:
            for s, v in after.items():
                if deps.get(s, 0) < v:
                    deps[s] = v
        for b in reads:
            if b.w is not None and deps.get(b.w[0], 0) < b.w[1]:
                deps[b.w[0]] = b.w[1]
        for b in writes:
            if b.w is not None and deps.get(b.w[0], 0) < b.w[1]:
                deps[b.w[0]] = b.w[1]
            for s, v in b.r.items():
                if deps.get(s, 0) < v:
                    deps[s] = v
        for s, v in deps.items():
            if eng.is_pe and s is eng.sem:
                continue
            if eng.waited.get(s, 0) >= v:
                continue
            eng.h.wait_ge(s.h, v)
            eng.waited[s] = v
            eng.n_waits += 1
        if dma:
            ds = eng.next_dma_sem()
            if ds.v > 0 and eng.waited.get(ds, 0) < ds.v:
                eng.h.wait_ge(ds.h, ds.v)
                eng.waited[ds] = ds.v
                eng.n_waits += 1
        ins = fn(eng.h)
        eng.n_ops += 1
        if dma:
            s = ds
            s.v += 16
            ins.then_inc(s.h, 16)
        else:
            if eng.sem.v >= 30000:
                eng.sem = self.new_sem(eng.name)
            s = eng.sem
            s.v += 1
            ins.then_inc(s.h, 1)
        ev = (s, s.v)
        for b in reads:
            if b.r.get(s, 0) < s.v:
                b.r[s] = s.v
        for b in writes:
            b.w = ev
            b.r = {}
        return ev

    def snapshot(self):
        return {s: s.v for s in self.sems if s.v > 0}

    def barrier(self, snap, engines=None):
        for e in (engines or self.engs):
            for s, v in snap.items():
                if e.waited.get(s, 0) >= v:
                    continue
                e.h.wait_ge(s.h, v)
                e.waited[s] = v
                e.n_waits += 1


class Prog:
    def __init__(self, stages):
        self.stages = stages
        self.nc = bass.Bass("TRN2", target_bir_lowering=False)
        self.ext_in = {}

    def dram_in(self, name, shape, dt=F32):
        t = self.nc.dram_tensor(name, list(shape), dt, kind="ExternalInput")
        self.ext_in[name] = tuple(shape)
        return t.ap()

    def dram_tmp(self, name, shape, dt):
        kind = "ExternalOutput" if DEBUG_SCRATCH else "Internal"
        return self.nc.dram_tensor(name, list(shape), dt, kind=kind).ap()


DEBUG_SCRATCH = False
DEBUG_ONLY = None
SUBPHASES = {"ffn": ["F1", "F2"], "moba": ["M1", "M2", "M3"], "hgrn": ["H1"]}
NEEDS = {"F1": ("A", "w_up", D, 2 * FF, 2048), "F2": ("B", "w_dn", FF, D, 1024),
         "M1": ("A", "w_qkv", D, 3 * D, 1024), "H1": ("A", "w_in", D, 4 * D, 2048),
         "M2": ("A", None, 0, 0, 0)}


def build_program(stages):
    P = Prog(stages)
    nc = P.nc
    with contextlib.ExitStack() as es:
        k = K(nc, es)
        G = _Globals(P, k, es)
        n = len(stages)
        subs = []
        for i, (kind, layer) in enumerate(stages):
            io = dict(src=G.xin if i == 0 else G.xres, dst=G.yout if i == n - 1 else G.xres,
                      sb_src=G.xin_bufs if i == 0 else G.xres_bufs,
                      sb_dst=G.yout_bufs if i == n - 1 else G.xres_bufs)
            for sp in SUBPHASES[kind]:
                subs.append((sp, kind, layer, io))
        loaded = {}
        last_user = {"A": -1, "B": -1}
        regs = {"A": G.regA, "B": G.regB}
        for i, (sp, kind, layer, io) in enumerate(subs):
            snap = phase_begin(G)
            for r in ("A", "B"):
                for j in range(i, len(subs)):
                    nd = NEEDS.get(subs[j][0])
                    if nd is not None and nd[0] == r:
                        if j not in loaded and last_user[r] < i:
                            W = G.w[(subs[j][1], subs[j][2])]
                            if nd[1] is None:
                                if j != i:
                                    break
                                loaded[j] = None
                            else:
                                loaded[j] = WRegion(G, regs[r], snap, W[nd[1]], nd[2], nd[3], blk=nd[4], name=nd[1])
                            last_user[r] = j
                        break
            wr = loaded.get(i)
            if DEBUG_ONLY is None or sp in DEBUG_ONLY:
                PHASE_FN[sp](G, layer, wr, **io)
        k.barrier(k.snapshot(), engines=[k.sp])
        P.stats = {e.name: (e.n_ops, e.n_waits) for e in k.engs}
        P.nsems = len(k.sems)
    return P


class _Globals:
    def uniq(self, name):
        self._uid = getattr(self, "_uid", 0) + 1
        return f"{name}_u{self._uid}"

    def __init__(self, P, k, es):
        self.P = P
        self.k = k
        self.es = es
        nc = P.nc
        self.nc = nc
        stages = P.stages
        self.xin = P.dram_in("xin", [128, 8, S])
        self.yout = nc.dram_tensor("yout", [128, 8, S], F32, kind="ExternalOutput").ap()
        self.xres = P.dram_tmp("xres", [128, 8, S], F32)
        self.xin_bufs = [Buf(f"xin{t}") for t in range(16)]
        self.yout_bufs = [Buf(f"yout{t}") for t in range(16)]
        self.xres_bufs = [Buf(f"xres{t}") for t in range(16)]
        kinds = {kd for kd, _ in stages}
        self.c_ones = P.dram_in("c_ones", [128, 128])
        self.c_ident = P.dram_in("c_ident", [128, 128])
        sb = lambda name, shape, dt: es.enter_context(nc.sbuf_tensor(name, list(shape), dt))
        self.sb = sb
        self.ones_bf = sb("ones_bf", [128, 128], BF16)
        self.ident_bf = sb("ident_bf", [128, 128], BF16)
        self.ident_f = sb("ident_f", [128, 128], F32)
        self.b_const = Buf("consts")
        k.op(k.pool, lambda e: e.dma_start(out=self.ones_bf[:], in_=self.c_ones[:, :]), writes=[self.b_const], dma=True)
        k.op(k.pool, lambda e: e.dma_start(out=self.ident_bf[:], in_=self.c_ident[:, :]), writes=[self.b_const], dma=True)
        k.op(k.sp, lambda e: e.dma_start(out=self.ident_f[:], in_=self.c_ident[:, :]), writes=[self.b_const], dma=True)
        self.regA = sb("regA", [128, 49152], BF16)
        self.regB = sb("regB", [128, 24576], BF16)
        self.regA_free = {}
        self.regB_free = {}
        self.ps = [es.enter_context(nc.psum_tensor(f"psb{i}", [128, 512], F32)) for i in range(8)]
        self.psb = [Buf(f"psb{i}") for i in range(8)]
        self.w = {}
        for kind, l in stages:
            if kind == "ffn":
                self.w[("ffn", l)] = dict(
                    w_up=P.dram_in(f"ffn_w_up_{l}", [D, 2 * FF]),
                    w_dn=P.dram_in(f"ffn_w_dn_{l}", [FF, D]),
                    vec=P.dram_in(f"ffn_vec_{l}", [128, 8 + 24 * 4]),
                )
            elif kind == "moba":
                self.w[("moba", l)] = dict(
                    w_qkv=P.dram_in(f"moba_w_qkv_{l}", [D, 3 * D]),
                    w_out=P.dram_in(f"moba_w_out_{l}", [D, D]),
                    vec=P.dram_in(f"moba_vec_{l}", [128, 8 + 2]),
                )
            elif kind == "hgrn":
                self.w[("hgrn", l)] = dict(
                    w_in=P.dram_in(f"hgrn_w_in_{l}", [D, 4 * D]),
                    w_out=P.dram_in(f"hgrn_w_out_{l}", [D, D]),
                    vec=P.dram_in(f"hgrn_vec_{l}", [128, 8 + 16 + 1]),
                )
        if "ffn" in kinds:
            self.gT = P.dram_tmp("gT", [128, 24, S], BF16)
            self.gT_bufs = [[Buf(f"gT{t}_{g}") for g in range(6)] for t in range(NT)]
        if "moba" in kinds:
            self.c_rot = P.dram_in("c_rot", [128, 128])
            self.c_cos = P.dram_in("c_cos", [128, S])
            self.c_sin = P.dram_in("c_sin", [128, S])
            self.c_past = P.dram_in("c_past", [128, 32 * 16])
            self.c_causal = P.dram_in("c_causal", [128, 2 * 256])
            self.c_selrow = P.dram_in("c_selrow", [128, 16 * 128])
            self.qT = P.dram_tmp("qT", [8, 128, S], BF16)
            self.kT = P.dram_tmp("kT", [8, 128, S], BF16)
            self.vtok = P.dram_tmp("vtok", [128, 32, D], BF16)
            self.oT = P.dram_tmp("oT", [128, 8, S], BF16)
            self.q_bufs = [[Buf(f"q{h}_{t}") for t in range(NT)] for h in range(8)]
            self.k_bufs = [[Buf(f"k{h}_{t}") for t in range(NT)] for h in range(8)]
            self.v_bufs = [Buf(f"v{t}") for t in range(NT)]
            self.o_bufs = [Buf(f"o{h}") for h in range(8)]
        if "hgrn" in kinds:
            self.c_tri = P.dram_in("c_tri", [128, 128])
            self.c_scan = P.dram_in("c_scan", [128, T])


class WRegion:
    def __init__(self, G, reg, free_after, w_ap, kdim, ncols, col0=0, blk=2048, name="w"):
        k = G.k
        self.kc = kdim // 128
        self.ncols = ncols
        self.blk = min(blk, ncols)
        self.view = reg[:, 0:self.kc * ncols].rearrange("p (kc n) -> p kc n", n=ncols)
        self.bufs = {}
        wv = w_ap.rearrange("(kc p) n -> p kc n", p=128)
        for kc in range(self.kc):
            for nb in range(ncols // self.blk):
                b = Buf(f"{name}_{kc}_{nb}")
                self.bufs[(kc, nb)] = b
                n0 = nb * self.blk
                k.op(k.pool,
                     lambda e, kc=kc, n0=n0: e.dma_start(out=self.view[:, kc, n0:n0 + self.blk],
                                                         in_=wv[:, kc, col0 + n0:col0 + n0 + self.blk]),
                     writes=[b], dma=True, after=free_after)

    def buf(self, kc, n0):
        return self.bufs[(kc, n0 // self.blk)]


def rmsnorm_tile(G, L, xt, b_xt, gain, hT, b_hT, ps_i, nfeat_chunks=8, ps_ap=None, ps_buf=None):
    k = G.k
    ps, pb = (G.ps[ps_i], G.psb[ps_i]) if ps_ap is None else (ps_ap, ps_buf)
    for c in range(nfeat_chunks):
        sq, b_sq = L["sq"][c % 2]
        k.op(k.act, lambda e, c=c, sq=sq: e.activation(out=sq[:], in_=xt[:, c, :], func=AF.Square),
             reads=[b_xt], writes=[b_sq])
        k.op(k.pe, lambda e, c=c, sq=sq: e.matmul(ps if ps_ap is not None else ps[:], lhsT=G.ones_bf[:], rhs=sq[:], start=(c == 0),
                                                  stop=(c == nfeat_chunks - 1)),
             reads=[b_sq, G.b_const], writes=(pb if isinstance(pb, list) else [pb]))
    rs, b_rs = L["rstd"]
    k.op(k.act, lambda e: e.activation(out=rs[:], in_=(ps if ps_ap is not None else ps[:]), func=AF.Ln, scale=1.0 / (128 * nfeat_chunks),
                                       bias=L["eps"][:, 0:1]),
         reads=(pb if isinstance(pb, list) else [pb]) + [L["b_eps"]], writes=[b_rs])
    k.op(k.act, lambda e: e.activation(out=rs[:], in_=rs[:], func=AF.Exp, scale=-0.5), reads=[b_rs], writes=[b_rs])
    for c in range(nfeat_chunks):
        k.op(k.dve, lambda e, c=c: e.scalar_tensor_tensor(out=hT[:, c, :], in0=xt[:, c, :], scalar=gain[:, c:c + 1],
                                                          in1=rs[:], op0=ALU.mult, op1=ALU.mult),
             reads=[b_xt, b_rs, L["b_vec"]], writes=[b_hT])


def phase_begin(G):
    snap = G.k.snapshot()
    G.k.barrier(snap)
    return snap


def phase_F1(G, layer, wup, src, dst, sb_src, sb_dst):
    k, nc = G.k, G.nc
    W = G.w[("ffn", layer)]
    with contextlib.ExitStack() as es:
        sb = lambda name, shape, dt: es.enter_context(nc.sbuf_tensor(G.uniq(name), list(shape), dt))
        vec = sb("f_vec", [128, 8 + 96], F32)
        b_vec = Buf("f_vec")
        k.op(k.sp, lambda e: e.dma_start(out=vec[:], in_=W["vec"][:, :]), writes=[b_vec], dma=True)
        eps = sb("f_eps", [128, 1], F32)
        b_eps = Buf("f_eps")
        k.op(k.pool, lambda e: e.memset(eps[:], EPS), writes=[b_eps])
        gain = vec[:, 0:8]
        cw = vec[:, 8:104].rearrange("p (j f) -> p j f", f=24)
        xt = sb("f_xt", [128, 8, T], F32)
        b_xt = Buf("f_xt")
        hTs = [(sb(f"f_hT{i}", [128, 8, T], BF16), Buf(f"f_hT{i}")) for i in range(2)]
        L = dict(sq=[(sb(f"f_sq{i}", [128, T], BF16), Buf(f"f_sq{i}")) for i in range(2)],
                 rstd=(sb("f_rstd", [128, T], F32), Buf("f_rstd")), eps=eps, b_eps=b_eps, b_vec=b_vec)
        abufs = [(sb(f"f_ab{i}", [128, T + 2], F32), Buf(f"f_ab{i}")) for i in range(2)]
        tbufs = [(sb(f"f_t{i}", [128, T], F32), Buf(f"f_t{i}")) for i in range(2)]
        gbufs = [(sb(f"f_g{i}", [128, 4, T], BF16), Buf(f"f_g{i}")) for i in range(2)]
        carry = sb("f_carry", [128, 24, 2], F32)
        b_carry = [Buf(f"f_carry{f}") for f in range(24)]
        k.op(k.pool, lambda e: e.memset(carry[:], 0.0), writes=b_carry)

        k.op(k.sp, lambda e: e.dma_start(out=xt[:], in_=src[:, :, 0:T]), reads=sb_src[0:2], writes=[b_xt], dma=True)
        for ti in range(NT):
            hT, b_hT = hTs[ti % 2]
            rmsnorm_tile(G, L, xt, b_xt, gain, hT, b_hT, ps_i=0)
            if ti + 1 < NT:
                k.op(k.sp, lambda e, ti=ti: e.dma_start(out=xt[:], in_=src[:, :, (ti + 1) * T:(ti + 2) * T]),
                     reads=sb_src[2 * ti + 2:2 * ti + 4], writes=[b_xt], dma=True)
            for fc in range(24):
                pa_i, pu_i = 1 + 2 * (fc % 3), 2 + 2 * (fc % 3)
                pa, pu = G.ps[pa_i], G.ps[pu_i]
                for kc in range(8):
                    k.op(k.pe, lambda e, kc=kc, fc=fc, pa=pa: e.matmul(
                        pa[:], lhsT=wup.view[:, kc, fc * 128:(fc + 1) * 128], rhs=hT[:, kc, :],
                        start=(kc == 0), stop=(kc == 7)),
                        reads=[wup.buf(kc, fc * 128), b_hT], writes=[G.psb[pa_i]])
                for kc in range(8):
                    k.op(k.pe, lambda e, kc=kc, fc=fc, pu=pu: e.matmul(
                        pu[:], lhsT=wup.view[:, kc, FF + fc * 128:FF + (fc + 1) * 128], rhs=hT[:, kc, :],
                        start=(kc == 0), stop=(kc == 7)),
                        reads=[wup.buf(kc, FF + fc * 128), b_hT], writes=[G.psb[pu_i]])
                ab, b_ab = abufs[fc % 2]
                tb, b_tb = tbufs[fc % 2]
                gb, b_gb = gbufs[(fc // 4) % 2]
                k.op(k.pool, lambda e, fc=fc, ab=ab: e.tensor_copy(out=ab[:, 0:2], in_=carry[:, fc, :]),
                     reads=[b_carry[fc]], writes=[b_ab])
                k.op(k.act, lambda e, ab=ab, pa=pa: e.activation(out=ab[:, 2:T + 2], in_=pa[:], func=AF.Copy),
                     reads=[G.psb[pa_i]], writes=[b_ab])
                k.op(k.pool, lambda e, fc=fc, ab=ab: e.tensor_copy(out=carry[:, fc, :], in_=ab[:, T:T + 2]),
                     reads=[b_ab], writes=[b_carry[fc]])
                k.op(k.act, lambda e, fc=fc, tb=tb, pa=pa: e.activation(out=tb[:], in_=pa[:], func=AF.Identity,
                                                                       bias=cw[:, 3, fc:fc + 1], scale=cw[:, 2, fc:fc + 1]),
                     reads=[G.psb[pa_i], b_vec], writes=[b_tb])
                k.op(k.dve, lambda e, fc=fc, tb=tb, ab=ab: e.scalar_tensor_tensor(
                    out=tb[:], in0=ab[:, 1:T + 1], scalar=cw[:, 1, fc:fc + 1], in1=tb[:], op0=ALU.mult, op1=ALU.add),
                    reads=[b_ab, b_tb, b_vec], writes=[b_tb])
                k.op(k.dve, lambda e, fc=fc, tb=tb, ab=ab: e.scalar_tensor_tensor(
                    out=tb[:], in0=ab[:, 0:T], scalar=cw[:, 0, fc:fc + 1], in1=tb[:], op0=ALU.mult, op1=ALU.add),
                    reads=[b_ab, b_tb, b_vec], writes=[b_tb])
                k.op(k.act, lambda e, tb=tb: e.activation(out=tb[:], in_=tb[:], func=AF.Silu), reads=[b_tb], writes=[b_tb])
                k.op(k.dve, lambda e, fc=fc, tb=tb, gb=gb, pu=pu: e.tensor_tensor(
                    out=gb[:, fc % 4, :], in0=tb[:], in1=pu[:], op=ALU.mult),
                    reads=[b_tb, G.psb[pu_i]], writes=[b_gb])
                if fc % 4 == 3:
                    f0 = fc - 3
                    k.op(k.sp, lambda e, f0=f0, gb=gb, ti=ti: e.dma_start(
                        out=G.gT[:, f0:f0 + 4, ti * T:(ti + 1) * T], in_=gb[:]),
                        reads=[b_gb], writes=[G.gT_bufs[ti][fc // 4]], dma=True)


def phase_F2(G, layer, wdn, src, dst, sb_src, sb_dst):
    k, nc = G.k, G.nc
    with contextlib.ExitStack() as es:
        sb = lambda name, shape, dt: es.enter_context(nc.sbuf_tensor(G.uniq(name), list(shape), dt))
        xts = [(sb(f"g_xt{i}", [128, 8, T], F32), Buf(f"g_xt{i}")) for i in range(2)]
        gts = [(sb(f"g_gt{i}", [128, 12, T], BF16), Buf(f"g_gt{i}")) for i in range(2)]
        for ti in range(NT):
            xt, b_xt = xts[ti % 2]
            k.op(k.sp, lambda e, ti=ti, xt=xt: e.dma_start(out=xt[:], in_=src[:, :, ti * T:(ti + 1) * T]),
                 reads=sb_src[2 * ti:2 * ti + 2], writes=[b_xt], dma=True)
            for half in range(2):
                gt, b_gt = gts[half]
                k.op(k.sp, lambda e, ti=ti, gt=gt, half=half: e.dma_start(
                    out=gt[:], in_=G.gT[:, half * 12:(half + 1) * 12, ti * T:(ti + 1) * T]),
                    reads=G.gT_bufs[ti][half * 3:(half + 1) * 3], writes=[b_gt], dma=True)
                for oc in range(8):
                    for f in range(12):
                        fc = half * 12 + f
                        k.op(k.pe, lambda e, oc=oc, fc=fc, f=f, gt=gt: e.matmul(
                            G.ps[oc][:], lhsT=wdn.view[:, fc, oc * 128:(oc + 1) * 128], rhs=gt[:, f, :],
                            start=(fc == 0), stop=(fc == 23)),
                            reads=[wdn.buf(fc, oc * 128), b_gt], writes=[G.psb[oc]])
            for oc in range(8):
                k.op(k.dve, lambda e, oc=oc, xt=xt: e.tensor_tensor(out=xt[:, oc, :], in0=G.ps[oc][:], in1=xt[:, oc, :],
                                                                   op=ALU.add),
                     reads=[G.psb[oc], b_xt], writes=[b_xt])
            k.op(k.sp, lambda e, ti=ti, xt=xt: e.dma_start(out=dst[:, :, ti * T:(ti + 1) * T], in_=xt[:]),
                 reads=[b_xt], writes=sb_dst[2 * ti:2 * ti + 2], dma=True)


def carve(reg, off, shape):
    n = int(np.prod(shape[1:]))
    v = reg[:, off:off + n]
    if len(shape) == 3:
        v = v.rearrange("p (a b) -> p a b", b=shape[2])
    return v, off + n


def phase_M1(G, layer, wqkv, src, dst, sb_src, sb_dst):
    k, nc = G.k, G.nc
    W = G.w[("moba", layer)]
    with contextlib.ExitStack() as es:
        sb = lambda name, shape, dt: es.enter_context(nc.sbuf_tensor(G.uniq(name), list(shape), dt))
        vec = sb("m_vec", [128, 10], F32)
        b_vec = Buf("m_vec")
        k.op(k.sp, lambda e: e.dma_start(out=vec[:], in_=W["vec"][:, :]), writes=[b_vec], dma=True)
        qkg = sb("m_qkg", [128, 2], F32)
        k.op(k.dve, lambda e: e.tensor_scalar(out=qkg[:, 0:1], in0=vec[:, 8:9], scalar1=128.0 ** -0.5, scalar2=None,
                                              op0=ALU.mult), reads=[b_vec], writes=[b_vec])
        k.op(k.dve, lambda e: e.tensor_copy(out=qkg[:, 1:2], in_=vec[:, 9:10]), reads=[b_vec], writes=[b_vec])
        eps = sb("m_eps", [128, 1], F32)
        b_eps = Buf("m_eps")
        k.op(k.pool, lambda e: e.memset(eps[:], EPS), writes=[b_eps])
        gain = vec[:, 0:8]
        xt = sb("m_xt", [128, 8, T], F32)
        b_xt = Buf("m_xt")
        cs = sb("m_cs", [128, 2, T], F32)
        b_cs = Buf("m_cs")
        off = 8 * 3 * D
        hTs = []
        for i in range(2):
            v, off = carve(G.regA, off, [128, 8, T])
            hTs.append((v, Buf(f"m_hT{i}")))
        vt, off = carve(G.regA, off, [128, 4, D])
        b_vt = Buf("m_vt")
        rot_bf, off = carve(G.regA, off, [128, 128])
        b_rot = Buf("m_rot")
        k.op(k.pool, lambda e: e.dma_start(out=rot_bf, in_=G.c_rot[:, :]), writes=[b_rot], dma=True)
        two = lambda nm: None
        sqs, sqh, qnb, qfs = [], [], [], []
        for i in range(2):
            v, off = carve(G.regA, off, [128, T]); sqs.append((v, Buf(f"m_sq{i}")))
            v, off = carve(G.regA, off, [128, T]); sqh.append((v, Buf(f"m_sqh{i}")))
            v, off = carve(G.regA, off, [128, T]); qnb.append((v, Buf(f"m_qnb{i}")))
            v, off = carve(G.regA, off, [128, T]); qfs.append((v, Buf(f"m_qf{i}")))
        assert off <= 49152
        L = dict(sq=sqs, rstd=(sb("m_rstd", [128, T], F32), Buf("m_rstd")), eps=eps, b_eps=b_eps, b_vec=b_vec)
        r2s = [(sb(f"m_r2{i}", [128, T], F32), Buf(f"m_r2{i}")) for i in range(2)]
        qns = [(sb(f"m_qn{i}", [128, T], F32), Buf(f"m_qn{i}")) for i in range(2)]
        t1s = [(sb(f"m_t1{i}", [128, T], F32), Buf(f"m_t1{i}")) for i in range(2)]
        t2s = [(sb(f"m_t2{i}", [128, T], F32), Buf(f"m_t2{i}")) for i in range(2)]

        k.op(k.sp, lambda e: e.dma_start(out=xt[:], in_=src[:, :, 0:T]), reads=sb_src[0:2], writes=[b_xt], dma=True)
        cnt = 0
        dbg = "vq"
        for ti in range(NT):
            hT, b_hT = hTs[ti % 2]
            rmsnorm_tile(G, L, xt, b_xt, gain, hT, b_hT, ps_i=0)
            if ti + 1 < NT:
                k.op(k.sp, lambda e, ti=ti: e.dma_start(out=xt[:], in_=src[:, :, (ti + 1) * T:(ti + 2) * T]),
                     reads=sb_src[2 * ti + 2:2 * ti + 4], writes=[b_xt], dma=True)
            k.op(k.sp, lambda e, ti=ti: e.dma_start(out=cs[:, 0, :], in_=G.c_cos[:, ti * T:(ti + 1) * T]),
                 writes=[b_cs], dma=True)
            k.op(k.sp, lambda e, ti=ti: e.dma_start(out=cs[:, 1, :], in_=G.c_sin[:, ti * T:(ti + 1) * T]),
                 writes=[b_cs], dma=True)
            for sub in range(4 if "v" in dbg else 0):
                for half in range(2):
                    pi = 1 + (sub * 2 + half) % 2
                    for kc in range(8):
                        k.op(k.pe, lambda e, kc=kc, sub=sub, half=half, pi=pi: e.matmul(
                            G.ps[pi][:], lhsT=hT[:, kc, sub * 128:(sub + 1) * 128],
                            rhs=wqkv.view[:, kc, 2 * D + half * 512:2 * D + (half + 1) * 512],
                            start=(kc == 0), stop=(kc == 7)),
                            reads=[b_hT, wqkv.buf(kc, 2 * D + half * 512)], writes=[G.psb[pi]])
                    k.op(k.act, lambda e, sub=sub, half=half, pi=pi: e.activation(
                        out=vt[:, sub, half * 512:(half + 1) * 512], in_=G.ps[pi][:], func=AF.Copy),
                        reads=[G.psb[pi]], writes=[b_vt])
            if "v" in dbg:
                k.op(k.sp, lambda e, ti=ti: e.dma_start(out=G.vtok[:, ti * 4:(ti + 1) * 4, :], in_=vt),
                     reads=[b_vt], writes=[G.v_bufs[ti]], dma=True)
            for which in range(2 if "q" in dbg else 0):
                for h in range(8):
                    col0 = which * D + h * 128
                    pi = 3 + cnt % 2
                    pr = 6 + cnt % 2
                    sq2, b_sq2 = sqh[cnt % 2]
                    r2, b_r2 = r2s[cnt % 2]
                    qn, b_qn = qns[cnt % 2]
                    qb, b_qb = qnb[cnt % 2]
                    t1, b_t1 = t1s[cnt % 2]
                    t2, b_t2 = t2s[cnt % 2]
                    qf, b_qf = qfs[cnt % 2]
                    cnt += 1
                    for kc in range(8):
                        k.op(k.pe, lambda e, kc=kc, col0=col0, pi=pi: e.matmul(
                            G.ps[pi][:], lhsT=wqkv.view[:, kc, col0:col0 + 128], rhs=hT[:, kc, :],
                            start=(kc == 0), stop=(kc == 7)),
                            reads=[b_hT, wqkv.buf(kc, col0)], writes=[G.psb[pi]])
                    k.op(k.act, lambda e, pi=pi, sq2=sq2: e.activation(out=sq2, in_=G.ps[pi][:], func=AF.Square),
                         reads=[G.psb[pi]], writes=[b_sq2])
                    k.op(k.pe, lambda e, sq2=sq2: e.matmul(G.ps[5][:], lhsT=G.ones_bf[:], rhs=sq2, start=True, stop=True),
                         reads=[b_sq2, G.b_const], writes=[G.psb[5]])
                    k.op(k.act, lambda e, r2=r2: e.activation(out=r2[:], in_=G.ps[5][:], func=AF.Ln, bias=eps[:, 0:1],
                                                              scale=1.0 / 128), reads=[G.psb[5], b_eps], writes=[b_r2])
                    k.op(k.act, lambda e, r2=r2: e.activation(out=r2[:], in_=r2[:], func=AF.Exp, scale=-0.5),
                         reads=[b_r2], writes=[b_r2])
                    k.op(k.dve, lambda e, pi=pi, qn=qn, r2=r2, which=which: e.scalar_tensor_tensor(
                        out=qn[:], in0=G.ps[pi][:], scalar=qkg[:, which:which + 1], in1=r2[:], op0=ALU.mult, op1=ALU.mult),
                        reads=[G.psb[pi], b_r2, b_vec], writes=[b_qn])
                    k.op(k.act, lambda e, qn=qn, qb=qb: e.activation(out=qb, in_=qn[:], func=AF.Copy),
                         reads=[b_qn], writes=[b_qb])
                    k.op(k.pe, lambda e, pr=pr, qb=qb: e.matmul(G.ps[pr][:], lhsT=rot_bf, rhs=qb, start=True, stop=True),
                         reads=[b_qb, b_rot], writes=[G.psb[pr]])
                    k.op(k.pool, lambda e, t1=t1, qn=qn: e.tensor_tensor(out=t1[:], in0=qn[:], in1=cs[:, 0, :], op=ALU.mult),
                         reads=[b_qn, b_cs], writes=[b_t1])
                    k.op(k.dve, lambda e, t2=t2, pr=pr: e.tensor_tensor(out=t2[:], in0=G.ps[pr][:], in1=cs[:, 1, :], op=ALU.mult),
                         reads=[G.psb[pr], b_cs], writes=[b_t2])
                    k.op(k.pool, lambda e, t1=t1, t2=t2, qf=qf: e.tensor_tensor(out=qf, in0=t1[:], in1=t2[:], op=ALU.add),
                         reads=[b_t1, b_t2], writes=[b_qf])
                    dT = G.qT if which == 0 else G.kT
                    db = G.q_bufs if which == 0 else G.k_bufs
                    k.op(k.sp, lambda e, dT=dT, h=h, ti=ti, qf=qf: e.dma_start(out=dT[h, :, ti * T:(ti + 1) * T], in_=qf),
                         reads=[b_qf], writes=[db[h][ti]], dma=True)


def phase_M2(G, layer, wr, src, dst, sb_src, sb_dst):
    k, nc = G.k, G.nc
    with contextlib.ExitStack() as es:
        sb = lambda name, shape, dt: es.enter_context(nc.sbuf_tensor(G.uniq(name), list(shape), dt))
        off = 0
        qT, off = carve(G.regA, off, [128, S]); b_q = Buf("a_q")
        kT, off = carve(G.regA, off, [128, S]); b_k = Buf("a_k")
        vt, off = carve(G.regA, off, [128, 32, 128]); b_v = Buf("a_v")
        oTh, off = carve(G.regA, off, [128, S]); b_o = Buf("a_o")
        biasT, off = carve(G.regA, off, [128, S]); b_bT = Buf("a_bT")
        causal, off = carve(G.regA, off, [128, 512]); b_cz = Buf("a_causal")
        selrow, off = carve(G.regA, off, [128, 2048]); b_sr = Buf("a_selrow")
        km_bf, off = carve(G.regA, off, [128, 16]); b_kmb = Buf("a_kmb")
        PTs = []
        for i in range(4):
            v, off = carve(G.regA, off, [128, 256]); PTs.append((v, Buf(f"a_PT{i}")))
        assert off <= 49152
        past = sb("a_past", [128, 512], F32); b_past = Buf("a_past")
        km = sb("a_km", [128, 16], F32); b_km = Buf("a_km")
        gm = sb("a_gm", [128, 512], F32); b_gm = Buf("a_gm")
        top8 = sb("a_top8", [128, 32, 8], F32); b_top8 = Buf("a_top8")
        thr = sb("a_thr", [128, 32], F32); b_thr = Buf("a_thr")
        sel = sb("a_sel", [128, 512], F32); b_sel = Buf("a_sel")
        rdens = [(sb(f"a_rden{i}", [128, 256], F32), Buf(f"a_rden{i}")) for i in range(2)]
        k.op(k.sp, lambda e: e.dma_start(out=past[:], in_=G.c_past[:, :]), writes=[b_past], dma=True)
        k.op(k.pool, lambda e: e.dma_start(out=causal, in_=G.c_causal[:, :]), writes=[b_cz], dma=True)
        k.op(k.pool, lambda e: e.dma_start(out=selrow, in_=G.c_selrow[:, :]), writes=[b_sr], dma=True)
        k.op(k.pool, lambda e: e.memset(biasT, 0.0), writes=[b_bT])
        scnt = 0
        for h in range(8):
            k.op(k.sp, lambda e, h=h: e.dma_start(out=qT, in_=G.qT[h, :, :]), reads=G.q_bufs[h], writes=[b_q], dma=True)
            k.op(k.sp, lambda e, h=h: e.dma_start(out=kT, in_=G.kT[h, :, :]), reads=G.k_bufs[h], writes=[b_k], dma=True)
            k.op(k.sp, lambda e, h=h: e.dma_start(out=vt, in_=G.vtok[:, :, h * 128:(h + 1) * 128]),
                 reads=G.v_bufs, writes=[b_v], dma=True)
            k.op(k.dve, lambda e: e.tensor_reduce(out=km[:], in_=kT.rearrange("p (n j) -> p n j", j=256), axis=AX.X,
                                                  op=ALU.add), reads=[b_k], writes=[b_km])
            k.op(k.dve, lambda e: e.tensor_scalar(out=km_bf, in0=km[:], scalar1=1.0 / 256, scalar2=None, op0=ALU.mult),
                 reads=[b_km], writes=[b_kmb])
            for i in range(32):
                k.op(k.pe, lambda e, i=i: e.matmul(G.ps[0][:, i * 16:(i + 1) * 16], lhsT=qT[:, i * 128:(i + 1) * 128],
                                                   rhs=km_bf, start=True, stop=True),
                     reads=[b_q, b_kmb], writes=[G.psb[0]])
            k.op(k.dve, lambda e: e.tensor_tensor(out=gm[:], in0=G.ps[0][:], in1=past[:], op=ALU.add),
                 reads=[G.psb[0], b_past], writes=[b_gm])
            for i in range(32):
                k.op(k.dve, lambda e, i=i: e.max(out=top8[:, i, :], in_=gm[:, i * 16:(i + 1) * 16]),
                     reads=[b_gm], writes=[b_top8])
            k.op(k.dve, lambda e: e.tensor_scalar(out=thr[:], in0=top8[:, :, 2], scalar1=-1e29, scalar2=None, op0=ALU.max),
                 reads=[b_top8], writes=[b_thr])
            k.op(k.dve, lambda e: e.tensor_tensor(
                out=sel[:].rearrange("p (i n) -> p i n", n=16), in0=gm[:].rearrange("p (i n) -> p i n", n=16),
                in1=thr[:].unsqueeze(2).to_broadcast([128, 32, 16]), op=ALU.is_ge),
                reads=[b_gm, b_thr], writes=[b_sel])
            k.op(k.dve, lambda e: e.tensor_scalar(out=sel[:], in0=sel[:], scalar1=-1.0, scalar2=-NEG, op0=ALU.add,
                                                  op1=ALU.mult), reads=[b_sel], writes=[b_sel])
            for g in range(8):
                pi = g % 2
                for i4 in range(4):
                    i = g * 4 + i4
                    k.op(k.pe, lambda e, i=i, i4=i4, pi=pi: e.transpose(
                        out=G.ps[pi][0:16, i4 * 128:(i4 + 1) * 128], in_=sel[:, i * 16:(i + 1) * 16], identity=G.ident_f[:]),
                        reads=[b_sel, G.b_const], writes=[G.psb[pi]])
                k.op(k.act, lambda e, g=g, pi=pi: e.activation(out=biasT[0:16, g * 512:(g + 1) * 512],
                                                                in_=G.ps[pi][0:16, :], func=AF.Copy),
                     reads=[G.psb[pi]], writes=[b_bT])
            for j in range(16):
                qs = slice(j * 256, (j + 1) * 256)
                po, pd = (6, 7) if j % 2 == 0 else (0, 1)
                nkt = 2 * (j + 1)
                idx = 0
                for n in range(j + 1):
                    for kt in range(2):
                        kc0 = n * 256 + kt * 128
                        pi = 2 + scnt % 4
                        PT, b_PT = PTs[scnt % 4]
                        scnt += 1
                        k.op(k.pe, lambda e, kc0=kc0, qs=qs, pi=pi: e.matmul(
                            G.ps[pi][:, 0:256], lhsT=kT[:, kc0:kc0 + 128], rhs=qT[:, qs], start=True, stop=False),
                            reads=[b_k, b_q], writes=[G.psb[pi]])
                        if n < j:
                            k.op(k.pe, lambda e, n=n, qs=qs, pi=pi: e.matmul(
                                G.ps[pi][:, 0:256], lhsT=selrow[:, n * 128:(n + 1) * 128], rhs=biasT[:, qs],
                                start=False, stop=True), reads=[b_sr, b_bT], writes=[G.psb[pi]])
                        else:
                            k.op(k.pe, lambda e, kt=kt, pi=pi: e.matmul(
                                G.ps[pi][:, 0:256], lhsT=G.ident_bf[:], rhs=causal[:, kt * 256:(kt + 1) * 256],
                                start=False, stop=True), reads=[b_cz, G.b_const], writes=[G.psb[pi]])
                        k.op(k.act, lambda e, pi=pi, PT=PT: e.activation(out=PT, in_=G.ps[pi][:, 0:256], func=AF.Exp),
                             reads=[G.psb[pi]], writes=[b_PT])
                        k.op(k.pe, lambda e, n=n, kt=kt, PT=PT, po=po, idx=idx, nkt=nkt: e.matmul(
                            G.ps[po][:, 0:256], lhsT=vt[:, n * 2 + kt, :], rhs=PT, start=(idx == 0), stop=(idx == nkt - 1)),
                            reads=[b_v, b_PT], writes=[G.psb[po]])
                        k.op(k.pe, lambda e, PT=PT, pd=pd, idx=idx, nkt=nkt: e.matmul(
                            G.ps[pd][:, 0:256], lhsT=G.ones_bf[:], rhs=PT, start=(idx == 0), stop=(idx == nkt - 1)),
                            reads=[G.b_const, b_PT], writes=[G.psb[pd]])
                        idx += 1
                rd, b_rd = rdens[j % 2]
                k.op(k.dve, lambda e, rd=rd, pd=pd: e.reciprocal(out=rd[:], in_=G.ps[pd][:, 0:256]),
                     reads=[G.psb[pd]], writes=[b_rd])
                k.op(k.dve, lambda e, rd=rd, po=po, qs=qs: e.tensor_tensor(out=oTh[:, qs], in0=G.ps[po][:, 0:256], in1=rd[:],
                                                                          op=ALU.mult),
                     reads=[G.psb[po], b_rd], writes=[b_o])
            k.op(k.sp, lambda e, h=h: e.dma_start(out=G.oT[:, h, :], in_=oTh), reads=[b_o], writes=[G.o_bufs[h]], dma=True)


def phase_M3(G, layer, wr, src, dst, sb_src, sb_dst):
    k, nc = G.k, G.nc
    W = G.w[("moba", layer)]
    with contextlib.ExitStack() as es:
        sb = lambda name, shape, dt: es.enter_context(nc.sbuf_tensor(G.uniq(name), list(shape), dt))
        regC = sb("regC", [128, 8 * D], BF16)
        wout = WRegion(G, regC, None, W["w_out"], D, D, blk=1024, name="wout")
        xts = [(sb(f"o_xt{i}", [128, 8, T], F32), Buf(f"o_xt{i}")) for i in range(1)]
        ots = [(sb(f"o_ot{i}", [128, 8, T], BF16), Buf(f"o_ot{i}")) for i in range(2)]
        for ti in range(NT):
            xt, b_xt = xts[0]
            ot, b_ot = ots[ti % 2]
            k.op(k.sp, lambda e, ti=ti, xt=xt: e.dma_start(out=xt[:], in_=src[:, :, ti * T:(ti + 1) * T]),
                 reads=sb_src[2 * ti:2 * ti + 2], writes=[b_xt], dma=True)
            k.op(k.sp, lambda e, ti=ti, ot=ot: e.dma_start(out=ot[:], in_=G.oT[:, :, ti * T:(ti + 1) * T]),
                 reads=G.o_bufs, writes=[b_ot], dma=True)
            for oc in range(8):
                for h in range(8):
                    k.op(k.pe, lambda e, oc=oc, h=h, ot=ot: e.matmul(
                        G.ps[oc][:], lhsT=wout.view[:, h, oc * 128:(oc + 1) * 128], rhs=ot[:, h, :],
                        start=(h == 0), stop=(h == 7)),
                        reads=[wout.buf(h, oc * 128), b_ot], writes=[G.psb[oc]])
                k.op(k.dve, lambda e, oc=oc, xt=xt: e.tensor_tensor(out=xt[:, oc, :], in0=G.ps[oc][:], in1=xt[:, oc, :],
                                                                   op=ALU.add),
                     reads=[G.psb[oc], b_xt], writes=[b_xt])
            k.op(k.sp, lambda e, ti=ti, xt=xt: e.dma_start(out=dst[:, :, ti * T:(ti + 1) * T], in_=xt[:]),
                 reads=[b_xt], writes=sb_dst[2 * ti:2 * ti + 2], dma=True)


TH = 256
NTH = S // TH


def phase_H1(G, layer, win, src, dst, sb_src, sb_dst):
    k, nc = G.k, G.nc
    W = G.w[("hgrn", layer)]
    slot = layer // 2
    with contextlib.ExitStack() as es:
        sb = lambda name, shape, dt: es.enter_context(nc.sbuf_tensor(G.uniq(name), list(shape), dt))
        regC = sb("regC", [128, 8 * D], BF16)
        wout = WRegion(G, regC, None, W["w_out"], D, D, blk=1024, name="hwout")
        vec = sb("h_vec", [128, 25], F32); b_vec = Buf("h_vec")
        k.op(k.sp, lambda e: e.dma_start(out=vec[:], in_=W["vec"][:, :]), writes=[b_vec], dma=True)
        gain = vec[:, 0:8]
        ogain = vec[:, 24:25]
        cst = sb("h_cst", [128, 2], F32); b_cst = Buf("h_cst")
        k.op(k.pool, lambda e: e.memset(cst[:, 0:1], EPS), writes=[b_cst])
        k.op(k.pool, lambda e: e.memset(cst[:, 1:2], 1.0), writes=[b_cst])
        eps = cst
        lbv = sb("h_lb", [128, 3, 8], F32); b_lb = Buf("h_lb")
        if slot == 0:
            k.op(k.pool, lambda e: e.memset(lbv[:, 0, :], 0.0), writes=[b_lb])
        else:
            k.op(k.dve, lambda e: e.tensor_tensor(out=lbv[:, 0, :], in0=vec[:, 8:16], in1=vec[:, 16:24], op=ALU.subtract),
                 reads=[b_vec], writes=[b_lb])
            k.op(k.act, lambda e: e.activation(out=lbv[:, 0, :], in_=lbv[:, 0, :], func=AF.Exp), reads=[b_lb], writes=[b_lb])
            k.op(k.dve, lambda e: e.tensor_scalar(out=lbv[:, 0, :], in0=lbv[:, 0, :], scalar1=1.0, scalar2=None, op0=ALU.add),
                 reads=[b_lb], writes=[b_lb])
            k.op(k.dve, lambda e: e.reciprocal(out=lbv[:, 0, :], in_=lbv[:, 0, :]), reads=[b_lb], writes=[b_lb])
        k.op(k.dve, lambda e: e.tensor_scalar(out=lbv[:, 1, :], in0=lbv[:, 0, :], scalar1=-1.0, scalar2=1.0, op0=ALU.mult,
                                              op1=ALU.add), reads=[b_lb], writes=[b_lb])
        k.op(k.dve, lambda e: e.tensor_scalar(out=lbv[:, 2, :], in0=lbv[:, 1, :], scalar1=-1.0, scalar2=None, op0=ALU.mult),
             reads=[b_lb], writes=[b_lb])
        tri = sb("h_tri", [128, 128], F32); b_tri = Buf("h_tri")
        k.op(k.sp, lambda e: e.dma_start(out=tri[:], in_=G.c_tri[:, :]), writes=[b_tri], dma=True)
        smask = sb("h_smask", [128, TH], F32); b_sm = Buf("h_smask")
        k.op(k.sp, lambda e: e.dma_start(out=smask[:], in_=G.c_scan[:, 0:TH]), writes=[b_sm], dma=True)
        xt = sb("h_xt", [128, 8, TH], F32); b_xt = Buf("h_xt")
        xo = sb("h_xo", [128, 8, TH], F32); b_xo = Buf("h_xo")
        St = sb("h_S", [128, 8, 128], F32); b_S = [Buf(f"h_S{h}") for h in range(8)]
        k.op(k.pool, lambda e: e.memset(St[:], 0.0), writes=b_S)
        rstd = (sb("h_rstd", [128, TH], F32), Buf("h_rstd"))
        sets = []
        for i in range(2):
            d_ = dict(
                b=[(sb(f"h_b{i}_{j}", [128, TH], F32), Buf(f"h_b{i}_{j}")) for j in range(4)],
                qin=(sb(f"h_qin{i}", [128, TH], F32), Buf(f"h_qin{i}")),
                egl=(sb(f"h_egl{i}", [128, 4], F32), Buf(f"h_egl{i}")),
                r2=(sb(f"h_r2{i}", [128, TH], F32), Buf(f"h_r2{i}")),
                tmp=(sb(f"h_tmp{i}", [128, TH], F32), Buf(f"h_tmp{i}")),
            )
            sets.append(d_)
        off = 8 * 4 * D
        hTs = []
        for i in range(2):
            v, off = carve(G.regA, off, [128, 8, TH]); hTs.append((v, Buf(f"h_hT{i}")))
        vtok, off = carve(G.regA, off, [128, 2, D]); b_vtok = Buf("h_vtok")
        sgate, off = carve(G.regA, off, [128, 8, TH]); b_sg = [Buf(f"h_sg{h}") for h in range(8)]
        oTn, off = carve(G.regA, off, [128, 8, TH]); b_oTn = [Buf(f"h_oTn{h}") for h in range(8)]
        sqs = []
        for i in range(2):
            v, off = carve(G.regA, off, [128, TH]); sqs.append((v, Buf(f"h_sq{i}")))
        for i in range(2):
            d_ = sets[i]
            v, off = carve(G.regA, off, [128, TH]); d_["qrel"] = (v, Buf(f"h_qrel{i}"))
            v, off = carve(G.regA, off, [128, TH]); d_["krel"] = (v, Buf(f"h_krel{i}"))
            v, off = carve(G.regA, off, [128, 2, 128]); d_["attT"] = (v, Buf(f"h_attT{i}"))
            v, off = carve(G.regA, off, [128, 4, 128]); d_["kz"] = (v, Buf(f"h_kz{i}"))
            v, off = carve(G.regA, off, [128, TH]); d_["sqo"] = (v, Buf(f"h_sqo{i}"))
            k.op(k.pool, lambda e, v=d_["kz"][0]: e.memset(v, 0.0), writes=[d_["kz"][1]])
        assert off <= 49152, off
        L = dict(sq=sqs, rstd=rstd, eps=eps, b_eps=b_cst, b_vec=b_vec)
        pA = [Buf(f"h_pA{b}") for b in range(8)]
        pB = [Buf(f"h_pB{b}") for b in range(8)]
        lo = lambda b: G.ps[b][:, 0:TH]
        hi = lambda b: G.ps[b][:, TH:2 * TH]
        b_dS = [[Buf(f"h_dS{i}_{c}") for c in range(4)] for i in range(2)]
        dS_ps = lambda i, c: G.ps[7 - i][:, c * 128:(c + 1) * 128]
        bank = lambda b: [pA[b], pB[b]] if b < 6 else b_dS[7 - b]
        SCL = 128.0 ** -0.5

        k.op(k.sp, lambda e: e.dma_start(out=xt[:], in_=src[:, :, 0:TH]), reads=[sb_src[0]], writes=[b_xt], dma=True)
        for ti in range(NTH):
            c0 = ti * TH
            hT, b_hT = hTs[ti % 2]
            rmsnorm_tile(G, L, xt, b_xt, gain, hT, b_hT, ps_i=0, ps_ap=lo(0), ps_buf=bank(0))
            if ti + 1 < NTH:
                k.op(k.sp, lambda e, ti=ti: e.dma_start(out=xt[:], in_=src[:, :, (ti + 1) * TH:(ti + 2) * TH]),
                     reads=[sb_src[ti + 1]], writes=[b_xt], dma=True)
            k.op(k.sp, lambda e, c0=c0: e.dma_start(out=xo[:], in_=src[:, :, c0:c0 + TH]), reads=[sb_src[ti]],
                 writes=[b_xo], dma=True)
            for sub in range(2):
                for half in range(2):
                    pi = 1 + (sub * 2 + half) % 2
                    for kc in range(8):
                        k.op(k.pe, lambda e, kc=kc, sub=sub, half=half, pi=pi: e.matmul(
                            G.ps[pi][:], lhsT=hT[:, kc, sub * 128:(sub + 1) * 128],
                            rhs=win.view[:, kc, 2 * D + half * 512:2 * D + (half + 1) * 512],
                            start=(kc == 0), stop=(kc == 7)),
                            reads=[b_hT, win.buf(kc, 2 * D + half * 512)], writes=bank(pi))
                    k.op(k.act, lambda e, sub=sub, half=half, pi=pi: e.activation(
                        out=vtok[:, sub, half * 512:(half + 1) * 512], in_=G.ps[pi][:], func=AF.Copy),
                        reads=[pA[pi], pB[pi]], writes=[b_vtok])
            for h in range(8):
                pi = 3 + h % 2
                for kc in range(8):
                    k.op(k.pe, lambda e, kc=kc, h=h, pi=pi: e.matmul(
                        lo(pi), lhsT=win.view[:, kc, 3 * D + h * 128:3 * D + (h + 1) * 128], rhs=hT[:, kc, :],
                        start=(kc == 0), stop=(kc == 7)),
                        reads=[b_hT, win.buf(kc, 3 * D + h * 128)], writes=bank(pi))
                k.op(k.act, lambda e, h=h, pi=pi: e.activation(out=sgate[:, h, :], in_=lo(pi), func=AF.Silu),
                     reads=[pA[pi]], writes=[b_sg[h]])
            for hp in range(4):
                hh = (2 * hp, 2 * hp + 1)
                for i, h in enumerate(hh):
                    st = sets[i]
                    pq, pz = 0 + i, 2 + i
                    (b1, B1), (b2, B2), (b3, B3), (b4, B4) = st["b"]
                    for kc in range(8):
                        k.op(k.pe, lambda e, kc=kc, h=h, pq=pq: e.matmul(
                            lo(pq), lhsT=win.view[:, kc, h * 128:(h + 1) * 128], rhs=hT[:, kc, :],
                            start=(kc == 0), stop=(kc == 7)), reads=[b_hT, win.buf(kc, h * 128)], writes=bank(pq))
                    for kc in range(8):
                        k.op(k.pe, lambda e, kc=kc, h=h, pz=pz: e.matmul(
                            lo(pz), lhsT=win.view[:, kc, D + h * 128:D + (h + 1) * 128], rhs=hT[:, kc, :],
                            start=(kc == 0), stop=(kc == 7)), reads=[b_hT, win.buf(kc, D + h * 128)], writes=bank(pz))
                    k.op(k.act, lambda e, b1=b1, pz=pz: e.activation(out=b1[:], in_=lo(pz), func=AF.Exp, scale=-1.0),
                         reads=[pA[pz]], writes=[B1])
                    k.op(k.pool, lambda e, b1=b1: e.tensor_scalar(out=b1[:], in0=b1[:], scalar1=1.0, scalar2=None, op0=ALU.add),
                         reads=[B1], writes=[B1])
                    k.op(k.dve, lambda e, b1=b1: e.reciprocal(out=b1[:], in_=b1[:]), reads=[B1], writes=[B1])
                    k.op(k.act, lambda e, b1=b1, b2=b2, h=h: e.activation(out=b2[:], in_=b1[:], func=AF.Ln, bias=lbv[:, 0, h:h + 1],
                                                                        scale=lbv[:, 1, h:h + 1]),
                         reads=[B1, b_lb], writes=[B2])
                    k.op(k.dve, lambda e, b1=b1, h=h: e.tensor_scalar(out=b1[:], in0=b1[:], scalar1=lbv[:, 2, h:h + 1],
                                                                     scalar2=lbv[:, 1, h:h + 1], op0=ALU.mult, op1=ALU.add),
                         reads=[B1, b_lb], writes=[B1])
                    k.op(k.dve, lambda e, b2=b2, b3=b3: e.tensor_tensor_scan(out=b3[:], data0=smask[:], data1=b2[:], initial=0.0,
                                                                            op0=ALU.mult, op1=ALU.add),
                         reads=[B2, b_sm], writes=[B3])
                    G3 = b3[:].rearrange("p (c t) -> p c t", t=64)
                    k.op(k.dve, lambda e, b2=b2, G3=G3: e.tensor_tensor(
                        out=b2[:].rearrange("p (c t) -> p c t", t=64), in0=G3, in1=G3[:, :, 31:32].to_broadcast([128, 4, 64]),
                        op=ALU.subtract), reads=[B3], writes=[B2])
                    k.op(k.act, lambda e, b2=b2, b4=b4: e.activation(out=b4[:], in_=b2[:], func=AF.Exp), reads=[B2], writes=[B4])
                    qrel, Bqrel = st["qrel"]
                    k.op(k.dve, lambda e, qrel=qrel, b4=b4, pq=pq: e.scalar_tensor_tensor(
                        out=qrel, in0=lo(pq), scalar=SCL, in1=b4[:], op0=ALU.mult, op1=ALU.mult),
                        reads=[pA[pq], B4], writes=[Bqrel])
                    k.op(k.act, lambda e, b2=b2: e.activation(out=b2[:], in_=b2[:], func=AF.Exp, scale=-1.0), reads=[B2], writes=[B2])
                    krel, Bkrel = st["krel"]
                    k.op(k.dve, lambda e, krel=krel, b1=b1, b2=b2: e.tensor_tensor(out=krel, in0=b1[:], in1=b2[:], op=ALU.mult),
                         reads=[B1, B2], writes=[Bkrel])
                    k.op(k.act, lambda e, b3=b3, b4=b4: e.activation(out=b4[:], in_=b3[:], func=AF.Exp), reads=[B3], writes=[B4])
                    qin, Bqin = st["qin"]
                    k.op(k.dve, lambda e, qin=qin, b4=b4, pq=pq: e.scalar_tensor_tensor(
                        out=qin[:], in0=lo(pq), scalar=SCL, in1=b4[:], op0=ALU.mult, op1=ALU.mult),
                        reads=[pA[pq], B4], writes=[Bqin])
                    k.op(k.dve, lambda e, b2=b2, G3=G3: e.tensor_tensor(
                        out=b2[:].rearrange("p (c t) -> p c t", t=64), in0=G3, in1=G3[:, :, 63:64].to_broadcast([128, 4, 64]),
                        op=ALU.subtract), reads=[B3], writes=[B2])
                    k.op(k.act, lambda e, b2=b2: e.activation(out=b2[:], in_=b2[:], func=AF.Exp, scale=-1.0), reads=[B2], writes=[B2])
                    k.op(k.dve, lambda e, b1=b1, b2=b2, b4=b4: e.tensor_tensor(out=b4[:], in0=b1[:], in1=b2[:], op=ALU.mult),
                         reads=[B1, B2], writes=[B4])
                    egl, Begl = st["egl"]
                    k.op(k.act, lambda e, egl=egl, G3=G3: e.activation(out=egl[:], in_=G3[:, :, 63], func=AF.Exp),
                         reads=[B3], writes=[Begl])
                    kTp = lo(5) if i == 0 else hi(5)
                    BkT = pA[5] if i == 0 else pB[5]
                    for pr in range(2):
                        k.op(k.pe, lambda e, pr=pr, b4=b4, kTp=kTp: e.transpose(
                            out=kTp[:, pr * 128:(pr + 1) * 128], in_=b4[:, pr * 128:(pr + 1) * 128],
                            identity=G.ident_f[:]), reads=[B4, G.b_const], writes=bank(5))
                    kz, Bkz = st["kz"]
                    kzv = kz.rearrange("p (pr cc) d -> p pr cc d", cc=2)
                    k.op(k.act, lambda e, kzv=kzv, kTp=kTp: e.activation(
                        out=kzv[0:64, :, 0, :], in_=kTp[0:64, :].rearrange("p (pr d) -> p pr d", d=128),
                        func=AF.Copy), reads=[BkT], writes=[Bkz])
                    k.op(k.act, lambda e, kzv=kzv, kTp=kTp: e.activation(
                        out=kzv[64:128, :, 1, :], in_=kTp[64:128, :].rearrange("p (pr d) -> p pr d", d=128),
                        func=AF.Copy), reads=[BkT], writes=[Bkz])
                    aTp = lo(4) if i == 0 else hi(4)
                    BaT = pA[4] if i == 0 else pB[4]
                    for pr in range(2):
                        k.op(k.pe, lambda e, pr=pr, aTp=aTp, krel=krel, qrel=qrel: e.matmul(
                            aTp[:, pr * 128:(pr + 1) * 128], lhsT=krel[:, pr * 128:(pr + 1) * 128],
                            rhs=qrel[:, pr * 128:(pr + 1) * 128], start=True, stop=True),
                            reads=[Bkrel, Bqrel], writes=bank(4))
                    attT, BattT = st["attT"]
                    k.op(k.dve, lambda e, attT=attT, aTp=aTp: e.tensor_tensor(
                        out=attT, in0=aTp.rearrange("p (pr t) -> p pr t", t=128),
                        in1=tri[:].unsqueeze(1).to_broadcast([128, 2, 128]), op=ALU.mult),
                        reads=[BaT, b_tri], writes=[BattT])
                    for c in range(4):
                        k.op(k.pe, lambda e, c=c, i=i, kz=kz, h=h: e.matmul(
                            dS_ps(i, c), lhsT=kz[:, c, :], rhs=vtok[:, c // 2, h * 128:(h + 1) * 128], start=True, stop=True),
                            reads=[Bkz, b_vtok], writes=bank(7 - i))
                for c in range(4):
                    for i, h in enumerate(hh):
                        st = sets[i]
                        qin, Bqin = st["qin"]
                        attT, BattT = st["attT"]
                        egl, Begl = st["egl"]
                        pr, cc = c // 2, c % 2
                        k.op(k.pe, lambda e, c=c, i=i, h=h, qin=qin: e.matmul(
                            hi(i)[:, c * 64:(c + 1) * 64], lhsT=St[:, h, :], rhs=qin[:, c * 64:(c + 1) * 64],
                            start=True, stop=False), reads=[b_S[h], Bqin], writes=bank(i))
                        k.op(k.pe, lambda e, c=c, i=i, h=h, attT=attT, pr=pr, cc=cc: e.matmul(
                            hi(i)[:, c * 64:(c + 1) * 64], lhsT=vtok[:, pr, h * 128:(h + 1) * 128],
                            rhs=attT[:, pr, cc * 64:(cc + 1) * 64], start=False, stop=True),
                            reads=[b_vtok, BattT], writes=bank(i))
                        k.op(k.dve, lambda e, c=c, i=i, h=h, egl=egl: e.scalar_tensor_tensor(
                            out=St[:, h, :], in0=St[:, h, :], scalar=egl[:, c:c + 1], in1=dS_ps(i, c),
                            op0=ALU.mult, op1=ALU.add),
                            reads=[b_S[h], Begl, b_dS[i][c]], writes=[b_S[h]])
                for i, h in enumerate(hh):
                    st = sets[i]
                    sqo, Bsqo = st["sqo"]
                    r2, Br2 = st["r2"]
                    tmp, Btmp = st["tmp"]
                    k.op(k.act, lambda e, sqo=sqo, i=i: e.activation(out=sqo, in_=hi(i), func=AF.Square),
                         reads=[pB[i]], writes=[Bsqo])
                    k.op(k.pe, lambda e, sqo=sqo, i=i: e.matmul(hi(2 + i), lhsT=G.ones_bf[:], rhs=sqo, start=True, stop=True),
                         reads=[Bsqo, G.b_const], writes=bank(2 + i))
                    k.op(k.act, lambda e, r2=r2, i=i: e.activation(out=r2[:], in_=hi(2 + i), func=AF.Ln,
                                                                   bias=cst[:, 0:1], scale=1.0 / 128),
                         reads=[pB[2 + i], b_cst], writes=[Br2])
                    k.op(k.act, lambda e, r2=r2: e.activation(out=r2[:], in_=r2[:], func=AF.Exp, scale=-0.5), reads=[Br2], writes=[Br2])
                    k.op(k.dve, lambda e, tmp=tmp, r2=r2, i=i: e.scalar_tensor_tensor(
                        out=tmp[:], in0=hi(i), scalar=ogain, in1=r2[:], op0=ALU.mult, op1=ALU.mult),
                        reads=[pB[i], Br2, b_vec], writes=[Btmp])
                    k.op(k.pool, lambda e, tmp=tmp, h=h: e.tensor_tensor(out=oTn[:, h, :], in0=tmp[:], in1=sgate[:, h, :], op=ALU.mult),
                         reads=[Btmp, b_sg[h]], writes=[b_oTn[h]])
            for oc in range(8):
                pi = oc % 2
                for h in range(8):
                    k.op(k.pe, lambda e, oc=oc, h=h, pi=pi: e.matmul(
                        lo(pi), lhsT=wout.view[:, h, oc * 128:(oc + 1) * 128], rhs=oTn[:, h, :],
                        start=(h == 0), stop=(h == 7)), reads=[wout.buf(h, oc * 128), b_oTn[h]], writes=bank(pi))
                k.op(k.dve, lambda e, oc=oc, pi=pi: e.tensor_tensor(out=xo[:, oc, :], in0=lo(pi), in1=xo[:, oc, :],
                                                                   op=ALU.add), reads=[pA[pi], b_xo], writes=[b_xo])
            k.op(k.sp, lambda e, c0=c0: e.dma_start(out=dst[:, :, c0:c0 + TH], in_=xo[:]), reads=[b_xo], writes=[sb_dst[ti]],
                 dma=True)


PHASE_FN = {"F1": phase_F1, "F2": phase_F2, "M1": phase_M1, "M2": phase_M2, "M3": phase_M3, "H1": phase_H1}


def fm(v, n):
    return np.ascontiguousarray(np.asarray(v, np.float32).reshape(n, 128).T)


def host_consts(kinds):
    c = {"c_ones": np.ones((128, 128), np.float32), "c_ident": np.eye(128, dtype=np.float32)}
    if "moba" in kinds:
        rot = np.zeros((128, 128), np.float32)
        for m_ in range(64):
            rot[m_ + 64, m_] = -1.0
            rot[m_, m_ + 64] = 1.0
        c["c_rot"] = rot
        inv = (1.0 / (np.float32(10000.0) ** (np.arange(0, 128, 2, dtype=np.float32) / np.float32(128)))).astype(np.float32)
        ang = (np.arange(S, dtype=np.float32)[:, None] * inv[None, :]).astype(np.float32)
        ang = np.concatenate([ang, ang], axis=-1)
        c["c_cos"] = np.ascontiguousarray(np.cos(ang).astype(np.float32).T)
        c["c_sin"] = np.ascontiguousarray(np.sin(ang).astype(np.float32).T)
        past = np.full((32, 16), -1e30, np.float32)
        for i in range(32):
            past[i, :i // 2] = 0.0
        c["c_past"] = np.ascontiguousarray(np.broadcast_to(past.reshape(1, 512), (128, 512)))
        causal = np.full((128, 2, 256), NEG, np.float32)
        for kt in range(2):
            for p in range(128):
                causal[p, kt, kt * 128 + p:] = 0.0
        c["c_causal"] = causal.reshape(128, 512)
        selrow = np.zeros((128, 16, 128), np.float32)
        for n_ in range(16):
            selrow[n_, n_, :] = 1.0
        c["c_selrow"] = selrow.reshape(128, 2048)
    if "hgrn" in kinds:
        tri = np.zeros((128, 128), np.float32)
        for s_ in range(128):
            for t_ in range(128):
                if s_ // 64 == t_ // 64 and s_ <= t_:
                    tri[s_, t_] = 1.0
        c["c_tri"] = tri
        sm = np.ones((128, T), np.float32)
        sm[:, ::64] = 0.0
        c["c_scan"] = sm
    return c


def stage_inputs(inputs, stages):
    m = {}
    for kind, l in stages:
        if kind == "ffn":
            m[f"ffn_w_up_{l}"] = np.ascontiguousarray(inputs["ffn_w_up"][l])
            m[f"ffn_w_dn_{l}"] = np.ascontiguousarray(inputs["ffn_w_down"][l])
            cw = inputs["ffn_conv_w"][l]
            m[f"ffn_vec_{l}"] = np.ascontiguousarray(np.concatenate(
                [fm(inputs["ffn_norm"][l], 8), fm(cw[0], 24), fm(cw[1], 24), fm(cw[2], 24),
                 fm(inputs["ffn_conv_b"][l], 24)], axis=1))
        elif kind == "hgrn":
            sl = l // 2
            m[f"hgrn_w_in_{l}"] = np.ascontiguousarray(inputs["hgrn_w_in"][sl])
            m[f"hgrn_w_out_{l}"] = np.ascontiguousarray(inputs["hgrn_w_out"][sl])
            m[f"hgrn_vec_{l}"] = np.ascontiguousarray(np.concatenate(
                [fm(inputs["attn_norm"][l], 8), fm(inputs["hgrn_lb"][0], 8), fm(inputs["hgrn_lb"][1], 8),
                 fm(inputs["hgrn_out_norm"][sl], 1)], axis=1))
        elif kind == "moba":
            sl = l // 2
            m[f"moba_w_qkv_{l}"] = np.ascontiguousarray(inputs["moba_w_qkv"][sl])
            m[f"moba_w_out_{l}"] = np.ascontiguousarray(inputs["moba_w_out"][sl])
            m[f"moba_vec_{l}"] = np.ascontiguousarray(np.concatenate(
                [fm(inputs["attn_norm"][l], 8), fm(inputs["moba_q_norm"][sl], 1), fm(inputs["moba_k_norm"][sl], 1)], axis=1))
    return m


def x_to_dev(xb):
    return np.ascontiguousarray(xb.T.reshape(8, 128, S).transpose(1, 0, 2))


def x_from_dev(y):
    return np.ascontiguousarray(y.transpose(1, 0, 2).reshape(D, S).T)


FUSED = True
LAYER_STAGES = [[("hgrn", 0), ("ffn", 0)], [("moba", 1), ("ffn", 1)], [("hgrn", 2), ("ffn", 2)], [("moba", 3), ("ffn", 3)]]


def kernel(**inputs):
    inputs = {k_: np.asarray(v) for k_, v in inputs.items()}
    x = inputs["x"].astype(np.float32)
    nb = x.shape[0]
    groups = [sum(LAYER_STAGES, [])] if FUSED else LAYER_STAGES
    xs = [x_to_dev(x[b]) for b in range(nb)]
    for stages in groups:
        P = build_program(stages)
        shared = dict(host_consts({kd for kd, _ in stages}))
        shared.update(stage_inputs(inputs, stages))
        shared = {k_: v for k_, v in shared.items() if k_ in P.ext_in}
        missing = set(P.ext_in) - set(shared) - {"xin"}
        assert not missing, missing
        in_maps = [dict(shared, xin=xs[b]) for b in range(nb)]
        res = run_bass_kernel_spmd(P.nc, in_maps, core_ids=list(range(nb)))
        xs = [np.asarray(res.results[b]["yout"], dtype=np.float32) for b in range(nb)]
    return np.stack([x_from_dev(xs[b]) for b in range(nb)]).astype(np.float32)
```

```python
import contextlib
import numpy as np
import concourse.bass as bass
import concourse.mybir as mybir
from concourse.bass_utils import run_bass_kernel_spmd

F32 = mybir.dt.float32
BF16 = mybir.dt.bfloat16
AF = mybir.ActivationFunctionType
ALU = mybir.AluOpType
AX = mybir.AxisListType

D = 1024
S = 4096
T = 512
NT = S // T
DEPTH = 4
FF = 3072
EPS = 1e-6
NEG = -30000.0


class Sem:
    __slots__ = ("h", "v")

    def __init__(self, h):
        self.h = h
        self.v = 0


class Buf:
    __slots__ = ("name", "w", "r")

    def __init__(self, name=""):
        self.name = name
        self.w = None
        self.r = {}


class Eng:
    def __init__(self, k, name, h, is_pe=False):
        self.k = k
        self.name = name
        self.h = h
        self.is_pe = is_pe
        self.sem = k.new_sem(name)
        self.dma_sems = []
        self.dma_i = 0
        self.waited = {}
        self.n_ops = 0
        self.n_waits = 0

    def next_dma_sem(self):
        if not self.dma_sems:
            n = 24 if self.name == "pool" else 16
            self.dma_sems = [self.k.new_sem(f"{self.name}_dma{i}") for i in range(n)]
        s = self.dma_sems[self.dma_i % len(self.dma_sems)]
        self.dma_i += 1
        return s


class K:
    def __init__(self, nc, es):
        self.nc = nc
        self.es = es
        self.sems = []
        self.pe = Eng(self, "pe", nc.tensor, is_pe=True)
        self.act = Eng(self, "act", nc.scalar)
        self.dve = Eng(self, "dve", nc.vector)
        self.pool = Eng(self, "pool", nc.gpsimd)
        self.sp = Eng(self, "sp", nc.sync)
        self.engs = [self.pe, self.act, self.dve, self.pool, self.sp]

    def new_sem(self, name):
        h = self.es.enter_context(self.nc.semaphore(f"s_{name}_{len(self.sems)}"))
        s = Sem(h)
        self.sems.append(s)
        return s

    def op(self, eng, fn, reads=(), writes=(), dma=False, after=None):
        deps = {}
        if after:
            for s, v in after.items():
                if deps.get(s, 0) < v:
                    deps[s] = v
        for b in reads:
            if b.w is not None and deps.get(b.w[0], 0) < b.w[1]:
                deps[b.w[0]] = b.w[1]
        for b in writes:
            if b.w is not None and deps.get(b.w[0], 0) < b.w[1]:
                deps[b.w[0]] = b.w[1]
            for s, v in b.r.items():
                if deps.get(s, 0) < v:
                    deps[s] = v
        for s, v in deps.items():
            if eng.is_pe and s is eng.sem:
                continue
            if eng.waited.get(s, 0) >= v:
                continue
            eng.h.wait_ge(s.h, v)
            eng.waited[s] = v
            eng.n_waits += 1
        if dma:
            ds = eng.next_dma_sem()
            if ds.v > 0 and eng.waited.get(ds, 0) < ds.v:
                eng.h.wait_ge(ds.h, ds.v)
                eng.waited[ds] = ds.v
                eng.n_waits += 1
        ins = fn(eng.h)
        eng.n_ops += 1
        if dma:
            s = ds
            s.v += 16
            ins.then_inc(s.h, 16)
        else:
            if eng.sem.v >= 30000:
                eng.sem = self.new_sem(eng.name)
            s = eng.sem
            s.v += 1
            ins.then_inc(s.h, 1)
        ev = (s, s.v)
        for b in reads:
            if b.r.get(s, 0) < s.v:
                b.r[s] = s.v
        for b in writes:
            b.w = ev
            b.r = {}
        return ev

    def snapshot(self):
        return {s: s.v for s in self.sems if s.v > 0}

    def barrier(self, snap, engines=None):
        for e in (engines or self.engs):
            for s, v in snap.items():
                if e.waited.get(s, 0) >= v:
                    continue
                e.h.wait_ge(s.h, v)
                e.waited[s] = v
                e.n_waits += 1


class Prog:
    def __init__(self, stages):
        self.stages = stages
        self.nc = bass.Bass("TRN2", target_bir_lowering=False)
        self.ext_in = {}

    def dram_in(self, name, shape, dt=F32):
        t = self.nc.dram_tensor(name, list(shape), dt, kind="ExternalInput")
        self.ext_in[name] = tuple(shape)
        return t.ap()

    def dram_tmp(self, name, shape, dt):
        kind = "ExternalOutput" if DEBUG_SCRATCH else "Internal"
        return self.nc.dram_tensor(name, list(shape), dt, kind=kind).ap()


DEBUG_SCRATCH = False
DEBUG_ONLY = None
SUBPHASES = {"ffn": ["F1", "F2"], "moba": ["M1", "M2", "M3"], "hgrn": ["H1"]}
NEEDS = {"F1": ("A", "w_up", D, 2 * FF, 2048), "F2": ("B", "w_dn", FF, D, 1024),
         "M1": ("A", "w_qkv", D, 3 * D, 1024), "H1": ("A", "w_in", D, 4 * D, 2048),
         "M2": ("A", None, 0, 0, 0)}


def build_program(stages):
    P = Prog(stages)
    nc = P.nc
    with contextlib.ExitStack() as es:
        k = K(nc, es)
        G = _Globals(P, k, es)
        n = len(stages)
        subs = []
        for i, (kind, layer) in enumerate(stages):
            io = dict(src=G.xin if i == 0 else G.xres, dst=G.yout if i == n - 1 else G.xres,
                      sb_src=G.xin_bufs if i == 0 else G.xres_bufs,
                      sb_dst=G.yout_bufs if i == n - 1 else G.xres_bufs)
            for sp in SUBPHASES[kind]:
                subs.append((sp, kind, layer, io))
        loaded = {}
        last_user = {"A": -1, "B": -1}
        regs = {"A": G.regA, "B": G.regB}
        for i, (sp, kind, layer, io) in enumerate(subs):
            snap = phase_begin(G)
            for r in ("A", "B"):
                for j in range(i, len(subs)):
                    nd = NEEDS.get(subs[j][0])
                    if nd is not None and nd[0] == r:
                        if j not in loaded and last_user[r] < i:
                            W = G.w[(subs[j][1], subs[j][2])]
                            if nd[1] is None:
                                if j != i:
                                    break
                                loaded[j] = None
                            else:
                                loaded[j] = WRegion(G, regs[r], snap, W[nd[1]], nd[2], nd[3], blk=nd[4], name=nd[1])
                            last_user[r] = j
                        break
            wr = loaded.get(i)
            if DEBUG_ONLY is None or sp in DEBUG_ONLY:
                PHASE_FN[sp](G, layer, wr, **io)
        k.barrier(k.snapshot(), engines=[k.sp])
        P.stats = {e.name: (e.n_ops, e.n_waits) for e in k.engs}
        P.nsems = len(k.sems)
    return P


class _Globals:
    def uniq(self, name):
        self._uid = getattr(self, "_uid", 0) + 1
        return f"{name}_u{self._uid}"

    def __init__(self, P, k, es):
        self.P = P
        self.k = k
        self.es = es
        nc = P.nc
        self.nc = nc
        stages = P.stages
        self.xin = P.dram_in("xin", [128, 8, S])
        self.yout = nc.dram_tensor("yout", [128, 8, S], F32, kind="ExternalOutput").ap()
        self.xres = P.dram_tmp("xres", [128, 8, S], F32)
        self.xin_bufs = [Buf(f"xin{t}") for t in range(16)]
        self.yout_bufs = [Buf(f"yout{t}") for t in range(16)]
        self.xres_bufs = [Buf(f"xres{t}") for t in range(16)]
        kinds = {kd for kd, _ in stages}
        self.c_ones = P.dram_in("c_ones", [128, 128])
        self.c_ident = P.dram_in("c_ident", [128, 128])
        sb = lambda name, shape, dt: es.enter_context(nc.sbuf_tensor(name, list(shape), dt))
        self.sb = sb
        self.ones_bf = sb("ones_bf", [128, 128], BF16)
        self.ident_bf = sb("ident_bf", [128, 128], BF16)
        self.ident_f = sb("ident_f", [128, 128], F32)
        self.b_const = Buf("consts")
        k.op(k.pool, lambda e: e.dma_start(out=self.ones_bf[:], in_=self.c_ones[:, :]), writes=[self.b_const], dma=True)
        k.op(k.pool, lambda e: e.dma_start(out=self.ident_bf[:], in_=self.c_ident[:, :]), writes=[self.b_const], dma=True)
        k.op(k.sp, lambda e: e.dma_start(out=self.ident_f[:], in_=self.c_ident[:, :]), writes=[self.b_const], dma=True)
        self.regA = sb("regA", [128, 49152], BF16)
        self.regB = sb("regB", [128, 24576], BF16)
        self.regA_free = {}
        self.regB_free = {}
        self.ps = [es.enter_context(nc.psum_tensor(f"psb{i}", [128, 512], F32)) for i in range(8)]
        self.psb = [Buf(f"psb{i}") for i in range(8)]
        self.w = {}
        for kind, l in stages:
            if kind == "ffn":
                self.w[("ffn", l)] = dict(
                    w_up=P.dram_in(f"ffn_w_up_{l}", [D, 2 * FF]),
                    w_dn=P.dram_in(f"ffn_w_dn_{l}", [FF, D]),
                    vec=P.dram_in(f"ffn_vec_{l}", [128, 8 + 24 * 4]),
                )
            elif kind == "moba":
                self.w[("moba", l)] = dict(
                    w_qkv=P.dram_in(f"moba_w_qkv_{l}", [D, 3 * D]),
                    w_out=P.dram_in(f"moba_w_out_{l}", [D, D]),
                    vec=P.dram_in(f"moba_vec_{l}", [128, 8 + 2]),
                )
            elif kind == "hgrn":
                self.w[("hgrn", l)] = dict(
                    w_in=P.dram_in(f"hgrn_w_in_{l}", [D, 4 * D]),
                    w_out=P.dram_in(f"hgrn_w_out_{l}", [D, D]),
                    vec=P.dram_in(f"hgrn_vec_{l}", [128, 8 + 16 + 1]),
                )
        if "ffn" in kinds:
            self.gT = P.dram_tmp("gT", [128, 24, S], BF16)
            self.gT_bufs = [[Buf(f"gT{t}_{g}") for g in range(6)] for t in range(NT)]
        if "moba" in kinds:
            self.c_rot = P.dram_in("c_rot", [128, 128])
            self.c_cos = P.dram_in("c_cos", [128, S])
            self.c_sin = P.dram_in("c_sin", [128, S])
            self.c_past = P.dram_in("c_past", [128, 32 * 16])
            self.c_causal = P.dram_in("c_causal", [128, 2 * 256])
            self.c_selrow = P.dram_in("c_selrow", [128, 16 * 128])
            self.qT = P.dram_tmp("qT", [8, 128, S], BF16)
            self.kT = P.dram_tmp("kT", [8, 128, S], BF16)
            self.vtok = P.dram_tmp("vtok", [128, 32, D], BF16)
            self.oT = P.dram_tmp("oT", [128, 8, S], BF16)
            self.q_bufs = [[Buf(f"q{h}_{t}") for t in range(NT)] for h in range(8)]
            self.k_bufs = [[Buf(f"k{h}_{t}") for t in range(NT)] for h in range(8)]
            self.v_bufs = [Buf(f"v{t}") for t in range(NT)]
            self.o_bufs = [Buf(f"o{h}") for h in range(8)]
        if "hgrn" in kinds:
            self.c_tri = P.dram_in("c_tri", [128, 128])
            self.c_scan = P.dram_in("c_scan", [128, T])


class WRegion:
    def __init__(self, G, reg, free_after, w_ap, kdim, ncols, col0=0, blk=2048, name="w"):
        k = G.k
        self.kc = kdim // 128
        self.ncols = ncols
        self.blk = min(blk, ncols)
        self.view = reg[:, 0:self.kc * ncols].rearrange("p (kc n) -> p kc n", n=ncols)
        self.bufs = {}
        wv = w_ap.rearrange("(kc p) n -> p kc n", p=128)
        for kc in range(self.kc):
            for nb in range(ncols // self.blk):
                b = Buf(f"{name}_{kc}_{nb}")
                self.bufs[(kc, nb)] = b
                n0 = nb * self.blk
                k.op(k.pool,
                     lambda e, kc=kc, n0=n0: e.dma_start(out=self.view[:, kc, n0:n0 + self.blk],
                                                         in_=wv[:, kc, col0 + n0:col0 + n0 + self.blk]),
                     writes=[b], dma=True, after=free_after)

    def buf(self, kc, n0):
        return self.bufs[(kc, n0 // self.blk)]


def rmsnorm_tile(G, L, xt, b_xt, gain, hT, b_hT, ps_i, nfeat_chunks=8, ps_ap=None, ps_buf=None):
    k = G.k
    ps, pb = (G.ps[ps_i], G.psb[ps_i]) if ps_ap is None else (ps_ap, ps_buf)
    for c in range(nfeat_chunks):
        sq, b_sq = L["sq"][c % 2]
        k.op(k.act, lambda e, c=c, sq=sq: e.activation(out=sq[:], in_=xt[:, c, :], func=AF.Square),
             reads=[b_xt], writes=[b_sq])
        k.op(k.pe, lambda e, c=c, sq=sq: e.matmul(ps if ps_ap is not None else ps[:], lhsT=G.ones_bf[:], rhs=sq[:], start=(c == 0),
                                                  stop=(c == nfeat_chunks - 1)),
             reads=[b_sq, G.b_const], writes=(pb if isinstance(pb, list) else [pb]))
    rs, b_rs = L["rstd"]
    k.op(k.act, lambda e: e.activation(out=rs[:], in_=(ps if ps_ap is not None else ps[:]), func=AF.Ln, scale=1.0 / (128 * nfeat_chunks),
                                       bias=L["eps"][:, 0:1]),
         reads=(pb if isinstance(pb, list) else [pb]) + [L["b_eps"]], writes=[b_rs])
    k.op(k.act, lambda e: e.activation(out=rs[:], in_=rs[:], func=AF.Exp, scale=-0.5), reads=[b_rs], writes=[b_rs])
    for c in range(nfeat_chunks):
        k.op(k.dve, lambda e, c=c: e.scalar_tensor_tensor(out=hT[:, c, :], in0=xt[:, c, :], scalar=gain[:, c:c + 1],
                                                          in1=rs[:], op0=ALU.mult, op1=ALU.mult),
             reads=[b_xt, b_rs, L["b_vec"]], writes=[b_hT])


def phase_begin(G):
    snap = G.k.snapshot()
    G.k.barrier(snap)
    return snap


def phase_F1(G, layer, wup, src, dst, sb_src, sb_dst):
    k, nc = G.k, G.nc
    W = G.w[("ffn", layer)]
    with contextlib.ExitStack() as es:
        sb = lambda name, shape, dt: es.enter_context(nc.sbuf_tensor(G.uniq(name), list(shape), dt))
        vec = sb("f_vec", [128, 8 + 96], F32)
        b_vec = Buf("f_vec")
        k.op(k.sp, lambda e: e.dma_start(out=vec[:], in_=W["vec"][:, :]), writes=[b_vec], dma=True)
        eps = sb("f_eps", [128, 1], F32)
        b_eps = Buf("f_eps")
        k.op(k.pool, lambda e: e.memset(eps[:], EPS), writes=[b_eps])
        gain = vec[:, 0:8]
        cw = vec[:, 8:104].rearrange("p (j f) -> p j f", f=24)
        xt = sb("f_xt", [128, 8, T], F32)
        b_xt = Buf("f_xt")
        hTs = [(sb(f"f_hT{i}", [128, 8, T], BF16), Buf(f"f_hT{i}")) for i in range(2)]
        L = dict(sq=[(sb(f"f_sq{i}", [128, T], BF16), Buf(f"f_sq{i}")) for i in range(2)],
                 rstd=(sb("f_rstd", [128, T], F32), Buf("f_rstd")), eps=eps, b_eps=b_eps, b_vec=b_vec)
        abufs = [(sb(f"f_ab{i}", [128, T + 2], F32), Buf(f"f_ab{i}")) for i in range(2)]
        tbufs = [(sb(f"f_t{i}", [128, T], F32), Buf(f"f_t{i}")) for i in range(2)]
        gbufs = [(sb(f"f_g{i}", [128, 4, T], BF16), Buf(f"f_g{i}")) for i in range(2)]
        carry = sb("f_carry", [128, 24, 2], F32)
        b_carry = [Buf(f"f_carry{f}") for f in range(24)]
        k.op(k.pool, lambda e: e.memset(carry[:], 0.0), writes=b_carry)

        k.op(k.sp, lambda e: e.dma_start(out=xt[:], in_=src[:, :, 0:T]), reads=sb_src[0:2], writes=[b_xt], dma=True)
        for ti in range(NT):
            hT, b_hT = hTs[ti % 2]
            rmsnorm_tile(G, L, xt, b_xt, gain, hT, b_hT, ps_i=0)
            if ti + 1 < NT:
                k.op(k.sp, lambda e, ti=ti: e.dma_start(out=xt[:], in_=src[:, :, (ti + 1) * T:(ti + 2) * T]),
                     reads=sb_src[2 * ti + 2:2 * ti + 4], writes=[b_xt], dma=True)
            for fc in range(24):
                pa_i, pu_i = 1 + 2 * (fc % 3), 2 + 2 * (fc % 3)
                pa, pu = G.ps[pa_i], G.ps[pu_i]
                for kc in range(8):
                    k.op(k.pe, lambda e, kc=kc, fc=fc, pa=pa: e.matmul(
                        pa[:], lhsT=wup.view[:, kc, fc * 128:(fc + 1) * 128], rhs=hT[:, kc, :],
                        start=(kc == 0), stop=(kc == 7)),
                        reads=[wup.buf(kc, fc * 128), b_hT], writes=[G.psb[pa_i]])
                for kc in range(8):
                    k.op(k.pe, lambda e, kc=kc, fc=fc, pu=pu: e.matmul(
                        pu[:], lhsT=wup.view[:, kc, FF + fc * 128:FF + (fc + 1) * 128], rhs=hT[:, kc, :],
                        start=(kc == 0), stop=(kc == 7)),
                        reads=[wup.buf(kc, FF + fc * 128), b_hT], writes=[G.psb[pu_i]])
                ab, b_ab = abufs[fc % 2]
                tb, b_tb = tbufs[fc % 2]
                gb, b_gb = gbufs[(fc // 4) % 2]
                k.op(k.pool, lambda e, fc=fc, ab=ab: e.tensor_copy(out=ab[:, 0:2], in_=carry[:, fc, :]),
                     reads=[b_carry[fc]], writes=[b_ab])
                k.op(k.act, lambda e, ab=ab, pa=pa: e.activation(out=ab[:, 2:T + 2], in_=pa[:], func=AF.Copy),
                     reads=[G.psb[pa_i]], writes=[b_ab])
                k.op(k.pool, lambda e, fc=fc, ab=ab: e.tensor_copy(out=carry[:, fc, :], in_=ab[:, T:T + 2]),
                     reads=[b_ab], writes=[b_carry[fc]])
                k.op(k.act, lambda e, fc=fc, tb=tb, pa=pa: e.activation(out=tb[:], in_=pa[:], func=AF.Identity,
                                                                       bias=cw[:, 3, fc:fc + 1], scale=cw[:, 2, fc:fc + 1]),
                     reads=[G.psb[pa_i], b_vec], writes=[b_tb])
                k.op(k.dve, lambda e, fc=fc, tb=tb, ab=ab: e.scalar_tensor_tensor(
                    out=tb[:], in0=ab[:, 1:T + 1], scalar=cw[:, 1, fc:fc + 1], in1=tb[:], op0=ALU.mult, op1=ALU.add),
                    reads=[b_ab, b_tb, b_vec], writes=[b_tb])
                k.op(k.dve, lambda e, fc=fc, tb=tb, ab=ab: e.scalar_tensor_tensor(
                    out=tb[:], in0=ab[:, 0:T], scalar=cw[:, 0, fc:fc + 1], in1=tb[:], op0=ALU.mult, op1=ALU.add),
                    reads=[b_ab, b_tb, b_vec], writes=[b_tb])
                k.op(k.act, lambda e, tb=tb: e.activation(out=tb[:], in_=tb[:], func=AF.Silu), reads=[b_tb], writes=[b_tb])
                k.op(k.dve, lambda e, fc=fc, tb=tb, gb=gb, pu=pu: e.tensor_tensor(
                    out=gb[:, fc % 4, :], in0=tb[:], in1=pu[:], op=ALU.mult),
                    reads=[b_tb, G.psb[pu_i]], writes=[b_gb])
                if fc % 4 == 3:
                    f0 = fc - 3
                    k.op(k.sp, lambda e, f0=f0, gb=gb, ti=ti: e.dma_start(
                        out=G.gT[:, f0:f0 + 4, ti * T:(ti + 1) * T], in_=gb[:]),
                        reads=[b_gb], writes=[G.gT_bufs[ti][fc // 4]], dma=True)


def phase_F2(G, layer, wdn, src, dst, sb_src, sb_dst):
    k, nc = G.k, G.nc
    with contextlib.ExitStack() as es:
        sb = lambda name, shape, dt: es.enter_context(nc.sbuf_tensor(G.uniq(name), list(shape), dt))
        xts = [(sb(f"g_xt{i}", [128, 8, T], F32), Buf(f"g_xt{i}")) for i in range(2)]
        gts = [(sb(f"g_gt{i}", [128, 12, T], BF16), Buf(f"g_gt{i}")) for i in range(2)]
        for ti in range(NT):
            xt, b_xt = xts[ti % 2]
            k.op(k.sp, lambda e, ti=ti, xt=xt: e.dma_start(out=xt[:], in_=src[:, :, ti * T:(ti + 1) * T]),
                 reads=sb_src[2 * ti:2 * ti + 2], writes=[b_xt], dma=True)
            for half in range(2):
                gt, b_gt = gts[half]
                k.op(k.sp, lambda e, ti=ti, gt=gt, half=half: e.dma_start(
                    out=gt[:], in_=G.gT[:, half * 12:(half + 1) * 12, ti * T:(ti + 1) * T]),
                    reads=G.gT_bufs[ti][half * 3:(half + 1) * 3], writes=[b_gt], dma=True)
                for oc in range(8):
                    for f in range(12):
                        fc = half * 12 + f
                        k.op(k.pe, lambda e, oc=oc, fc=fc, f=f, gt=gt: e.matmul(
                            G.ps[oc][:], lhsT=wdn.view[:, fc, oc * 128:(oc + 1) * 128], rhs=gt[:, f, :],
                            start=(fc == 0), stop=(fc == 23)),
                            reads=[wdn.buf(fc, oc * 128), b_gt], writes=[G.psb[oc]])
            for oc in range(8):
                k.op(k.dve, lambda e, oc=oc, xt=xt: e.tensor_tensor(out=xt[:, oc, :], in0=G.ps[oc][:], in1=xt[:, oc, :],
                                                                   op=ALU.add),
                     reads=[G.psb[oc], b_xt], writes=[b_xt])
            k.op(k.sp, lambda e, ti=ti, xt=xt: e.dma_start(out=dst[:, :, ti * T:(ti + 1) * T], in_=xt[:]),
                 reads=[b_xt], writes=sb_dst[2 * ti:2 * ti + 2], dma=True)


def interleave(gens):
    gens = list(gens)
    while gens:
        for g in list(gens):
            try:
                next(g)
            except StopIteration:
                gens.remove(g)


def carve(reg, off, shape):
    n = int(np.prod(shape[1:]))
    v = reg[:, off:off + n]
    if len(shape) == 3:
        v = v.rearrange("p (a b) -> p a b", b=shape[2])
    return v, off + n


def phase_M1(G, layer, wqkv, src, dst, sb_src, sb_dst):
    k, nc = G.k, G.nc
    W = G.w[("moba", layer)]
    with contextlib.ExitStack() as es:
        sb = lambda name, shape, dt: es.enter_context(nc.sbuf_tensor(G.uniq(name), list(shape), dt))
        vec = sb("m_vec", [128, 10], F32)
        b_vec = Buf("m_vec")
        k.op(k.sp, lambda e: e.dma_start(out=vec[:], in_=W["vec"][:, :]), writes=[b_vec], dma=True)
        qkg = sb("m_qkg", [128, 2], F32)
        k.op(k.dve, lambda e: e.tensor_scalar(out=qkg[:, 0:1], in0=vec[:, 8:9], scalar1=128.0 ** -0.5, scalar2=None,
                                              op0=ALU.mult), reads=[b_vec], writes=[b_vec])
        k.op(k.dve, lambda e: e.tensor_copy(out=qkg[:, 1:2], in_=vec[:, 9:10]), reads=[b_vec], writes=[b_vec])
        eps = sb("m_eps", [128, 1], F32)
        b_eps = Buf("m_eps")
        k.op(k.pool, lambda e: e.memset(eps[:], EPS), writes=[b_eps])
        gain = vec[:, 0:8]
        xt = sb("m_xt", [128, 8, T], F32)
        b_xt = Buf("m_xt")
        cs = sb("m_cs", [128, 2, T], F32)
        b_cs = Buf("m_cs")
        off = 8 * 3 * D
        hTs = []
        for i in range(2):
            v, off = carve(G.regA, off, [128, 8, T])
            hTs.append((v, Buf(f"m_hT{i}")))
        vt, off = carve(G.regA, off, [128, 4, D])
        b_vt = Buf("m_vt")
        rot_bf, off = carve(G.regA, off, [128, 128])
        b_rot = Buf("m_rot")
        k.op(k.pool, lambda e: e.dma_start(out=rot_bf, in_=G.c_rot[:, :]), writes=[b_rot], dma=True)
        two = lambda nm: None
        sqs, sqh, qnb, qfs = [], [], [], []
        NS = 4
        for i in range(2):
            v, off = carve(G.regA, off, [128, T]); sqs.append((v, Buf(f"m_sq{i}")))
        for i in range(NS):
            v, off = carve(G.regA, off, [128, T]); sqh.append((v, Buf(f"m_sqh{i}")))
            v, off = carve(G.regA, off, [128, T]); qnb.append((v, Buf(f"m_qnb{i}")))
            v, off = carve(G.regA, off, [128, T]); qfs.append((v, Buf(f"m_qf{i}")))
        assert off <= 49152
        L = dict(sq=sqs, rstd=(sb("m_rstd", [128, T], F32), Buf("m_rstd")), eps=eps, b_eps=b_eps, b_vec=b_vec)
        r2s = [(sb(f"m_r2{i}", [128, T], F32), Buf(f"m_r2{i}")) for i in range(NS)]
        qns = [(sb(f"m_qn{i}", [128, T], F32), Buf(f"m_qn{i}")) for i in range(NS)]
        t1s = [(sb(f"m_t1{i}", [128, T], F32), Buf(f"m_t1{i}")) for i in range(NS)]
        t2s = [(sb(f"m_t2{i}", [128, T], F32), Buf(f"m_t2{i}")) for i in range(NS)]

        k.op(k.sp, lambda e: e.dma_start(out=xt[:], in_=src[:, :, 0:T]), reads=sb_src[0:2], writes=[b_xt], dma=True)
        cnt = 0
        dbg = "vq"
        for ti in range(NT):
            hT, b_hT = hTs[ti % 2]
            rmsnorm_tile(G, L, xt, b_xt, gain, hT, b_hT, ps_i=0)
            if ti + 1 < NT:
                k.op(k.sp, lambda e, ti=ti: e.dma_start(out=xt[:], in_=src[:, :, (ti + 1) * T:(ti + 2) * T]),
                     reads=sb_src[2 * ti + 2:2 * ti + 4], writes=[b_xt], dma=True)
            k.op(k.sp, lambda e, ti=ti: e.dma_start(out=cs[:, 0, :], in_=G.c_cos[:, ti * T:(ti + 1) * T]),
                 writes=[b_cs], dma=True)
            k.op(k.sp, lambda e, ti=ti: e.dma_start(out=cs[:, 1, :], in_=G.c_sin[:, ti * T:(ti + 1) * T]),
                 writes=[b_cs], dma=True)
            for sub in range(4 if "v" in dbg else 0):
                for half in range(2):
                    pi = 1 + (sub * 2 + half) % 2
                    for kc in range(8):
                        k.op(k.pe, lambda e, kc=kc, sub=sub, half=half, pi=pi: e.matmul(
                            G.ps[pi][:], lhsT=hT[:, kc, sub * 128:(sub + 1) * 128],
                            rhs=wqkv.view[:, kc, 2 * D + half * 512:2 * D + (half + 1) * 512],
                            start=(kc == 0), stop=(kc == 7)),
                            reads=[b_hT, wqkv.buf(kc, 2 * D + half * 512)], writes=[G.psb[pi]])
                    k.op(k.act, lambda e, sub=sub, half=half, pi=pi: e.activation(
                        out=vt[:, sub, half * 512:(half + 1) * 512], in_=G.ps[pi][:], func=AF.Copy),
                        reads=[G.psb[pi]], writes=[b_vt])
            if "v" in dbg:
                k.op(k.sp, lambda e, ti=ti: e.dma_start(out=G.vtok[:, ti * 4:(ti + 1) * 4, :], in_=vt),
                     reads=[b_vt], writes=[G.v_bufs[ti]], dma=True)
            def qk_chain(which, h, sl, g):
                col0 = which * D + h * 128
                pi = 1 + (g % 2) * 2 + sl
                pss = 5 if sl == 0 else 0
                pr = 6 + sl
                sl = (g % 2) * 2 + sl
                sq2, b_sq2 = sqh[sl]
                r2, b_r2 = r2s[sl]
                qn, b_qn = qns[sl]
                qb, b_qb = qnb[sl]
                t1, b_t1 = t1s[sl]
                t2, b_t2 = t2s[sl]
                qf, b_qf = qfs[sl]
                for kc in range(8):
                    k.op(k.pe, lambda e: e.matmul(
                        G.ps[pi][:], lhsT=wqkv.view[:, kc, col0:col0 + 128], rhs=hT[:, kc, :],
                        start=(kc == 0), stop=(kc == 7)),
                        reads=[b_hT, wqkv.buf(kc, col0)], writes=[G.psb[pi]])
                yield
                k.op(k.act, lambda e: e.activation(out=sq2, in_=G.ps[pi][:], func=AF.Square),
                     reads=[G.psb[pi]], writes=[b_sq2])
                yield
                k.op(k.pe, lambda e: e.matmul(G.ps[pss][:], lhsT=G.ones_bf[:], rhs=sq2, start=True, stop=True),
                     reads=[b_sq2, G.b_const], writes=[G.psb[pss]])
                yield
                k.op(k.act, lambda e: e.activation(out=r2[:], in_=G.ps[pss][:], func=AF.Ln, bias=eps[:, 0:1],
                                                   scale=1.0 / 128), reads=[G.psb[pss], b_eps], writes=[b_r2])
                yield
                k.op(k.act, lambda e: e.activation(out=r2[:], in_=r2[:], func=AF.Exp, scale=-0.5),
                     reads=[b_r2], writes=[b_r2])
                yield
                k.op(k.dve, lambda e: e.scalar_tensor_tensor(
                    out=qn[:], in0=G.ps[pi][:], scalar=qkg[:, which:which + 1], in1=r2[:], op0=ALU.mult, op1=ALU.mult),
                    reads=[G.psb[pi], b_r2, b_vec], writes=[b_qn])
                yield
                k.op(k.act, lambda e: e.activation(out=qb, in_=qn[:], func=AF.Copy), reads=[b_qn], writes=[b_qb])
                yield
                k.op(k.pe, lambda e: e.matmul(G.ps[pr][:], lhsT=rot_bf, rhs=qb, start=True, stop=True),
                     reads=[b_qb, b_rot], writes=[G.psb[pr]])
                yield
                k.op(k.pool, lambda e: e.tensor_tensor(out=t1[:], in0=qn[:], in1=cs[:, 0, :], op=ALU.mult),
                     reads=[b_qn, b_cs], writes=[b_t1])
                yield
                k.op(k.dve, lambda e: e.tensor_tensor(out=t2[:], in0=G.ps[pr][:], in1=cs[:, 1, :], op=ALU.mult),
                     reads=[G.psb[pr], b_cs], writes=[b_t2])
                yield
                k.op(k.pool, lambda e: e.tensor_tensor(out=qf, in0=t1[:], in1=t2[:], op=ALU.add),
                     reads=[b_t1, b_t2], writes=[b_qf])
                yield
                dT = G.qT if which == 0 else G.kT
                db = G.q_bufs if which == 0 else G.k_bufs
                k.op(k.sp, lambda e: e.dma_start(out=dT[h, :, ti * T:(ti + 1) * T], in_=qf),
                     reads=[b_qf], writes=[db[h][ti]], dma=True)
                yield

            chains = [(w_, h_) for w_ in range(2) for h_ in range(8)]
            for g in range(8):
                interleave([qk_chain(w_, h_, sl, g) for sl, (w_, h_) in enumerate(chains[2 * g:2 * g + 2])])


def phase_M2(G, layer, wr, src, dst, sb_src, sb_dst):
    k, nc = G.k, G.nc
    with contextlib.ExitStack() as es:
        sb = lambda name, shape, dt: es.enter_context(nc.sbuf_tensor(G.uniq(name), list(shape), dt))
        off = 0
        qT, off = carve(G.regA, off, [128, S]); b_q = Buf("a_q")
        kT, off = carve(G.regA, off, [128, S]); b_k = Buf("a_k")
        vt, off = carve(G.regA, off, [128, 32, 128]); b_v = Buf("a_v")
        oTh, off = carve(G.regA, off, [128, S]); b_o = Buf("a_o")
        biasT, off = carve(G.regA, off, [128, S]); b_bT = Buf("a_bT")
        causal, off = carve(G.regA, off, [128, 512]); b_cz = Buf("a_causal")
        selrow, off = carve(G.regA, off, [128, 2048]); b_sr = Buf("a_selrow")
        km_bf, off = carve(G.regA, off, [128, 16]); b_kmb = Buf("a_kmb")
        PTs = []
        for i in range(4):
            v, off = carve(G.regA, off, [128, 256]); PTs.append((v, Buf(f"a_PT{i}")))
        assert off <= 49152
        past = sb("a_past", [128, 512], F32); b_past = Buf("a_past")
        km = sb("a_km", [128, 16], F32); b_km = Buf("a_km")
        gm = sb("a_gm", [128, 512], F32); b_gm = Buf("a_gm")
        top8 = sb("a_top8", [128, 32, 8], F32); b_top8 = Buf("a_top8")
        thr = sb("a_thr", [128, 32], F32); b_thr = Buf("a_thr")
        sel = sb("a_sel", [128, 512], F32); b_sel = Buf("a_sel")
        rdens = [(sb(f"a_rden{i}", [128, 256], F32), Buf(f"a_rden{i}")) for i in range(2)]
        k.op(k.sp, lambda e: e.dma_start(out=past[:], in_=G.c_past[:, :]), writes=[b_past], dma=True)
        k.op(k.pool, lambda e: e.dma_start(out=causal, in_=G.c_causal[:, :]), writes=[b_cz], dma=True)
        k.op(k.pool, lambda e: e.dma_start(out=selrow, in_=G.c_selrow[:, :]), writes=[b_sr], dma=True)
        k.op(k.pool, lambda e: e.memset(biasT, 0.0), writes=[b_bT])
        scnt = 0
        for h in range(8):
            k.op(k.sp, lambda e, h=h: e.dma_start(out=qT, in_=G.qT[h, :, :]), reads=G.q_bufs[h], writes=[b_q], dma=True)
            k.op(k.sp, lambda e, h=h: e.dma_start(out=kT, in_=G.kT[h, :, :]), reads=G.k_bufs[h], writes=[b_k], dma=True)
            k.op(k.sp, lambda e, h=h: e.dma_start(out=vt, in_=G.vtok[:, :, h * 128:(h + 1) * 128]),
                 reads=G.v_bufs, writes=[b_v], dma=True)
            k.op(k.dve, lambda e: e.tensor_reduce(out=km[:], in_=kT.rearrange("p (n j) -> p n j", j=256), axis=AX.X,
                                                  op=ALU.add), reads=[b_k], writes=[b_km])
            k.op(k.dve, lambda e: e.tensor_scalar(out=km_bf, in0=km[:], scalar1=1.0 / 256, scalar2=None, op0=ALU.mult),
                 reads=[b_km], writes=[b_kmb])
            for i in range(32):
                k.op(k.pe, lambda e, i=i: e.matmul(G.ps[0][:, i * 16:(i + 1) * 16], lhsT=qT[:, i * 128:(i + 1) * 128],
                                                   rhs=km_bf, start=True, stop=True),
                     reads=[b_q, b_kmb], writes=[G.psb[0]])
            k.op(k.dve, lambda e: e.tensor_tensor(out=gm[:], in0=G.ps[0][:], in1=past[:], op=ALU.add),
                 reads=[G.psb[0], b_past], writes=[b_gm])
            for i in range(32):
                k.op(k.dve, lambda e, i=i: e.max(out=top8[:, i, :], in_=gm[:, i * 16:(i + 1) * 16]),
                     reads=[b_gm], writes=[b_top8])
            k.op(k.dve, lambda e: e.tensor_scalar(out=thr[:], in0=top8[:, :, 2], scalar1=-1e29, scalar2=None, op0=ALU.max),
                 reads=[b_top8], writes=[b_thr])
            k.op(k.dve, lambda e: e.tensor_tensor(
                out=sel[:].rearrange("p (i n) -> p i n", n=16), in0=gm[:].rearrange("p (i n) -> p i n", n=16),
                in1=thr[:].unsqueeze(2).to_broadcast([128, 32, 16]), op=ALU.is_ge),
                reads=[b_gm, b_thr], writes=[b_sel])
            k.op(k.dve, lambda e: e.tensor_scalar(out=sel[:], in0=sel[:], scalar1=-1.0, scalar2=-NEG, op0=ALU.add,
                                                  op1=ALU.mult), reads=[b_sel], writes=[b_sel])
            for g in range(8):
                pi = g % 2
                for i4 in range(4):
                    i = g * 4 + i4
                    k.op(k.pe, lambda e, i=i, i4=i4, pi=pi: e.transpose(
                        out=G.ps[pi][0:16, i4 * 128:(i4 + 1) * 128], in_=sel[:, i * 16:(i + 1) * 16], identity=G.ident_f[:]),
                        reads=[b_sel, G.b_const], writes=[G.psb[pi]])
                k.op(k.act, lambda e, g=g, pi=pi: e.activation(out=biasT[0:16, g * 512:(g + 1) * 512],
                                                                in_=G.ps[pi][0:16, :], func=AF.Copy),
                     reads=[G.psb[pi]], writes=[b_bT])
            items = [(j, n, kt) for j in range(16) for n in range(j + 1) for kt in range(2)]
            LA = 2

            def s_stage(ii):
                j, n, kt = items[ii]
                qs = slice(j * 256, (j + 1) * 256)
                kc0 = n * 256 + kt * 128
                pi = 2 + ii % 4
                k.op(k.pe, lambda e: e.matmul(
                    G.ps[pi][:, 0:256], lhsT=kT[:, kc0:kc0 + 128], rhs=qT[:, qs], start=True, stop=False),
                    reads=[b_k, b_q], writes=[G.psb[pi]])
                if n < j:
                    k.op(k.pe, lambda e: e.matmul(
                        G.ps[pi][:, 0:256], lhsT=selrow[:, n * 128:(n + 1) * 128], rhs=biasT[:, qs],
                        start=False, stop=True), reads=[b_sr, b_bT], writes=[G.psb[pi]])
                else:
                    k.op(k.pe, lambda e: e.matmul(
                        G.ps[pi][:, 0:256], lhsT=G.ident_bf[:], rhs=causal[:, kt * 256:(kt + 1) * 256],
                        start=False, stop=True), reads=[b_cz, G.b_const], writes=[G.psb[pi]])

            def p_stage(ii):
                j, n, kt = items[ii]
                qs = slice(j * 256, (j + 1) * 256)
                po, pd = (6, 7) if j % 2 == 0 else (0, 1)
                pi = 2 + ii % 4
                PT, b_PT = PTs[ii % 4]
                first = (n == 0 and kt == 0)
                last = (n == j and kt == 1)
                k.op(k.act, lambda e: e.activation(out=PT, in_=G.ps[pi][:, 0:256], func=AF.Exp),
                     reads=[G.psb[pi]], writes=[b_PT])
                k.op(k.pe, lambda e: e.matmul(
                    G.ps[po][:, 0:256], lhsT=vt[:, n * 2 + kt, :], rhs=PT, start=first, stop=last),
                    reads=[b_v, b_PT], writes=[G.psb[po]])
                k.op(k.pe, lambda e: e.matmul(
                    G.ps[pd][:, 0:256], lhsT=G.ones_bf[:], rhs=PT, start=first, stop=last),
                    reads=[G.b_const, b_PT], writes=[G.psb[pd]])
                if last:
                    rd, b_rd = rdens[j % 2]
                    k.op(k.dve, lambda e: e.reciprocal(out=rd[:], in_=G.ps[pd][:, 0:256]),
                         reads=[G.psb[pd]], writes=[b_rd])
                    k.op(k.dve, lambda e: e.tensor_tensor(out=oTh[:, qs], in0=G.ps[po][:, 0:256], in1=rd[:], op=ALU.mult),
                         reads=[G.psb[po], b_rd], writes=[b_o])

            for ii in range(len(items) + LA):
                if ii < len(items):
                    s_stage(ii)
                if ii >= LA:
                    p_stage(ii - LA)
            k.op(k.sp, lambda e, h=h: e.dma_start(out=G.oT[:, h, :], in_=oTh), reads=[b_o], writes=[G.o_bufs[h]], dma=True)


def phase_M3(G, layer, wr, src, dst, sb_src, sb_dst):
    k, nc = G.k, G.nc
    W = G.w[("moba", layer)]
    with contextlib.ExitStack() as es:
        sb = lambda name, shape, dt: es.enter_context(nc.sbuf_tensor(G.uniq(name), list(shape), dt))
        regC = sb("regC", [128, 8 * D], BF16)
        wout = WRegion(G, regC, None, W["w_out"], D, D, blk=1024, name="wout")
        xts = [(sb(f"o_xt{i}", [128, 8, T], F32), Buf(f"o_xt{i}")) for i in range(1)]
        ots = [(sb(f"o_ot{i}", [128, 8, T], BF16), Buf(f"o_ot{i}")) for i in range(2)]
        for ti in range(NT):
            xt, b_xt = xts[0]
            ot, b_ot = ots[ti % 2]
            k.op(k.sp, lambda e, ti=ti, xt=xt: e.dma_start(out=xt[:], in_=src[:, :, ti * T:(ti + 1) * T]),
                 reads=sb_src[2 * ti:2 * ti + 2], writes=[b_xt], dma=True)
            k.op(k.sp, lambda e, ti=ti, ot=ot: e.dma_start(out=ot[:], in_=G.oT[:, :, ti * T:(ti + 1) * T]),
                 reads=G.o_bufs, writes=[b_ot], dma=True)
            for oc in range(8):
                for h in range(8):
                    k.op(k.pe, lambda e, oc=oc, h=h, ot=ot: e.matmul(
                        G.ps[oc][:], lhsT=wout.view[:, h, oc * 128:(oc + 1) * 128], rhs=ot[:, h, :],
                        start=(h == 0), stop=(h == 7)),
                        reads=[wout.buf(h, oc * 128), b_ot], writes=[G.psb[oc]])
                k.op(k.dve, lambda e, oc=oc, xt=xt: e.tensor_tensor(out=xt[:, oc, :], in0=G.ps[oc][:], in1=xt[:, oc, :],
                                                                   op=ALU.add),
                     reads=[G.psb[oc], b_xt], writes=[b_xt])
            k.op(k.sp, lambda e, ti=ti, xt=xt: e.dma_start(out=dst[:, :, ti * T:(ti + 1) * T], in_=xt[:]),
                 reads=[b_xt], writes=sb_dst[2 * ti:2 * ti + 2], dma=True)


TH = 256
NTH = S // TH


def phase_H1(G, layer, win, src, dst, sb_src, sb_dst):
    k, nc = G.k, G.nc
    W = G.w[("hgrn", layer)]
    slot = layer // 2
    with contextlib.ExitStack() as es:
        sb = lambda name, shape, dt: es.enter_context(nc.sbuf_tensor(G.uniq(name), list(shape), dt))
        regC = sb("regC", [128, 8 * D], BF16)
        wout = WRegion(G, regC, None, W["w_out"], D, D, blk=1024, name="hwout")
        vec = sb("h_vec", [128, 25], F32); b_vec = Buf("h_vec")
        k.op(k.sp, lambda e: e.dma_start(out=vec[:], in_=W["vec"][:, :]), writes=[b_vec], dma=True)
        gain = vec[:, 0:8]
        ogain = vec[:, 24:25]
        cst = sb("h_cst", [128, 2], F32); b_cst = Buf("h_cst")
        k.op(k.pool, lambda e: e.memset(cst[:, 0:1], EPS), writes=[b_cst])
        k.op(k.pool, lambda e: e.memset(cst[:, 1:2], 1.0), writes=[b_cst])
        eps = cst
        lbv = sb("h_lb", [128, 3, 8], F32); b_lb = Buf("h_lb")
        if slot == 0:
            k.op(k.pool, lambda e: e.memset(lbv[:, 0, :], 0.0), writes=[b_lb])
        else:
            k.op(k.dve, lambda e: e.tensor_tensor(out=lbv[:, 0, :], in0=vec[:, 8:16], in1=vec[:, 16:24], op=ALU.subtract),
                 reads=[b_vec], writes=[b_lb])
            k.op(k.act, lambda e: e.activation(out=lbv[:, 0, :], in_=lbv[:, 0, :], func=AF.Exp), reads=[b_lb], writes=[b_lb])
            k.op(k.dve, lambda e: e.tensor_scalar(out=lbv[:, 0, :], in0=lbv[:, 0, :], scalar1=1.0, scalar2=None, op0=ALU.add),
                 reads=[b_lb], writes=[b_lb])
            k.op(k.dve, lambda e: e.reciprocal(out=lbv[:, 0, :], in_=lbv[:, 0, :]), reads=[b_lb], writes=[b_lb])
        k.op(k.dve, lambda e: e.tensor_scalar(out=lbv[:, 1, :], in0=lbv[:, 0, :], scalar1=-1.0, scalar2=1.0, op0=ALU.mult,
                                              op1=ALU.add), reads=[b_lb], writes=[b_lb])
        k.op(k.dve, lambda e: e.tensor_scalar(out=lbv[:, 2, :], in0=lbv[:, 1, :], scalar1=-1.0, scalar2=None, op0=ALU.mult),
             reads=[b_lb], writes=[b_lb])
        tri = sb("h_tri", [128, 128], F32); b_tri = Buf("h_tri")
        k.op(k.sp, lambda e: e.dma_start(out=tri[:], in_=G.c_tri[:, :]), writes=[b_tri], dma=True)
        smask = sb("h_smask", [128, TH], F32); b_sm = Buf("h_smask")
        k.op(k.sp, lambda e: e.dma_start(out=smask[:], in_=G.c_scan[:, 0:TH]), writes=[b_sm], dma=True)
        xt = sb("h_xt", [128, 8, TH], F32); b_xt = Buf("h_xt")
        xo = sb("h_xo", [128, 8, TH], F32); b_xo = Buf("h_xo")
        St = sb("h_S", [128, 8, 128], F32); b_S = [Buf(f"h_S{h}") for h in range(8)]
        k.op(k.pool, lambda e: e.memset(St[:], 0.0), writes=b_S)
        rstd = (sb("h_rstd", [128, TH], F32), Buf("h_rstd"))
        sets = []
        for i in range(2):
            d_ = dict(
                b=[(sb(f"h_b{i}_{j}", [128, TH], F32), Buf(f"h_b{i}_{j}")) for j in range(4)],
                qin=(sb(f"h_qin{i}", [128, TH], F32), Buf(f"h_qin{i}")),
                egl=(sb(f"h_egl{i}", [128, 4], F32), Buf(f"h_egl{i}")),
                r2=(sb(f"h_r2{i}", [128, TH], F32), Buf(f"h_r2{i}")),
                tmp=(sb(f"h_tmp{i}", [128, TH], F32), Buf(f"h_tmp{i}")),
            )
            sets.append(d_)
        off = 8 * 4 * D
        hTs = []
        for i in range(2):
            v, off = carve(G.regA, off, [128, 8, TH]); hTs.append((v, Buf(f"h_hT{i}")))
        vtok, off = carve(G.regA, off, [128, 2, D]); b_vtok = Buf("h_vtok")
        sgate, off = carve(G.regA, off, [128, 8, TH]); b_sg = [Buf(f"h_sg{h}") for h in range(8)]
        oTn, off = carve(G.regA, off, [128, 8, TH]); b_oTn = [Buf(f"h_oTn{h}") for h in range(8)]
        sqs = []
        for i in range(2):
            v, off = carve(G.regA, off, [128, TH]); sqs.append((v, Buf(f"h_sq{i}")))
        for i in range(2):
            d_ = sets[i]
            v, off = carve(G.regA, off, [128, TH]); d_["qrel"] = (v, Buf(f"h_qrel{i}"))
            v, off = carve(G.regA, off, [128, TH]); d_["krel"] = (v, Buf(f"h_krel{i}"))
            v, off = carve(G.regA, off, [128, 2, 128]); d_["attT"] = (v, Buf(f"h_attT{i}"))
            v, off = carve(G.regA, off, [128, 4, 128]); d_["kz"] = (v, Buf(f"h_kz{i}"))
            v, off = carve(G.regA, off, [128, TH]); d_["sqo"] = (v, Buf(f"h_sqo{i}"))
            k.op(k.pool, lambda e, v=d_["kz"][0]: e.memset(v, 0.0), writes=[d_["kz"][1]])
        assert off <= 49152, off
        L = dict(sq=sqs, rstd=rstd, eps=eps, b_eps=b_cst, b_vec=b_vec)
        pA = [Buf(f"h_pA{b}") for b in range(8)]
        pB = [Buf(f"h_pB{b}") for b in range(8)]
        lo = lambda b: G.ps[b][:, 0:TH]
        hi = lambda b: G.ps[b][:, TH:2 * TH]
        b_dS = [[Buf(f"h_dS{i}_{c}") for c in range(4)] for i in range(2)]
        dS_ps = lambda i, c: G.ps[7 - i][:, c * 128:(c + 1) * 128]
        bank = lambda b: [pA[b], pB[b]] if b < 6 else b_dS[7 - b]
        SCL = 128.0 ** -0.5

        k.op(k.sp, lambda e: e.dma_start(out=xt[:], in_=src[:, :, 0:TH]), reads=[sb_src[0]], writes=[b_xt], dma=True)
        for ti in range(NTH):
            c0 = ti * TH
            hT, b_hT = hTs[ti % 2]
            rmsnorm_tile(G, L, xt, b_xt, gain, hT, b_hT, ps_i=0, ps_ap=lo(0), ps_buf=bank(0))
            if ti + 1 < NTH:
                k.op(k.sp, lambda e, ti=ti: e.dma_start(out=xt[:], in_=src[:, :, (ti + 1) * TH:(ti + 2) * TH]),
                     reads=[sb_src[ti + 1]], writes=[b_xt], dma=True)
            k.op(k.sp, lambda e, c0=c0: e.dma_start(out=xo[:], in_=src[:, :, c0:c0 + TH]), reads=[sb_src[ti]],
                 writes=[b_xo], dma=True)
            for sub in range(2):
                for half in range(2):
                    pi = 1 + (sub * 2 + half) % 2
                    for kc in range(8):
                        k.op(k.pe, lambda e, kc=kc, sub=sub, half=half, pi=pi: e.matmul(
                            G.ps[pi][:], lhsT=hT[:, kc, sub * 128:(sub + 1) * 128],
                            rhs=win.view[:, kc, 2 * D + half * 512:2 * D + (half + 1) * 512],
                            start=(kc == 0), stop=(kc == 7)),
                            reads=[b_hT, win.buf(kc, 2 * D + half * 512)], writes=bank(pi))
                    k.op(k.act, lambda e, sub=sub, half=half, pi=pi: e.activation(
                        out=vtok[:, sub, half * 512:(half + 1) * 512], in_=G.ps[pi][:], func=AF.Copy),
                        reads=[pA[pi], pB[pi]], writes=[b_vtok])
            for h in range(8):
                pi = 3 + h % 2
                for kc in range(8):
                    k.op(k.pe, lambda e, kc=kc, h=h, pi=pi: e.matmul(
                        lo(pi), lhsT=win.view[:, kc, 3 * D + h * 128:3 * D + (h + 1) * 128], rhs=hT[:, kc, :],
                        start=(kc == 0), stop=(kc == 7)),
                        reads=[b_hT, win.buf(kc, 3 * D + h * 128)], writes=bank(pi))
                k.op(k.act, lambda e, h=h, pi=pi: e.activation(out=sgate[:, h, :], in_=lo(pi), func=AF.Silu),
                     reads=[pA[pi]], writes=[b_sg[h]])
            for hp in range(4):
                hh = (2 * hp, 2 * hp + 1)
                def head_elem(i, h):
                    st = sets[i]
                    pq, pz = 0 + i, 2 + i
                    (b1, B1), (b2, B2), (b3, B3), (b4, B4) = st["b"]
                    for kc in range(8):
                        k.op(k.pe, lambda e, kc=kc, h=h, pq=pq: e.matmul(
                            lo(pq), lhsT=win.view[:, kc, h * 128:(h + 1) * 128], rhs=hT[:, kc, :],
                            start=(kc == 0), stop=(kc == 7)), reads=[b_hT, win.buf(kc, h * 128)], writes=bank(pq))
                    for kc in range(8):
                        k.op(k.pe, lambda e, kc=kc, h=h, pz=pz: e.matmul(
                            lo(pz), lhsT=win.view[:, kc, D + h * 128:D + (h + 1) * 128], rhs=hT[:, kc, :],
                            start=(kc == 0), stop=(kc == 7)), reads=[b_hT, win.buf(kc, D + h * 128)], writes=bank(pz))
                    k.op(k.act, lambda e, b1=b1, pz=pz: e.activation(out=b1[:], in_=lo(pz), func=AF.Exp, scale=-1.0),
                         reads=[pA[pz]], writes=[B1])
                    yield
                    k.op(k.pool, lambda e, b1=b1: e.tensor_scalar(out=b1[:], in0=b1[:], scalar1=1.0, scalar2=None, op0=ALU.add),
                         reads=[B1], writes=[B1])
                    yield
                    k.op(k.dve, lambda e, b1=b1: e.reciprocal(out=b1[:], in_=b1[:]), reads=[B1], writes=[B1])
                    yield
                    k.op(k.act, lambda e, b1=b1, b2=b2, h=h: e.activation(out=b2[:], in_=b1[:], func=AF.Ln, bias=lbv[:, 0, h:h + 1],
                                                                        scale=lbv[:, 1, h:h + 1]),
                         reads=[B1, b_lb], writes=[B2])
                    yield
                    k.op(k.dve, lambda e, b1=b1, h=h: e.tensor_scalar(out=b1[:], in0=b1[:], scalar1=lbv[:, 2, h:h + 1],
                                                                     scalar2=lbv[:, 1, h:h + 1], op0=ALU.mult, op1=ALU.add),
                         reads=[B1, b_lb], writes=[B1])
                    yield
                    k.op(k.dve, lambda e, b2=b2, b3=b3: e.tensor_tensor_scan(out=b3[:], data0=smask[:], data1=b2[:], initial=0.0,
                                                                            op0=ALU.mult, op1=ALU.add),
                         reads=[B2, b_sm], writes=[B3])
                    yield
                    G3 = b3[:].rearrange("p (c t) -> p c t", t=64)
                    k.op(k.dve, lambda e, b2=b2, G3=G3: e.tensor_tensor(
                        out=b2[:].rearrange("p (c t) -> p c t", t=64), in0=G3, in1=G3[:, :, 31:32].to_broadcast([128, 4, 64]),
                        op=ALU.subtract), reads=[B3], writes=[B2])
                    yield
                    k.op(k.act, lambda e, b2=b2, b4=b4: e.activation(out=b4[:], in_=b2[:], func=AF.Exp), reads=[B2], writes=[B4])
                    yield
                    qrel, Bqrel = st["qrel"]
                    k.op(k.dve, lambda e, qrel=qrel, b4=b4, pq=pq: e.scalar_tensor_tensor(
                        out=qrel, in0=lo(pq), scalar=SCL, in1=b4[:], op0=ALU.mult, op1=ALU.mult),
                        reads=[pA[pq], B4], writes=[Bqrel])
                    yield
                    k.op(k.act, lambda e, b2=b2: e.activation(out=b2[:], in_=b2[:], func=AF.Exp, scale=-1.0), reads=[B2], writes=[B2])
                    yield
                    krel, Bkrel = st["krel"]
                    k.op(k.dve, lambda e, krel=krel, b1=b1, b2=b2: e.tensor_tensor(out=krel, in0=b1[:], in1=b2[:], op=ALU.mult),
                         reads=[B1, B2], writes=[Bkrel])
                    yield
                    k.op(k.act, lambda e, b3=b3, b4=b4: e.activation(out=b4[:], in_=b3[:], func=AF.Exp), reads=[B3], writes=[B4])
                    yield
                    qin, Bqin = st["qin"]
                    k.op(k.dve, lambda e, qin=qin, b4=b4, pq=pq: e.scalar_tensor_tensor(
                        out=qin[:], in0=lo(pq), scalar=SCL, in1=b4[:], op0=ALU.mult, op1=ALU.mult),
                        reads=[pA[pq], B4], writes=[Bqin])
                    yield
                    k.op(k.dve, lambda e, b2=b2, G3=G3: e.tensor_tensor(
                        out=b2[:].rearrange("p (c t) -> p c t", t=64), in0=G3, in1=G3[:, :, 63:64].to_broadcast([128, 4, 64]),
                        op=ALU.subtract), reads=[B3], writes=[B2])
                    yield
                    k.op(k.act, lambda e, b2=b2: e.activation(out=b2[:], in_=b2[:], func=AF.Exp, scale=-1.0), reads=[B2], writes=[B2])
                    yield
                    k.op(k.dve, lambda e, b1=b1, b2=b2, b4=b4: e.tensor_tensor(out=b4[:], in0=b1[:], in1=b2[:], op=ALU.mult),
                         reads=[B1, B2], writes=[B4])
                    yield
                    egl, Begl = st["egl"]
                    k.op(k.act, lambda e, egl=egl, G3=G3: e.activation(out=egl[:], in_=G3[:, :, 63], func=AF.Exp),
                         reads=[B3], writes=[Begl])
                    yield
                    kTp = lo(5) if i == 0 else hi(5)
                    BkT = pA[5] if i == 0 else pB[5]
                    for pr in range(2):
                        k.op(k.pe, lambda e, pr=pr, b4=b4, kTp=kTp: e.transpose(
                            out=kTp[:, pr * 128:(pr + 1) * 128], in_=b4[:, pr * 128:(pr + 1) * 128],
                            identity=G.ident_f[:]), reads=[B4, G.b_const], writes=bank(5))
                    kz, Bkz = st["kz"]
                    kzv = kz.rearrange("p (pr cc) d -> p pr cc d", cc=2)
                    k.op(k.act, lambda e, kzv=kzv, kTp=kTp: e.activation(
                        out=kzv[0:64, :, 0, :], in_=kTp[0:64, :].rearrange("p (pr d) -> p pr d", d=128),
                        func=AF.Copy), reads=[BkT], writes=[Bkz])
                    yield
                    k.op(k.act, lambda e, kzv=kzv, kTp=kTp: e.activation(
                        out=kzv[64:128, :, 1, :], in_=kTp[64:128, :].rearrange("p (pr d) -> p pr d", d=128),
                        func=AF.Copy), reads=[BkT], writes=[Bkz])
                    yield
                    aTp = lo(4) if i == 0 else hi(4)
                    BaT = pA[4] if i == 0 else pB[4]
                    for pr in range(2):
                        k.op(k.pe, lambda e, pr=pr, aTp=aTp, krel=krel, qrel=qrel: e.matmul(
                            aTp[:, pr * 128:(pr + 1) * 128], lhsT=krel[:, pr * 128:(pr + 1) * 128],
                            rhs=qrel[:, pr * 128:(pr + 1) * 128], start=True, stop=True),
                            reads=[Bkrel, Bqrel], writes=bank(4))
                    attT, BattT = st["attT"]
                    k.op(k.dve, lambda e, attT=attT, aTp=aTp: e.tensor_tensor(
                        out=attT, in0=aTp.rearrange("p (pr t) -> p pr t", t=128),
                        in1=tri[:].unsqueeze(1).to_broadcast([128, 2, 128]), op=ALU.mult),
                        reads=[BaT, b_tri], writes=[BattT])
                    yield
                    for c in range(4):
                        k.op(k.pe, lambda e, c=c, i=i, kz=kz, h=h: e.matmul(
                            dS_ps(i, c), lhsT=kz[:, c, :], rhs=vtok[:, c // 2, h * 128:(h + 1) * 128], start=True, stop=True),
                            reads=[Bkz, b_vtok], writes=bank(7 - i))

                interleave([head_elem(i_, h_) for i_, h_ in enumerate(hh)])
                for c in range(4):
                    for i, h in enumerate(hh):
                        st = sets[i]
                        qin, Bqin = st["qin"]
                        attT, BattT = st["attT"]
                        egl, Begl = st["egl"]
                        pr, cc = c // 2, c % 2
                        k.op(k.pe, lambda e, c=c, i=i, h=h, qin=qin: e.matmul(
                            hi(i)[:, c * 64:(c + 1) * 64], lhsT=St[:, h, :], rhs=qin[:, c * 64:(c + 1) * 64],
                            start=True, stop=False), reads=[b_S[h], Bqin], writes=bank(i))
                        k.op(k.pe, lambda e, c=c, i=i, h=h, attT=attT, pr=pr, cc=cc: e.matmul(
                            hi(i)[:, c * 64:(c + 1) * 64], lhsT=vtok[:, pr, h * 128:(h + 1) * 128],
                            rhs=attT[:, pr, cc * 64:(cc + 1) * 64], start=False, stop=True),
                            reads=[b_vtok, BattT], writes=bank(i))
                        k.op(k.dve, lambda e, c=c, i=i, h=h, egl=egl: e.scalar_tensor_tensor(
                            out=St[:, h, :], in0=St[:, h, :], scalar=egl[:, c:c + 1], in1=dS_ps(i, c),
                            op0=ALU.mult, op1=ALU.add),
                            reads=[b_S[h], Begl, b_dS[i][c]], writes=[b_S[h]])
                def head_norm(i, h):
                    st = sets[i]
                    sqo, Bsqo = st["sqo"]
                    r2, Br2 = st["r2"]
                    tmp, Btmp = st["tmp"]
                    k.op(k.act, lambda e, sqo=sqo, i=i: e.activation(out=sqo, in_=hi(i), func=AF.Square),
                         reads=[pB[i]], writes=[Bsqo])
                    yield
                    k.op(k.pe, lambda e, sqo=sqo, i=i: e.matmul(hi(2 + i), lhsT=G.ones_bf[:], rhs=sqo, start=True, stop=True),
                         reads=[Bsqo, G.b_const], writes=bank(2 + i))
                    yield
                    k.op(k.act, lambda e, r2=r2, i=i: e.activation(out=r2[:], in_=hi(2 + i), func=AF.Ln,
                                                                   bias=cst[:, 0:1], scale=1.0 / 128),
                         reads=[pB[2 + i], b_cst], writes=[Br2])
                    yield
                    k.op(k.act, lambda e, r2=r2: e.activation(out=r2[:], in_=r2[:], func=AF.Exp, scale=-0.5), reads=[Br2], writes=[Br2])
                    yield
                    k.op(k.dve, lambda e, tmp=tmp, r2=r2, i=i: e.scalar_tensor_tensor(
                        out=tmp[:], in0=hi(i), scalar=ogain, in1=r2[:], op0=ALU.mult, op1=ALU.mult),
                        reads=[pB[i], Br2, b_vec], writes=[Btmp])
                    yield
                    k.op(k.pool, lambda e, tmp=tmp, h=h: e.tensor_tensor(out=oTn[:, h, :], in0=tmp[:], in1=sgate[:, h, :], op=ALU.mult),
                         reads=[Btmp, b_sg[h]], writes=[b_oTn[h]])
                    yield

                interleave([head_norm(i_, h_) for i_, h_ in enumerate(hh)])
            for oc in range(8):
                pi = oc % 2
                for h in range(8):
                    k.op(k.pe, lambda e, oc=oc, h=h, pi=pi: e.matmul(
                        lo(pi), lhsT=wout.view[:, h, oc * 128:(oc + 1) * 128], rhs=oTn[:, h, :],
                        start=(h == 0), stop=(h == 7)), reads=[wout.buf(h, oc * 128), b_oTn[h]], writes=bank(pi))
                k.op(k.dve, lambda e, oc=oc, pi=pi: e.tensor_tensor(out=xo[:, oc, :], in0=lo(pi), in1=xo[:, oc, :],
                                                                   op=ALU.add), reads=[pA[pi], b_xo], writes=[b_xo])
            k.op(k.sp, lambda e, c0=c0: e.dma_start(out=dst[:, :, c0:c0 + TH], in_=xo[:]), reads=[b_xo], writes=[sb_dst[ti]],
                 dma=True)


PHASE_FN = {"F1": phase_F1, "F2": phase_F2, "M1": phase_M1, "M2": phase_M2, "M3": phase_M3, "H1": phase_H1}


def fm(v, n):
    return np.ascontiguousarray(np.asarray(v, np.float32).reshape(n, 128).T)


def host_consts(kinds):
    c = {"c_ones": np.ones((128, 128), np.float32), "c_ident": np.eye(128, dtype=np.float32)}
    if "moba" in kinds:
        rot = np.zeros((128, 128), np.float32)
        for m_ in range(64):
            rot[m_ + 64, m_] = -1.0
            rot[m_, m_ + 64] = 1.0
        c["c_rot"] = rot
        inv = (1.0 / (np.float32(10000.0) ** (np.arange(0, 128, 2, dtype=np.float32) / np.float32(128)))).astype(np.float32)
        ang = (np.arange(S, dtype=np.float32)[:, None] * inv[None, :]).astype(np.float32)
        ang = np.concatenate([ang, ang], axis=-1)
        c["c_cos"] = np.ascontiguousarray(np.cos(ang).astype(np.float32).T)
        c["c_sin"] = np.ascontiguousarray(np.sin(ang).astype(np.float32).T)
        past = np.full((32, 16), -1e30, np.float32)
        for i in range(32):
            past[i, :i // 2] = 0.0
        c["c_past"] = np.ascontiguousarray(np.broadcast_to(past.reshape(1, 512), (128, 512)))
        causal = np.full((128, 2, 256), NEG, np.float32)
        for kt in range(2):
            for p in range(128):
                causal[p, kt, kt * 128 + p:] = 0.0
        c["c_causal"] = causal.reshape(128, 512)
        selrow = np.zeros((128, 16, 128), np.float32)
        for n_ in range(16):
            selrow[n_, n_, :] = 1.0
        c["c_selrow"] = selrow.reshape(128, 2048)
    if "hgrn" in kinds:
        tri = np.zeros((128, 128), np.float32)
        for s_ in range(128):
            for t_ in range(128):
                if s_ // 64 == t_ // 64 and s_ <= t_:
                    tri[s_, t_] = 1.0
        c["c_tri"] = tri
        sm = np.ones((128, T), np.float32)
        sm[:, ::64] = 0.0
        c["c_scan"] = sm
    return c


def stage_inputs(inputs, stages):
    m = {}
    for kind, l in stages:
        if kind == "ffn":
            m[f"ffn_w_up_{l}"] = np.ascontiguousarray(inputs["ffn_w_up"][l])
            m[f"ffn_w_dn_{l}"] = np.ascontiguousarray(inputs["ffn_w_down"][l])
            cw = inputs["ffn_conv_w"][l]
            m[f"ffn_vec_{l}"] = np.ascontiguousarray(np.concatenate(
                [fm(inputs["ffn_norm"][l], 8), fm(cw[0], 24), fm(cw[1], 24), fm(cw[2], 24),
                 fm(inputs["ffn_conv_b"][l], 24)], axis=1))
        elif kind == "hgrn":
            sl = l // 2
            m[f"hgrn_w_in_{l}"] = np.ascontiguousarray(inputs["hgrn_w_in"][sl])
            m[f"hgrn_w_out_{l}"] = np.ascontiguousarray(inputs["hgrn_w_out"][sl])
            m[f"hgrn_vec_{l}"] = np.ascontiguousarray(np.concatenate(
                [fm(inputs["attn_norm"][l], 8), fm(inputs["hgrn_lb"][0], 8), fm(inputs["hgrn_lb"][1], 8),
                 fm(inputs["hgrn_out_norm"][sl], 1)], axis=1))
        elif kind == "moba":
            sl = l // 2
            m[f"moba_w_qkv_{l}"] = np.ascontiguousarray(inputs["moba_w_qkv"][sl])
            m[f"moba_w_out_{l}"] = np.ascontiguousarray(inputs["moba_w_out"][sl])
            m[f"moba_vec_{l}"] = np.ascontiguousarray(np.concatenate(
                [fm(inputs["attn_norm"][l], 8), fm(inputs["moba_q_norm"][sl], 1), fm(inputs["moba_k_norm"][sl], 1)], axis=1))
    return m


def x_to_dev(xb):
    return np.ascontiguousarray(xb.T.reshape(8, 128, S).transpose(1, 0, 2))


def x_from_dev(y):
    return np.ascontiguousarray(y.transpose(1, 0, 2).reshape(D, S).T)


FUSED = True
LAYER_STAGES = [[("hgrn", 0), ("ffn", 0)], [("moba", 1), ("ffn", 1)], [("hgrn", 2), ("ffn", 2)], [("moba", 3), ("ffn", 3)]]


def kernel(**inputs):
    inputs = {k_: np.asarray(v) for k_, v in inputs.items()}
    x = inputs["x"].astype(np.float32)
    nb = x.shape[0]
    groups = [sum(LAYER_STAGES, [])] if FUSED else LAYER_STAGES
    xs = [x_to_dev(x[b]) for b in range(nb)]
    for stages in groups:
        P = build_program(stages)
        shared = dict(host_consts({kd for kd, _ in stages}))
        shared.update(stage_inputs(inputs, stages))
        shared = {k_: v for k_, v in shared.items() if k_ in P.ext_in}
        missing = set(P.ext_in) - set(shared) - {"xin"}
        assert not missing, missing
        in_maps = [dict(shared, xin=xs[b]) for b in range(nb)]
        res = run_bass_kernel_spmd(P.nc, in_maps, core_ids=list(range(nb)))
        xs = [np.asarray(res.results[b]["yout"], dtype=np.float32) for b in range(nb)]
    return np.stack([x_from_dev(xs[b]) for b in range(nb)]).astype(np.float32)
```

```python
import contextlib
import numpy as np
import concourse.bass as bass
import concourse.mybir as mybir
from concourse.bass_utils import run_bass_kernel_spmd

F32 = mybir.dt.float32
BF16 = mybir.dt.bfloat16
AF = mybir.ActivationFunctionType
ALU = mybir.AluOpType
AX = mybir.AxisListType

D = 1024
S = 4096
T = 512
NT = S // T
DEPTH = 4
FF = 3072
EPS = 1e-6
NEG = -30000.0


class Sem:
    __slots__ = ("h", "v")

    def __init__(self, h):
        self.h = h
        self.v = 0


class Buf:
    __slots__ = ("name", "w", "r")

    def __init__(self, name=""):
        self.name = name
        self.w = None
        self.r = {}


class Eng:
    def __init__(self, k, name, h, is_pe=False):
        self.k = k
        self.name = name
        self.h = h
        self.is_pe = is_pe
        self.sem = k.new_sem(name)
        self.dma_sems = []
        self.dma_i = 0
        self.waited = {}
        self.n_ops = 0
        self.n_waits = 0

    def next_dma_sem(self):
        if not self.dma_sems:
            n = 32 if self.name == "pool" else 16
            self.dma_sems = [self.k.new_sem(f"{self.name}_dma{i}") for i in range(n)]
        s = self.dma_sems[self.dma_i % len(self.dma_sems)]
        self.dma_i += 1
        return s


class K:
    def __init__(self, nc, es):
        self.nc = nc
        self.es = es
        self.sems = []
        self.pe = Eng(self, "pe", nc.tensor, is_pe=True)
        self.act = Eng(self, "act", nc.scalar)
        self.dve = Eng(self, "dve", nc.vector)
        self.pool = Eng(self, "pool", nc.gpsimd)
        self.sp = Eng(self, "sp", nc.sync)
        self.engs = [self.pe, self.act, self.dve, self.pool, self.sp]

    def new_sem(self, name):
        h = self.es.enter_context(self.nc.semaphore(f"s_{name}_{len(self.sems)}"))
        s = Sem(h)
        self.sems.append(s)
        return s

    def op(self, eng, fn, reads=(), writes=(), dma=False, after=None):
        deps = {}
        if after:
            for s, v in after.items():
                if deps.get(s, 0) < v:
                    deps[s] = v
        for b in reads:
            if b.w is not None and deps.get(b.w[0], 0) < b.w[1]:
                deps[b.w[0]] = b.w[1]
        for b in writes:
            if b.w is not None and deps.get(b.w[0], 0) < b.w[1]:
                deps[b.w[0]] = b.w[1]
            for s, v in b.r.items():
                if deps.get(s, 0) < v:
                    deps[s] = v
        for s, v in deps.items():
            if eng.is_pe and s is eng.sem:
                continue
            if eng.waited.get(s, 0) >= v:
                continue
            eng.h.wait_ge(s.h, v)
            eng.waited[s] = v
            eng.n_waits += 1
        if dma:
            ds = eng.next_dma_sem()
            if ds.v > 0 and eng.waited.get(ds, 0) < ds.v:
                eng.h.wait_ge(ds.h, ds.v)
                eng.waited[ds] = ds.v
                eng.n_waits += 1
        ins = fn(eng.h)
        eng.n_ops += 1
        if dma:
            s = ds
            s.v += 16
            ins.then_inc(s.h, 16)
        else:
            if eng.sem.v >= 30000:
                eng.sem = self.new_sem(eng.name)
            s = eng.sem
            s.v += 1
            ins.then_inc(s.h, 1)
        ev = (s, s.v)
        for b in reads:
            if b.r.get(s, 0) < s.v:
                b.r[s] = s.v
        for b in writes:
            b.w = ev
            b.r = {}
        return ev

    def snapshot(self):
        return {s: s.v for s in self.sems if s.v > 0}

    def barrier(self, snap, engines=None):
        for e in (engines or self.engs):
            for s, v in snap.items():
                if e.waited.get(s, 0) >= v:
                    continue
                e.h.wait_ge(s.h, v)
                e.waited[s] = v
                e.n_waits += 1


class Prog:
    def __init__(self, stages):
        self.stages = stages
        self.nc = bass.Bass("TRN2", target_bir_lowering=False)
        self.ext_in = {}

    def dram_in(self, name, shape, dt=F32):
        t = self.nc.dram_tensor(name, list(shape), dt, kind="ExternalInput")
        self.ext_in[name] = tuple(shape)
        return t.ap()

    def dram_tmp(self, name, shape, dt):
        kind = "ExternalOutput" if DEBUG_SCRATCH else "Internal"
        return self.nc.dram_tensor(name, list(shape), dt, kind=kind).ap()


DEBUG_SCRATCH = False
DEBUG_ONLY = None
SUBPHASES = {"ffn": ["F1", "F2"], "moba": ["M1", "M2", "M3"], "hgrn": ["H1"]}
NEEDS = {"F1": ("A", "F1", 0, 0, 0),
         "M1": ("A", "w_qkv", D, 3 * D, 1024), "H1": ("A", "w_in", D, 4 * D, 2048),
         "M2": ("A", None, 0, 0, 0)}


def build_program(stages):
    P = Prog(stages)
    nc = P.nc
    with contextlib.ExitStack() as es:
        k = K(nc, es)
        G = _Globals(P, k, es)
        n = len(stages)
        subs = []
        for i, (kind, layer) in enumerate(stages):
            io = dict(src=G.xin if i == 0 else G.xres, dst=G.yout if i == n - 1 else G.xres,
                      sb_src=G.xin_bufs if i == 0 else G.xres_bufs,
                      sb_dst=G.yout_bufs if i == n - 1 else G.xres_bufs)
            for sp in SUBPHASES[kind]:
                subs.append((sp, kind, layer, io))
        loaded = {}
        last_user = {"A": -1, "B": -1}
        regs = {"A": G.regA, "B": G.regB}
        for i, (sp, kind, layer, io) in enumerate(subs):
            snap = phase_begin(G)
            for r in ("A",):
                for j in range(i, len(subs)):
                    nd = NEEDS.get(subs[j][0])
                    if nd is not None and nd[0] == r:
                        if j not in loaded and last_user[r] < i:
                            W = G.w[(subs[j][1], subs[j][2])]
                            if nd[1] is None:
                                if j != i:
                                    break
                                loaded[j] = None
                            elif nd[1] == "F1":
                                lay = subs[j][2]
                                loaded[j] = [wup_third(G, lay, 1, G.regA, 0, snap), wup_third(G, lay, 2, G.regA, 16384, snap)]
                            else:
                                loaded[j] = WRegion(G, regs[r], snap, W[nd[1]], nd[2], nd[3], blk=nd[4], name=nd[1])
                            last_user[r] = j
                        break
            wr = loaded.get(i)
            if sp in ("M1", "H1") or (sp == "F1" and ("ffn", layer) not in G.wup0):
                for j in range(i, len(subs)):
                    if subs[j][0] == "F1":
                        lay = subs[j][2]
                        if ("ffn", lay) not in G.wup0:
                            G.wup0[("ffn", lay)] = wup_third(G, lay, 0, G.regB, 0, snap)
                        break
            if DEBUG_ONLY is None or sp in DEBUG_ONLY:
                PHASE_FN[sp](G, layer, wr, **io)
        k.barrier(k.snapshot(), engines=[k.sp])
        P.stats = {e.name: (e.n_ops, e.n_waits) for e in k.engs}
        P.nsems = len(k.sems)
    return P


class _Globals:
    def uniq(self, name):
        self._uid = getattr(self, "_uid", 0) + 1
        return f"{name}_u{self._uid}"

    def __init__(self, P, k, es):
        self.P = P
        self.k = k
        self.es = es
        nc = P.nc
        self.nc = nc
        stages = P.stages
        self.xin = P.dram_in("xin", [128, 8, S])
        self.yout = nc.dram_tensor("yout", [128, 8, S], F32, kind="ExternalOutput").ap()
        self.xres = P.dram_tmp("xres", [128, 8, S], F32)
        self.xin_bufs = [Buf(f"xin{t}") for t in range(16)]
        self.yout_bufs = [Buf(f"yout{t}") for t in range(16)]
        self.xres_bufs = [Buf(f"xres{t}") for t in range(16)]
        kinds = {kd for kd, _ in stages}
        self.c_ones = P.dram_in("c_ones", [128, 128])
        self.c_ident = P.dram_in("c_ident", [128, 128])
        sb = lambda name, shape, dt: es.enter_context(nc.sbuf_tensor(name, list(shape), dt))
        self.sb = sb
        self.ones_bf = sb("ones_bf", [128, 128], BF16)
        self.ident_bf = sb("ident_bf", [128, 128], BF16)
        self.ident_f = sb("ident_f", [128, 128], F32)
        self.b_const = Buf("consts")
        k.op(k.pool, lambda e: e.dma_start(out=self.ones_bf[:], in_=self.c_ones[:, :]), writes=[self.b_const], dma=True)
        k.op(k.pool, lambda e: e.dma_start(out=self.ident_bf[:], in_=self.c_ident[:, :]), writes=[self.b_const], dma=True)
        k.op(k.sp, lambda e: e.dma_start(out=self.ident_f[:], in_=self.c_ident[:, :]), writes=[self.b_const], dma=True)
        self.regA = sb("regA", [128, 49152], BF16)
        self.regB = sb("regB", [128, 24576], BF16)
        self.wup0 = {}
        self.wdn = {}
        self.ps = [es.enter_context(nc.psum_tensor(f"psb{i}", [128, 512], F32)) for i in range(8)]
        self.psb = [Buf(f"psb{i}") for i in range(8)]
        self.w = {}
        for kind, l in stages:
            if kind == "ffn":
                self.w[("ffn", l)] = dict(
                    w_up=P.dram_in(f"ffn_w_up_{l}", [D, 2 * FF]),
                    w_dn=P.dram_in(f"ffn_w_dn_{l}", [FF, D]),
                    vec=P.dram_in(f"ffn_vec_{l}", [128, 8 + 24 * 4]),
                )
            elif kind == "moba":
                self.w[("moba", l)] = dict(
                    w_qkv=P.dram_in(f"moba_w_qkv_{l}", [D, 3 * D]),
                    w_out=P.dram_in(f"moba_w_out_{l}", [D, D]),
                    vec=P.dram_in(f"moba_vec_{l}", [128, 8 + 2]),
                )
            elif kind == "hgrn":
                self.w[("hgrn", l)] = dict(
                    w_in=P.dram_in(f"hgrn_w_in_{l}", [D, 4 * D]),
                    w_out=P.dram_in(f"hgrn_w_out_{l}", [D, D]),
                    vec=P.dram_in(f"hgrn_vec_{l}", [128, 8 + 16 + 1]),
                )
        if "ffn" in kinds:
            self.gT = P.dram_tmp("gT", [128, 24, S], BF16)
            self.gT_bufs = [[Buf(f"gT{t}_{g}") for g in range(6)] for t in range(NT)]
        if "moba" in kinds:
            self.c_rot = P.dram_in("c_rot", [128, 128])
            self.c_cos = P.dram_in("c_cos", [128, S])
            self.c_sin = P.dram_in("c_sin", [128, S])
            self.c_past = P.dram_in("c_past", [128, 32 * 16])
            self.c_causal = P.dram_in("c_causal", [128, 2 * 256])
            self.c_selrow = P.dram_in("c_selrow", [128, 16 * 128])
            self.qT = P.dram_tmp("qT", [8, 128, S], BF16)
            self.kT = P.dram_tmp("kT", [8, 128, S], BF16)
            self.vtok = P.dram_tmp("vtok", [128, 32, D], BF16)
            self.oT = P.dram_tmp("oT", [128, 8, S], BF16)
            self.q_bufs = [[Buf(f"q{h}_{t}") for t in range(NT)] for h in range(8)]
            self.k_bufs = [[Buf(f"k{h}_{t}") for t in range(NT)] for h in range(8)]
            self.v_bufs = [Buf(f"v{t}") for t in range(NT)]
            self.o_bufs = [Buf(f"o{h}") for h in range(8)]
        if "hgrn" in kinds:
            self.c_tri = P.dram_in("c_tri", [128, 128])
            self.c_scan = P.dram_in("c_scan", [128, T])


class WRegion:
    def __init__(self, G, reg, free_after, w_ap, kdim, ncols, col0=0, blk=2048, name="w", segs=None, reg_off=0):
        k = G.k
        if segs is None:
            segs = [(col0, ncols)]
        ncols = sum(n for _, n in segs)
        self.kc = kdim // 128
        self.ncols = ncols
        self.blk = min(blk, min(n for _, n in segs))
        self.view = reg[:, reg_off:reg_off + self.kc * ncols].rearrange("p (kc n) -> p kc n", n=ncols)
        self.bufs = {}
        wv = w_ap.rearrange("(kc p) n -> p kc n", p=128)
        for kc in range(self.kc):
            d0 = 0
            for (c0, n) in segs:
                for j in range(n // self.blk):
                    b = Buf(f"{name}_{kc}_{d0 // self.blk}")
                    self.bufs[(kc, d0 // self.blk)] = b
                    k.op(k.pool,
                         lambda e, kc=kc, d0=d0, s0=c0 + j * self.blk: e.dma_start(
                             out=self.view[:, kc, d0:d0 + self.blk], in_=wv[:, kc, s0:s0 + self.blk]),
                         writes=[b], dma=True, after=free_after)
                    d0 += self.blk

    def buf(self, kc, n0):
        return self.bufs[(kc, n0 // self.blk)]


def wup_third(G, layer, g, reg, reg_off, free_after):
    W = G.w[("ffn", layer)]
    return WRegion(G, reg, free_after, W["w_up"], D, 2048, blk=1024, name=f"wup{g}",
                   segs=[(g * 1024, 1024), (FF + g * 1024, 1024)], reg_off=reg_off)


def rmsnorm_tile(G, L, xt, b_xt, gain, hT, b_hT, ps_i, nfeat_chunks=8, ps_ap=None, ps_buf=None):
    k = G.k
    ps, pb = (G.ps[ps_i], G.psb[ps_i]) if ps_ap is None else (ps_ap, ps_buf)
    for c in range(nfeat_chunks):
        sq, b_sq = L["sq"][c % 2]
        k.op(k.act, lambda e, c=c, sq=sq: e.activation(out=sq[:], in_=xt[:, c, :], func=AF.Square),
             reads=[b_xt], writes=[b_sq])
        k.op(k.pe, lambda e, c=c, sq=sq: e.matmul(ps if ps_ap is not None else ps[:], lhsT=G.ones_bf[:], rhs=sq[:], start=(c == 0),
                                                  stop=(c == nfeat_chunks - 1)),
             reads=[b_sq, G.b_const], writes=(pb if isinstance(pb, list) else [pb]))
    rs, b_rs = L["rstd"]
    k.op(k.act, lambda e: e.activation(out=rs[:], in_=(ps if ps_ap is not None else ps[:]), func=AF.Ln, scale=1.0 / (128 * nfeat_chunks),
                                       bias=L["eps"][:, 0:1]),
         reads=(pb if isinstance(pb, list) else [pb]) + [L["b_eps"]], writes=[b_rs])
    k.op(k.act, lambda e: e.activation(out=rs[:], in_=rs[:], func=AF.Exp, scale=-0.5), reads=[b_rs], writes=[b_rs])
    for c in range(nfeat_chunks):
        k.op(k.dve, lambda e, c=c: e.scalar_tensor_tensor(out=hT[:, c, :], in0=xt[:, c, :], scalar=gain[:, c:c + 1],
                                                          in1=rs[:], op0=ALU.mult, op1=ALU.mult),
             reads=[b_xt, b_rs, L["b_vec"]], writes=[b_hT])


def phase_begin(G):
    snap = G.k.snapshot()
    G.k.barrier(snap)
    return snap


def phase_F1(G, layer, wups, src, dst, sb_src, sb_dst):
    k, nc = G.k, G.nc
    W = G.w[("ffn", layer)]
    with contextlib.ExitStack() as es:
        sb = lambda name, shape, dt: es.enter_context(nc.sbuf_tensor(G.uniq(name), list(shape), dt))
        vec = sb("f_vec", [128, 8 + 96], F32)
        b_vec = Buf("f_vec")
        k.op(k.sp, lambda e: e.dma_start(out=vec[:], in_=W["vec"][:, :]), writes=[b_vec], dma=True)
        eps = sb("f_eps", [128, 1], F32)
        b_eps = Buf("f_eps")
        k.op(k.pool, lambda e: e.memset(eps[:], EPS), writes=[b_eps])
        gain = vec[:, 0:8]
        cw = vec[:, 8:104].rearrange("p (j f) -> p j f", f=24)
        xt = sb("f_xt", [128, 8, T], F32)
        b_xt = Buf("f_xt")
        hTs = [(sb(f"f_hT{i}", [128, 8, T], BF16), Buf(f"f_hT{i}")) for i in range(2)]
        L = dict(sq=[(sb(f"f_sq{i}", [128, T], BF16), Buf(f"f_sq{i}")) for i in range(2)],
                 rstd=(sb("f_rstd", [128, T], F32), Buf("f_rstd")), eps=eps, b_eps=b_eps, b_vec=b_vec)
        abufs = [(sb(f"f_ab{i}", [128, T + 2], F32), Buf(f"f_ab{i}")) for i in range(2)]
        tbufs = [(sb(f"f_t{i}", [128, T], F32), Buf(f"f_t{i}")) for i in range(2)]
        gbufs = [(sb(f"f_g{i}", [128, 4, T], BF16), Buf(f"f_g{i}")) for i in range(2)]
        carry = sb("f_carry", [128, 24, 2], F32)
        b_carry = [Buf(f"f_carry{f}") for f in range(24)]
        k.op(k.pool, lambda e: e.memset(carry[:], 0.0), writes=b_carry)

        thirds = [G.wup0[("ffn", layer)], wups[0], wups[1]]
        k.op(k.sp, lambda e: e.dma_start(out=xt[:], in_=src[:, :, 0:T]), reads=sb_src[0:2], writes=[b_xt], dma=True)
        def emit_norm(idx):
            hT_, b_hT_ = hTs[idx % 2]
            rmsnorm_tile(G, L, xt, b_xt, gain, hT_, b_hT_, ps_i=0)
            if idx + 1 < 3 * NT:
                tn = (idx + 1) % NT
                k.op(k.sp, lambda e, tn=tn: e.dma_start(out=xt[:], in_=src[:, :, tn * T:(tn + 1) * T]),
                     reads=sb_src[2 * tn:2 * tn + 2], writes=[b_xt], dma=True)

        it = 0
        emit_norm(0)
        for fcg in range(3):
          wup = thirds[fcg]
          for ti in range(NT):
            hT, b_hT = hTs[it % 2]
            it += 1
            def fc_chain(f):
                fc = fcg * 8 + f
                pa_i, pu_i = 1 + 2 * (fc % 3), 2 + 2 * (fc % 3)
                pa, pu = G.ps[pa_i], G.ps[pu_i]
                for kc in range(8):
                    k.op(k.pe, lambda e, kc=kc, f=f, pa=pa: e.matmul(
                        pa[:], lhsT=wup.view[:, kc, f * 128:(f + 1) * 128], rhs=hT[:, kc, :],
                        start=(kc == 0), stop=(kc == 7)),
                        reads=[wup.buf(kc, f * 128), b_hT], writes=[G.psb[pa_i]])
                for kc in range(8):
                    k.op(k.pe, lambda e, kc=kc, f=f, pu=pu: e.matmul(
                        pu[:], lhsT=wup.view[:, kc, 1024 + f * 128:1024 + (f + 1) * 128], rhs=hT[:, kc, :],
                        start=(kc == 0), stop=(kc == 7)),
                        reads=[wup.buf(kc, 1024 + f * 128), b_hT], writes=[G.psb[pu_i]])
                ab, b_ab = abufs[fc % 2]
                tb, b_tb = tbufs[fc % 2]
                gb, b_gb = gbufs[(fc // 4) % 2]
                k.op(k.dve, lambda e, fc=fc, ab=ab: e.tensor_copy(out=ab[:, 0:2], in_=carry[:, fc, :]),
                     reads=[b_carry[fc]], writes=[b_ab])
                yield
                k.op(k.act, lambda e, ab=ab, pa=pa: e.activation(out=ab[:, 2:T + 2], in_=pa[:], func=AF.Copy),
                     reads=[G.psb[pa_i]], writes=[b_ab])
                yield
                k.op(k.dve, lambda e, fc=fc, ab=ab: e.tensor_copy(out=carry[:, fc, :], in_=ab[:, T:T + 2]),
                     reads=[b_ab], writes=[b_carry[fc]])
                yield
                k.op(k.act, lambda e, fc=fc, tb=tb, pa=pa: e.activation(out=tb[:], in_=pa[:], func=AF.Identity,
                                                                       bias=cw[:, 3, fc:fc + 1], scale=cw[:, 2, fc:fc + 1]),
                     reads=[G.psb[pa_i], b_vec], writes=[b_tb])
                yield
                k.op(k.dve, lambda e, fc=fc, tb=tb, ab=ab: e.scalar_tensor_tensor(
                    out=tb[:], in0=ab[:, 1:T + 1], scalar=cw[:, 1, fc:fc + 1], in1=tb[:], op0=ALU.mult, op1=ALU.add),
                    reads=[b_ab, b_tb, b_vec], writes=[b_tb])
                yield
                k.op(k.dve, lambda e, fc=fc, tb=tb, ab=ab: e.scalar_tensor_tensor(
                    out=tb[:], in0=ab[:, 0:T], scalar=cw[:, 0, fc:fc + 1], in1=tb[:], op0=ALU.mult, op1=ALU.add),
                    reads=[b_ab, b_tb, b_vec], writes=[b_tb])
                yield
                k.op(k.act, lambda e, tb=tb: e.activation(out=tb[:], in_=tb[:], func=AF.Silu), reads=[b_tb], writes=[b_tb])
                yield
                k.op(k.dve, lambda e, fc=fc, tb=tb, gb=gb, pu=pu: e.tensor_tensor(
                    out=gb[:, fc % 4, :], in0=tb[:], in1=pu[:], op=ALU.mult),
                    reads=[b_tb, G.psb[pu_i]], writes=[b_gb])
                yield
                if fc % 4 == 3:
                    f0 = fc - 3
                    k.op(k.sp, lambda e, f0=f0, gb=gb, ti=ti: e.dma_start(
                        out=G.gT[:, f0:f0 + 4, ti * T:(ti + 1) * T], in_=gb[:]),
                        reads=[b_gb], writes=[G.gT_bufs[ti][fc // 4]], dma=True)

            for f0 in range(0, 8, 2):
                interleave([fc_chain(f0), fc_chain(f0 + 1)])
                if f0 == 2 and it < 3 * NT:
                    emit_norm(it)
          if fcg == 0:
            G.wdn[layer] = WRegion(G, G.regB, k.snapshot(), W["w_dn"], FF, D, blk=1024, name="wdn")


def phase_F2(G, layer, wr, src, dst, sb_src, sb_dst):
    k, nc = G.k, G.nc
    wdn = G.wdn[layer]
    with contextlib.ExitStack() as es:
        sb = lambda name, shape, dt: es.enter_context(nc.sbuf_tensor(G.uniq(name), list(shape), dt))
        xts = [(sb(f"g_xt{i}", [128, 8, T], F32), Buf(f"g_xt{i}")) for i in range(2)]
        gts = [(sb(f"g_gt{i}", [128, 12, T], BF16), Buf(f"g_gt{i}")) for i in range(2)]
        for ti in range(NT):
            xt, b_xt = xts[ti % 2]
            k.op(k.sp, lambda e, ti=ti, xt=xt: e.dma_start(out=xt[:], in_=src[:, :, ti * T:(ti + 1) * T]),
                 reads=sb_src[2 * ti:2 * ti + 2], writes=[b_xt], dma=True)
            for half in range(2):
                gt, b_gt = gts[half]
                k.op(k.act, lambda e, ti=ti, gt=gt, half=half: e.dma_start(
                    out=gt[:], in_=G.gT[:, half * 12:(half + 1) * 12, ti * T:(ti + 1) * T]),
                    reads=G.gT_bufs[ti][half * 3:(half + 1) * 3], writes=[b_gt], dma=True)
                for oc in range(8):
                    for f in range(12):
                        fc = half * 12 + f
                        k.op(k.pe, lambda e, oc=oc, fc=fc, f=f, gt=gt: e.matmul(
                            G.ps[oc][:], lhsT=wdn.view[:, fc, oc * 128:(oc + 1) * 128], rhs=gt[:, f, :],
                            start=(fc == 0), stop=(fc == 23)),
                            reads=[wdn.buf(fc, oc * 128), b_gt], writes=[G.psb[oc]])
            for oc in range(8):
                k.op(k.dve, lambda e, oc=oc, xt=xt: e.tensor_tensor(out=xt[:, oc, :], in0=G.ps[oc][:], in1=xt[:, oc, :],
                                                                   op=ALU.add),
                     reads=[G.psb[oc], b_xt], writes=[b_xt])
            k.op(k.sp, lambda e, ti=ti, xt=xt: e.dma_start(out=dst[:, :, ti * T:(ti + 1) * T], in_=xt[:]),
                 reads=[b_xt], writes=sb_dst[2 * ti:2 * ti + 2], dma=True)


def interleave(gens):
    gens = list(gens)
    while gens:
        for g in list(gens):
            try:
                next(g)
            except StopIteration:
                gens.remove(g)


def carve(reg, off, shape):
    n = int(np.prod(shape[1:]))
    v = reg[:, off:off + n]
    if len(shape) == 3:
        v = v.rearrange("p (a b) -> p a b", b=shape[2])
    return v, off + n


def phase_M1(G, layer, wqkv, src, dst, sb_src, sb_dst):
    k, nc = G.k, G.nc
    W = G.w[("moba", layer)]
    with contextlib.ExitStack() as es:
        sb = lambda name, shape, dt: es.enter_context(nc.sbuf_tensor(G.uniq(name), list(shape), dt))
        vec = sb("m_vec", [128, 10], F32)
        b_vec = Buf("m_vec")
        k.op(k.sp, lambda e: e.dma_start(out=vec[:], in_=W["vec"][:, :]), writes=[b_vec], dma=True)
        qkg = sb("m_qkg", [128, 2], F32)
        k.op(k.dve, lambda e: e.tensor_scalar(out=qkg[:, 0:1], in0=vec[:, 8:9], scalar1=128.0 ** -0.5, scalar2=None,
                                              op0=ALU.mult), reads=[b_vec], writes=[b_vec])
        k.op(k.dve, lambda e: e.tensor_copy(out=qkg[:, 1:2], in_=vec[:, 9:10]), reads=[b_vec], writes=[b_vec])
        eps = sb("m_eps", [128, 1], F32)
        b_eps = Buf("m_eps")
        k.op(k.pool, lambda e: e.memset(eps[:], EPS), writes=[b_eps])
        gain = vec[:, 0:8]
        xt = sb("m_xt", [128, 8, T], F32)
        b_xt = Buf("m_xt")
        cs = sb("m_cs", [128, 2, T], F32)
        b_cs = Buf("m_cs")
        off = 8 * 3 * D
        hTs = []
        for i in range(2):
            v, off = carve(G.regA, off, [128, 8, T])
            hTs.append((v, Buf(f"m_hT{i}")))
        vt, off = carve(G.regA, off, [128, 4, D])
        b_vt = Buf("m_vt")
        rot_bf, off = carve(G.regA, off, [128, 128])
        b_rot = Buf("m_rot")
        k.op(k.pool, lambda e: e.dma_start(out=rot_bf, in_=G.c_rot[:, :]), writes=[b_rot], dma=True)
        two = lambda nm: None
        sqs, sqh, qnb, qfs = [], [], [], []
        NS = 4
        for i in range(2):
            v, off = carve(G.regA, off, [128, T]); sqs.append((v, Buf(f"m_sq{i}")))
        for i in range(NS):
            v, off = carve(G.regA, off, [128, T]); sqh.append((v, Buf(f"m_sqh{i}")))
            v, off = carve(G.regA, off, [128, T]); qnb.append((v, Buf(f"m_qnb{i}")))
            v, off = carve(G.regA, off, [128, T]); qfs.append((v, Buf(f"m_qf{i}")))
        assert off <= 49152
        L = dict(sq=sqs, rstd=(sb("m_rstd", [128, T], F32), Buf("m_rstd")), eps=eps, b_eps=b_eps, b_vec=b_vec)
        r2s = [(sb(f"m_r2{i}", [128, T], F32), Buf(f"m_r2{i}")) for i in range(NS)]
        qns = [(sb(f"m_qn{i}", [128, T], F32), Buf(f"m_qn{i}")) for i in range(NS)]
        t1s = [(sb(f"m_t1{i}", [128, T], F32), Buf(f"m_t1{i}")) for i in range(NS)]
        t2s = [(sb(f"m_t2{i}", [128, T], F32), Buf(f"m_t2{i}")) for i in range(NS)]

        k.op(k.sp, lambda e: e.dma_start(out=xt[:], in_=src[:, :, 0:T]), reads=sb_src[0:2], writes=[b_xt], dma=True)
        cnt = 0
        dbg = "vq"
        for ti in range(NT):
            hT, b_hT = hTs[ti % 2]
            rmsnorm_tile(G, L, xt, b_xt, gain, hT, b_hT, ps_i=0)
            if ti + 1 < NT:
                k.op(k.sp, lambda e, ti=ti: e.dma_start(out=xt[:], in_=src[:, :, (ti + 1) * T:(ti + 2) * T]),
                     reads=sb_src[2 * ti + 2:2 * ti + 4], writes=[b_xt], dma=True)
            k.op(k.sp, lambda e, ti=ti: e.dma_start(out=cs[:, 0, :], in_=G.c_cos[:, ti * T:(ti + 1) * T]),
                 writes=[b_cs], dma=True)
            k.op(k.sp, lambda e, ti=ti: e.dma_start(out=cs[:, 1, :], in_=G.c_sin[:, ti * T:(ti + 1) * T]),
                 writes=[b_cs], dma=True)
            for sub in range(4 if "v" in dbg else 0):
                for half in range(2):
                    pi = 1 + (sub * 2 + half) % 2
                    for kc in range(8):
                        k.op(k.pe, lambda e, kc=kc, sub=sub, half=half, pi=pi: e.matmul(
                            G.ps[pi][:], lhsT=hT[:, kc, sub * 128:(sub + 1) * 128],
                            rhs=wqkv.view[:, kc, 2 * D + half * 512:2 * D + (half + 1) * 512],
                            start=(kc == 0), stop=(kc == 7)),
                            reads=[b_hT, wqkv.buf(kc, 2 * D + half * 512)], writes=[G.psb[pi]])
                    k.op(k.act, lambda e, sub=sub, half=half, pi=pi: e.activation(
                        out=vt[:, sub, half * 512:(half + 1) * 512], in_=G.ps[pi][:], func=AF.Copy),
                        reads=[G.psb[pi]], writes=[b_vt])
            if "v" in dbg:
                k.op(k.sp, lambda e, ti=ti: e.dma_start(out=G.vtok[:, ti * 4:(ti + 1) * 4, :], in_=vt),
                     reads=[b_vt], writes=[G.v_bufs[ti]], dma=True)
            def qk_chain(which, h, sl, g):
                col0 = which * D + h * 128
                pi = 1 + (g % 2) * 2 + sl
                pss = 5 if sl == 0 else 0
                pr = 6 + sl
                sl = (g % 2) * 2 + sl
                sq2, b_sq2 = sqh[sl]
                r2, b_r2 = r2s[sl]
                qn, b_qn = qns[sl]
                qb, b_qb = qnb[sl]
                t1, b_t1 = t1s[sl]
                t2, b_t2 = t2s[sl]
                qf, b_qf = qfs[sl]
                for kc in range(8):
                    k.op(k.pe, lambda e: e.matmul(
                        G.ps[pi][:], lhsT=wqkv.view[:, kc, col0:col0 + 128], rhs=hT[:, kc, :],
                        start=(kc == 0), stop=(kc == 7)),
                        reads=[b_hT, wqkv.buf(kc, col0)], writes=[G.psb[pi]])
                yield
                k.op(k.act, lambda e: e.activation(out=sq2, in_=G.ps[pi][:], func=AF.Square),
                     reads=[G.psb[pi]], writes=[b_sq2])
                yield
                k.op(k.pe, lambda e: e.matmul(G.ps[pss][:], lhsT=G.ones_bf[:], rhs=sq2, start=True, stop=True),
                     reads=[b_sq2, G.b_const], writes=[G.psb[pss]])
                yield
                k.op(k.act, lambda e: e.activation(out=r2[:], in_=G.ps[pss][:], func=AF.Ln, bias=eps[:, 0:1],
                                                   scale=1.0 / 128), reads=[G.psb[pss], b_eps], writes=[b_r2])
                yield
                k.op(k.act, lambda e: e.activation(out=r2[:], in_=r2[:], func=AF.Exp, scale=-0.5),
                     reads=[b_r2], writes=[b_r2])
                yield
                k.op(k.dve, lambda e: e.scalar_tensor_tensor(
                    out=qn[:], in0=G.ps[pi][:], scalar=qkg[:, which:which + 1], in1=r2[:], op0=ALU.mult, op1=ALU.mult),
                    reads=[G.psb[pi], b_r2, b_vec], writes=[b_qn])
                yield
                k.op(k.act, lambda e: e.activation(out=qb, in_=qn[:], func=AF.Copy), reads=[b_qn], writes=[b_qb])
                yield
                k.op(k.pe, lambda e: e.matmul(G.ps[pr][:], lhsT=rot_bf, rhs=qb, start=True, stop=True),
                     reads=[b_qb, b_rot], writes=[G.psb[pr]])
                yield
                k.op(k.pool, lambda e: e.tensor_tensor(out=t1[:], in0=qn[:], in1=cs[:, 0, :], op=ALU.mult),
                     reads=[b_qn, b_cs], writes=[b_t1])
                yield
                k.op(k.dve, lambda e: e.tensor_tensor(out=t2[:], in0=G.ps[pr][:], in1=cs[:, 1, :], op=ALU.mult),
                     reads=[G.psb[pr], b_cs], writes=[b_t2])
                yield
                k.op(k.pool, lambda e: e.tensor_tensor(out=qf, in0=t1[:], in1=t2[:], op=ALU.add),
                     reads=[b_t1, b_t2], writes=[b_qf])
                yield
                dT = G.qT if which == 0 else G.kT
                db = G.q_bufs if which == 0 else G.k_bufs
                k.op(k.sp, lambda e: e.dma_start(out=dT[h, :, ti * T:(ti + 1) * T], in_=qf),
                     reads=[b_qf], writes=[db[h][ti]], dma=True)
                yield

            chains = [(w_, h_) for w_ in range(2) for h_ in range(8)]
            for g in range(8):
                interleave([qk_chain(w_, h_, sl, g) for sl, (w_, h_) in enumerate(chains[2 * g:2 * g + 2])])


def phase_M2(G, layer, wr, src, dst, sb_src, sb_dst):
    k, nc = G.k, G.nc
    with contextlib.ExitStack() as es:
        sb = lambda name, shape, dt: es.enter_context(nc.sbuf_tensor(G.uniq(name), list(shape), dt))
        off = 0
        qT, off = carve(G.regA, off, [128, S]); b_q = Buf("a_q")
        kT, off = carve(G.regA, off, [128, S]); b_k = Buf("a_k")
        vt, off = carve(G.regA, off, [128, 32, 128]); b_v = Buf("a_v")
        oTh, off = carve(G.regA, off, [128, S]); b_o = Buf("a_o")
        biasT, off = carve(G.regA, off, [128, S]); b_bT = Buf("a_bT")
        causal, off = carve(G.regA, off, [128, 512]); b_cz = Buf("a_causal")
        selrow, off = carve(G.regA, off, [128, 2048]); b_sr = Buf("a_selrow")
        km_bf, off = carve(G.regA, off, [128, 16]); b_kmb = Buf("a_kmb")
        PTs = []
        for i in range(4):
            v, off = carve(G.regA, off, [128, 256]); PTs.append((v, Buf(f"a_PT{i}")))
        assert off <= 49152
        past = sb("a_past", [128, 512], F32); b_past = Buf("a_past")
        km = sb("a_km", [128, 16], F32); b_km = Buf("a_km")
        gm = sb("a_gm", [128, 512], F32); b_gm = Buf("a_gm")
        top8 = sb("a_top8", [128, 32, 8], F32); b_top8 = Buf("a_top8")
        thr = sb("a_thr", [128, 32], F32); b_thr = Buf("a_thr")
        sel = sb("a_sel", [128, 512], F32); b_sel = Buf("a_sel")
        rdens = [(sb(f"a_rden{i}", [128, 256], F32), Buf(f"a_rden{i}")) for i in range(2)]
        k.op(k.sp, lambda e: e.dma_start(out=past[:], in_=G.c_past[:, :]), writes=[b_past], dma=True)
        k.op(k.pool, lambda e: e.dma_start(out=causal, in_=G.c_causal[:, :]), writes=[b_cz], dma=True)
        k.op(k.pool, lambda e: e.dma_start(out=selrow, in_=G.c_selrow[:, :]), writes=[b_sr], dma=True)
        k.op(k.pool, lambda e: e.memset(biasT, 0.0), writes=[b_bT])
        scnt = 0
        for h in range(8):
            k.op(k.sp, lambda e, h=h: e.dma_start(out=qT, in_=G.qT[h, :, :]), reads=G.q_bufs[h], writes=[b_q], dma=True)
            k.op(k.sp, lambda e, h=h: e.dma_start(out=kT, in_=G.kT[h, :, :]), reads=G.k_bufs[h], writes=[b_k], dma=True)
            k.op(k.sp, lambda e, h=h: e.dma_start(out=vt, in_=G.vtok[:, :, h * 128:(h + 1) * 128]),
                 reads=G.v_bufs, writes=[b_v], dma=True)
            k.op(k.dve, lambda e: e.tensor_reduce(out=km[:], in_=kT.rearrange("p (n j) -> p n j", j=256), axis=AX.X,
                                                  op=ALU.add), reads=[b_k], writes=[b_km])
            k.op(k.dve, lambda e: e.tensor_scalar(out=km_bf, in0=km[:], scalar1=1.0 / 256, scalar2=None, op0=ALU.mult),
                 reads=[b_km], writes=[b_kmb])
            for i in range(32):
                k.op(k.pe, lambda e, i=i: e.matmul(G.ps[0][:, i * 16:(i + 1) * 16], lhsT=qT[:, i * 128:(i + 1) * 128],
                                                   rhs=km_bf, start=True, stop=True),
                     reads=[b_q, b_kmb], writes=[G.psb[0]])
            k.op(k.dve, lambda e: e.tensor_tensor(out=gm[:], in0=G.ps[0][:], in1=past[:], op=ALU.add),
                 reads=[G.psb[0], b_past], writes=[b_gm])
            for i in range(32):
                k.op(k.dve, lambda e, i=i: e.max(out=top8[:, i, :], in_=gm[:, i * 16:(i + 1) * 16]),
                     reads=[b_gm], writes=[b_top8])
            k.op(k.dve, lambda e: e.tensor_scalar(out=thr[:], in0=top8[:, :, 2], scalar1=-1e29, scalar2=None, op0=ALU.max),
                 reads=[b_top8], writes=[b_thr])
            k.op(k.dve, lambda e: e.tensor_tensor(
                out=sel[:].rearrange("p (i n) -> p i n", n=16), in0=gm[:].rearrange("p (i n) -> p i n", n=16),
                in1=thr[:].unsqueeze(2).to_broadcast([128, 32, 16]), op=ALU.is_ge),
                reads=[b_gm, b_thr], writes=[b_sel])
            k.op(k.dve, lambda e: e.tensor_scalar(out=sel[:], in0=sel[:], scalar1=-1.0, scalar2=-NEG, op0=ALU.add,
                                                  op1=ALU.mult), reads=[b_sel], writes=[b_sel])
            for g in range(8):
                pi = g % 2
                for i4 in range(4):
                    i = g * 4 + i4
                    k.op(k.pe, lambda e, i=i, i4=i4, pi=pi: e.transpose(
                        out=G.ps[pi][0:16, i4 * 128:(i4 + 1) * 128], in_=sel[:, i * 16:(i + 1) * 16], identity=G.ident_f[:]),
                        reads=[b_sel, G.b_const], writes=[G.psb[pi]])
                k.op(k.act, lambda e, g=g, pi=pi: e.activation(out=biasT[0:16, g * 512:(g + 1) * 512],
                                                                in_=G.ps[pi][0:16, :], func=AF.Copy),
                     reads=[G.psb[pi]], writes=[b_bT])
            items = [(j, n, kt) for j in range(16) for n in range(j + 1) for kt in range(2)]
            LA = 2

            def s_stage(ii):
                j, n, kt = items[ii]
                qs = slice(j * 256, (j + 1) * 256)
                kc0 = n * 256 + kt * 128
                pi = 2 + ii % 4
                k.op(k.pe, lambda e: e.matmul(
                    G.ps[pi][:, 0:256], lhsT=kT[:, kc0:kc0 + 128], rhs=qT[:, qs], start=True, stop=False),
                    reads=[b_k, b_q], writes=[G.psb[pi]])
                if n < j:
                    k.op(k.pe, lambda e: e.matmul(
                        G.ps[pi][:, 0:256], lhsT=selrow[:, n * 128:(n + 1) * 128], rhs=biasT[:, qs],
                        start=False, stop=True), reads=[b_sr, b_bT], writes=[G.psb[pi]])
                else:
                    k.op(k.pe, lambda e: e.matmul(
                        G.ps[pi][:, 0:256], lhsT=G.ident_bf[:], rhs=causal[:, kt * 256:(kt + 1) * 256],
                        start=False, stop=True), reads=[b_cz, G.b_const], writes=[G.psb[pi]])

            def p_stage(ii):
                j, n, kt = items[ii]
                qs = slice(j * 256, (j + 1) * 256)
                po, pd = (6, 7) if j % 2 == 0 else (0, 1)
                pi = 2 + ii % 4
                PT, b_PT = PTs[ii % 4]
                first = (n == 0 and kt == 0)
                last = (n == j and kt == 1)
                k.op(k.act, lambda e: e.activation(out=PT, in_=G.ps[pi][:, 0:256], func=AF.Exp),
                     reads=[G.psb[pi]], writes=[b_PT])
                k.op(k.pe, lambda e: e.matmul(
                    G.ps[po][:, 0:256], lhsT=vt[:, n * 2 + kt, :], rhs=PT, start=first, stop=last),
                    reads=[b_v, b_PT], writes=[G.psb[po]])
                k.op(k.pe, lambda e: e.matmul(
                    G.ps[pd][:, 0:256], lhsT=G.ones_bf[:], rhs=PT, start=first, stop=last),
                    reads=[G.b_const, b_PT], writes=[G.psb[pd]])
                if last:
                    rd, b_rd = rdens[j % 2]
                    k.op(k.dve, lambda e: e.reciprocal(out=rd[:], in_=G.ps[pd][:, 0:256]),
                         reads=[G.psb[pd]], writes=[b_rd])
                    k.op(k.dve, lambda e: e.tensor_tensor(out=oTh[:, qs], in0=G.ps[po][:, 0:256], in1=rd[:], op=ALU.mult),
                         reads=[G.psb[po], b_rd], writes=[b_o])

            for ii in range(len(items) + LA):
                if ii < len(items):
                    s_stage(ii)
                if ii >= LA:
                    p_stage(ii - LA)
            k.op(k.sp, lambda e, h=h: e.dma_start(out=G.oT[:, h, :], in_=oTh), reads=[b_o], writes=[G.o_bufs[h]], dma=True)


def phase_M3(G, layer, wr, src, dst, sb_src, sb_dst):
    k, nc = G.k, G.nc
    W = G.w[("moba", layer)]
    with contextlib.ExitStack() as es:
        sb = lambda name, shape, dt: es.enter_context(nc.sbuf_tensor(G.uniq(name), list(shape), dt))
        regC = sb("regC", [128, 8 * D], BF16)
        wout = WRegion(G, regC, None, W["w_out"], D, D, blk=1024, name="wout")
        xts = [(sb(f"o_xt{i}", [128, 8, T], F32), Buf(f"o_xt{i}")) for i in range(1)]
        ots = [(sb(f"o_ot{i}", [128, 8, T], BF16), Buf(f"o_ot{i}")) for i in range(2)]
        for ti in range(NT):
            xt, b_xt = xts[0]
            ot, b_ot = ots[ti % 2]
            k.op(k.sp, lambda e, ti=ti, xt=xt: e.dma_start(out=xt[:], in_=src[:, :, ti * T:(ti + 1) * T]),
                 reads=sb_src[2 * ti:2 * ti + 2], writes=[b_xt], dma=True)
            k.op(k.act, lambda e, ti=ti, ot=ot: e.dma_start(out=ot[:], in_=G.oT[:, :, ti * T:(ti + 1) * T]),
                 reads=G.o_bufs, writes=[b_ot], dma=True)
            for oc in range(8):
                for h in range(8):
                    k.op(k.pe, lambda e, oc=oc, h=h, ot=ot: e.matmul(
                        G.ps[oc][:], lhsT=wout.view[:, h, oc * 128:(oc + 1) * 128], rhs=ot[:, h, :],
                        start=(h == 0), stop=(h == 7)),
                        reads=[wout.buf(h, oc * 128), b_ot], writes=[G.psb[oc]])
                k.op(k.dve, lambda e, oc=oc, xt=xt: e.tensor_tensor(out=xt[:, oc, :], in0=G.ps[oc][:], in1=xt[:, oc, :],
                                                                   op=ALU.add),
                     reads=[G.psb[oc], b_xt], writes=[b_xt])
            k.op(k.sp, lambda e, ti=ti, xt=xt: e.dma_start(out=dst[:, :, ti * T:(ti + 1) * T], in_=xt[:]),
                 reads=[b_xt], writes=sb_dst[2 * ti:2 * ti + 2], dma=True)


TH = 256
NTH = S // TH


def phase_H1(G, layer, win, src, dst, sb_src, sb_dst):
    k, nc = G.k, G.nc
    W = G.w[("hgrn", layer)]
    slot = layer // 2
    with contextlib.ExitStack() as es:
        sb = lambda name, shape, dt: es.enter_context(nc.sbuf_tensor(G.uniq(name), list(shape), dt))
        regC = sb("regC", [128, 8 * D], BF16)
        wout = WRegion(G, regC, None, W["w_out"], D, D, blk=1024, name="hwout")
        vec = sb("h_vec", [128, 25], F32); b_vec = Buf("h_vec")
        k.op(k.sp, lambda e: e.dma_start(out=vec[:], in_=W["vec"][:, :]), writes=[b_vec], dma=True)
        gain = vec[:, 0:8]
        ogain = vec[:, 24:25]
        cst = sb("h_cst", [128, 2], F32); b_cst = Buf("h_cst")
        k.op(k.pool, lambda e: e.memset(cst[:, 0:1], EPS), writes=[b_cst])
        k.op(k.pool, lambda e: e.memset(cst[:, 1:2], 1.0), writes=[b_cst])
        eps = cst
        lbv = sb("h_lb", [128, 3, 8], F32); b_lb = Buf("h_lb")
        if slot == 0:
            k.op(k.pool, lambda e: e.memset(lbv[:, 0, :], 0.0), writes=[b_lb])
        else:
            k.op(k.dve, lambda e: e.tensor_tensor(out=lbv[:, 0, :], in0=vec[:, 8:16], in1=vec[:, 16:24], op=ALU.subtract),
                 reads=[b_vec], writes=[b_lb])
            k.op(k.act, lambda e: e.activation(out=lbv[:, 0, :], in_=lbv[:, 0, :], func=AF.Exp), reads=[b_lb], writes=[b_lb])
            k.op(k.dve, lambda e: e.tensor_scalar(out=lbv[:, 0, :], in0=lbv[:, 0, :], scalar1=1.0, scalar2=None, op0=ALU.add),
                 reads=[b_lb], writes=[b_lb])
            k.op(k.dve, lambda e: e.reciprocal(out=lbv[:, 0, :], in_=lbv[:, 0, :]), reads=[b_lb], writes=[b_lb])
        k.op(k.dve, lambda e: e.tensor_scalar(out=lbv[:, 1, :], in0=lbv[:, 0, :], scalar1=-1.0, scalar2=1.0, op0=ALU.mult,
                                              op1=ALU.add), reads=[b_lb], writes=[b_lb])
        k.op(k.dve, lambda e: e.tensor_scalar(out=lbv[:, 2, :], in0=lbv[:, 1, :], scalar1=-1.0, scalar2=None, op0=ALU.mult),
             reads=[b_lb], writes=[b_lb])
        tri = sb("h_tri", [128, 128], F32); b_tri = Buf("h_tri")
        k.op(k.sp, lambda e: e.dma_start(out=tri[:], in_=G.c_tri[:, :]), writes=[b_tri], dma=True)
        smask = sb("h_smask", [128, TH], F32); b_sm = Buf("h_smask")
        k.op(k.sp, lambda e: e.dma_start(out=smask[:], in_=G.c_scan[:, 0:TH]), writes=[b_sm], dma=True)
        xt = sb("h_xt", [128, 8, TH], F32); b_xt = Buf("h_xt")
        xo = sb("h_xo", [128, 8, TH], F32); b_xo = Buf("h_xo")
        St = sb("h_S", [128, 8, 128], F32); b_S = [Buf(f"h_S{h}") for h in range(8)]
        k.op(k.pool, lambda e: e.memset(St[:], 0.0), writes=b_S)
        rstd = (sb("h_rstd", [128, TH], F32), Buf("h_rstd"))
        sets = []
        for i in range(2):
            d_ = dict(
                b=[(sb(f"h_b{i}_{j}", [128, TH], F32), Buf(f"h_b{i}_{j}")) for j in range(4)],
                qin=(sb(f"h_qin{i}", [128, TH], F32), Buf(f"h_qin{i}")),
                egl=(sb(f"h_egl{i}", [128, 4], F32), Buf(f"h_egl{i}")),
                r2=(sb(f"h_r2{i}", [128, TH], F32), Buf(f"h_r2{i}")),
                tmp=(sb(f"h_tmp{i}", [128, TH], F32), Buf(f"h_tmp{i}")),
            )
            sets.append(d_)
        off = 8 * 4 * D
        hTs = []
        for i in range(2):
            v, off = carve(G.regA, off, [128, 8, TH]); hTs.append((v, Buf(f"h_hT{i}")))
        vtok, off = carve(G.regA, off, [128, 2, D]); b_vtok = Buf("h_vtok")
        sgate, off = carve(G.regA, off, [128, 8, TH]); b_sg = [Buf(f"h_sg{h}") for h in range(8)]
        oTn, off = carve(G.regA, off, [128, 8, TH]); b_oTn = [Buf(f"h_oTn{h}") for h in range(8)]
        sqs = []
        for i in range(2):
            v, off = carve(G.regA, off, [128, TH]); sqs.append((v, Buf(f"h_sq{i}")))
        for i in range(2):
            d_ = sets[i]
            v, off = carve(G.regA, off, [128, TH]); d_["qrel"] = (v, Buf(f"h_qrel{i}"))
            v, off = carve(G.regA, off, [128, TH]); d_["krel"] = (v, Buf(f"h_krel{i}"))
            v, off = carve(G.regA, off, [128, 2, 128]); d_["attT"] = (v, Buf(f"h_attT{i}"))
            v, off = carve(G.regA, off, [128, 4, 128]); d_["kz"] = (v, Buf(f"h_kz{i}"))
            v, off = carve(G.regA, off, [128, TH]); d_["sqo"] = (v, Buf(f"h_sqo{i}"))
            k.op(k.pool, lambda e, v=d_["kz"][0]: e.memset(v, 0.0), writes=[d_["kz"][1]])
        assert off <= 49152, off
        L = dict(sq=sqs, rstd=rstd, eps=eps, b_eps=b_cst, b_vec=b_vec)
        pA = [Buf(f"h_pA{b}") for b in range(8)]
        pB = [Buf(f"h_pB{b}") for b in range(8)]
        lo = lambda b: G.ps[b][:, 0:TH]
        hi = lambda b: G.ps[b][:, TH:2 * TH]
        b_dS = [[Buf(f"h_dS{i}_{c}") for c in range(4)] for i in range(2)]
        dS_ps = lambda i, c: G.ps[7 - i][:, c * 128:(c + 1) * 128]
        bank = lambda b: [pA[b], pB[b]] if b < 6 else b_dS[7 - b]
        SCL = 128.0 ** -0.5

        k.op(k.sp, lambda e: e.dma_start(out=xt[:], in_=src[:, :, 0:TH]), reads=[sb_src[0]], writes=[b_xt], dma=True)
        for ti in range(NTH):
            c0 = ti * TH
            hT, b_hT = hTs[ti % 2]
            rmsnorm_tile(G, L, xt, b_xt, gain, hT, b_hT, ps_i=0, ps_ap=lo(0), ps_buf=bank(0))
            if ti + 1 < NTH:
                k.op(k.sp, lambda e, ti=ti: e.dma_start(out=xt[:], in_=src[:, :, (ti + 1) * TH:(ti + 2) * TH]),
                     reads=[sb_src[ti + 1]], writes=[b_xt], dma=True)
            k.op(k.sp, lambda e, c0=c0: e.dma_start(out=xo[:], in_=src[:, :, c0:c0 + TH]), reads=[sb_src[ti]],
                 writes=[b_xo], dma=True)
            for sub in range(2):
                for half in range(2):
                    pi = 1 + (sub * 2 + half) % 2
                    for kc in range(8):
                        k.op(k.pe, lambda e, kc=kc, sub=sub, half=half, pi=pi: e.matmul(
                            G.ps[pi][:], lhsT=hT[:, kc, sub * 128:(sub + 1) * 128],
                            rhs=win.view[:, kc, 2 * D + half * 512:2 * D + (half + 1) * 512],
                            start=(kc == 0), stop=(kc == 7)),
                            reads=[b_hT, win.buf(kc, 2 * D + half * 512)], writes=bank(pi))
                    k.op(k.act, lambda e, sub=sub, half=half, pi=pi: e.activation(
                        out=vtok[:, sub, half * 512:(half + 1) * 512], in_=G.ps[pi][:], func=AF.Copy),
                        reads=[pA[pi], pB[pi]], writes=[b_vtok])
            for h in range(8):
                pi = 3 + h % 2
                for kc in range(8):
                    k.op(k.pe, lambda e, kc=kc, h=h, pi=pi: e.matmul(
                        lo(pi), lhsT=win.view[:, kc, 3 * D + h * 128:3 * D + (h + 1) * 128], rhs=hT[:, kc, :],
                        start=(kc == 0), stop=(kc == 7)),
                        reads=[b_hT, win.buf(kc, 3 * D + h * 128)], writes=bank(pi))
                k.op(k.act, lambda e, h=h, pi=pi: e.activation(out=sgate[:, h, :], in_=lo(pi), func=AF.Silu),
                     reads=[pA[pi]], writes=[b_sg[h]])
            for hp in range(4):
                hh = (2 * hp, 2 * hp + 1)
                def head_elem(i, h):
                    st = sets[i]
                    pq, pz = 0 + i, 2 + i
                    (b1, B1), (b2, B2), (b3, B3), (b4, B4) = st["b"]
                    for kc in range(8):
                        k.op(k.pe, lambda e, kc=kc, h=h, pq=pq: e.matmul(
                            lo(pq), lhsT=win.view[:, kc, h * 128:(h + 1) * 128], rhs=hT[:, kc, :],
                            start=(kc == 0), stop=(kc == 7)), reads=[b_hT, win.buf(kc, h * 128)], writes=bank(pq))
                    for kc in range(8):
                        k.op(k.pe, lambda e, kc=kc, h=h, pz=pz: e.matmul(
                            lo(pz), lhsT=win.view[:, kc, D + h * 128:D + (h + 1) * 128], rhs=hT[:, kc, :],
                            start=(kc == 0), stop=(kc == 7)), reads=[b_hT, win.buf(kc, D + h * 128)], writes=bank(pz))
                    k.op(k.act, lambda e, b1=b1, pz=pz: e.activation(out=b1[:], in_=lo(pz), func=AF.Exp, scale=-1.0),
                         reads=[pA[pz]], writes=[B1])
                    yield
                    k.op(k.pool, lambda e, b1=b1: e.tensor_scalar(out=b1[:], in0=b1[:], scalar1=1.0, scalar2=None, op0=ALU.add),
                         reads=[B1], writes=[B1])
                    yield
                    k.op(k.dve, lambda e, b1=b1: e.reciprocal(out=b1[:], in_=b1[:]), reads=[B1], writes=[B1])
                    yield
                    k.op(k.act, lambda e, b1=b1, b2=b2, h=h: e.activation(out=b2[:], in_=b1[:], func=AF.Ln, bias=lbv[:, 0, h:h + 1],
                                                                        scale=lbv[:, 1, h:h + 1]),
                         reads=[B1, b_lb], writes=[B2])
                    yield
                    k.op(k.dve, lambda e, b1=b1, h=h: e.tensor_scalar(out=b1[:], in0=b1[:], scalar1=lbv[:, 2, h:h + 1],
                                                                     scalar2=lbv[:, 1, h:h + 1], op0=ALU.mult, op1=ALU.add),
                         reads=[B1, b_lb], writes=[B1])
                    yield
                    k.op(k.dve, lambda e, b2=b2, b3=b3: e.tensor_tensor_scan(out=b3[:], data0=smask[:], data1=b2[:], initial=0.0,
                                                                            op0=ALU.mult, op1=ALU.add),
                         reads=[B2, b_sm], writes=[B3])
                    yield
                    G3 = b3[:].rearrange("p (c t) -> p c t", t=64)
                    k.op(k.dve, lambda e, b2=b2, G3=G3: e.tensor_tensor(
                        out=b2[:].rearrange("p (c t) -> p c t", t=64), in0=G3, in1=G3[:, :, 31:32].to_broadcast([128, 4, 64]),
                        op=ALU.subtract), reads=[B3], writes=[B2])
                    yield
                    k.op(k.act, lambda e, b2=b2, b4=b4: e.activation(out=b4[:], in_=b2[:], func=AF.Exp), reads=[B2], writes=[B4])
                    yield
                    qrel, Bqrel = st["qrel"]
                    k.op(k.dve, lambda e, qrel=qrel, b4=b4, pq=pq: e.scalar_tensor_tensor(
                        out=qrel, in0=lo(pq), scalar=SCL, in1=b4[:], op0=ALU.mult, op1=ALU.mult),
                        reads=[pA[pq], B4], writes=[Bqrel])
                    yield
                    k.op(k.act, lambda e, b2=b2: e.activation(out=b2[:], in_=b2[:], func=AF.Exp, scale=-1.0), reads=[B2], writes=[B2])
                    yield
                    krel, Bkrel = st["krel"]
                    k.op(k.dve, lambda e, krel=krel, b1=b1, b2=b2: e.tensor_tensor(out=krel, in0=b1[:], in1=b2[:], op=ALU.mult),
                         reads=[B1, B2], writes=[Bkrel])
                    yield
                    k.op(k.act, lambda e, b3=b3, b4=b4: e.activation(out=b4[:], in_=b3[:], func=AF.Exp), reads=[B3], writes=[B4])
                    yield
                    qin, Bqin = st["qin"]
                    k.op(k.dve, lambda e, qin=qin, b4=b4, pq=pq: e.scalar_tensor_tensor(
                        out=qin[:], in0=lo(pq), scalar=SCL, in1=b4[:], op0=ALU.mult, op1=ALU.mult),
                        reads=[pA[pq], B4], writes=[Bqin])
                    yield
                    k.op(k.dve, lambda e, b2=b2, G3=G3: e.tensor_tensor(
                        out=b2[:].rearrange("p (c t) -> p c t", t=64), in0=G3, in1=G3[:, :, 63:64].to_broadcast([128, 4, 64]),
                        op=ALU.subtract), reads=[B3], writes=[B2])
                    yield
                    k.op(k.act, lambda e, b2=b2: e.activation(out=b2[:], in_=b2[:], func=AF.Exp, scale=-1.0), reads=[B2], writes=[B2])
                    yield
                    k.op(k.dve, lambda e, b1=b1, b2=b2, b4=b4: e.tensor_tensor(out=b4[:], in0=b1[:], in1=b2[:], op=ALU.mult),
                         reads=[B1, B2], writes=[B4])
                    yield
                    egl, Begl = st["egl"]
                    k.op(k.act, lambda e, egl=egl, G3=G3: e.activation(out=egl[:], in_=G3[:, :, 63], func=AF.Exp),
                         reads=[B3], writes=[Begl])
                    yield
                    kTp = lo(5) if i == 0 else hi(5)
                    BkT = pA[5] if i == 0 else pB[5]
                    for pr in range(2):
                        k.op(k.pe, lambda e, pr=pr, b4=b4, kTp=kTp: e.transpose(
                            out=kTp[:, pr * 128:(pr + 1) * 128], in_=b4[:, pr * 128:(pr + 1) * 128],
                            identity=G.ident_f[:]), reads=[B4, G.b_const], writes=bank(5))
                    kz, Bkz = st["kz"]
                    kzv = kz.rearrange("p (pr cc) d -> p pr cc d", cc=2)
                    k.op(k.act, lambda e, kzv=kzv, kTp=kTp: e.activation(
                        out=kzv[0:64, :, 0, :], in_=kTp[0:64, :].rearrange("p (pr d) -> p pr d", d=128),
                        func=AF.Copy), reads=[BkT], writes=[Bkz])
                    yield
                    k.op(k.act, lambda e, kzv=kzv, kTp=kTp: e.activation(
                        out=kzv[64:128, :, 1, :], in_=kTp[64:128, :].rearrange("p (pr d) -> p pr d", d=128),
                        func=AF.Copy), reads=[BkT], writes=[Bkz])
                    yield
                    aTp = lo(4) if i == 0 else hi(4)
                    BaT = pA[4] if i == 0 else pB[4]
                    for pr in range(2):
                        k.op(k.pe, lambda e, pr=pr, aTp=aTp, krel=krel, qrel=qrel: e.matmul(
                            aTp[:, pr * 128:(pr + 1) * 128], lhsT=krel[:, pr * 128:(pr + 1) * 128],
                            rhs=qrel[:, pr * 128:(pr + 1) * 128], start=True, stop=True),
                            reads=[Bkrel, Bqrel], writes=bank(4))
                    attT, BattT = st["attT"]
                    k.op(k.dve, lambda e, attT=attT, aTp=aTp: e.tensor_tensor(
                        out=attT, in0=aTp.rearrange("p (pr t) -> p pr t", t=128),
                        in1=tri[:].unsqueeze(1).to_broadcast([128, 2, 128]), op=ALU.mult),
                        reads=[BaT, b_tri], writes=[BattT])
                    yield
                    for c in range(4):
                        k.op(k.pe, lambda e, c=c, i=i, kz=kz, h=h: e.matmul(
                            dS_ps(i, c), lhsT=kz[:, c, :], rhs=vtok[:, c // 2, h * 128:(h + 1) * 128], start=True, stop=True),
                            reads=[Bkz, b_vtok], writes=bank(7 - i))

                interleave([head_elem(i_, h_) for i_, h_ in enumerate(hh)])
                for c in range(4):
                    for i, h in enumerate(hh):
                        st = sets[i]
                        qin, Bqin = st["qin"]
                        attT, BattT = st["attT"]
                        egl, Begl = st["egl"]
                        pr, cc = c // 2, c % 2
                        k.op(k.pe, lambda e, c=c, i=i, h=h, qin=qin: e.matmul(
                            hi(i)[:, c * 64:(c + 1) * 64], lhsT=St[:, h, :], rhs=qin[:, c * 64:(c + 1) * 64],
                            start=True, stop=False), reads=[b_S[h], Bqin], writes=bank(i))
                        k.op(k.pe, lambda e, c=c, i=i, h=h, attT=attT, pr=pr, cc=cc: e.matmul(
                            hi(i)[:, c * 64:(c + 1) * 64], lhsT=vtok[:, pr, h * 128:(h + 1) * 128],
                            rhs=attT[:, pr, cc * 64:(cc + 1) * 64], start=False, stop=True),
                            reads=[b_vtok, BattT], writes=bank(i))
                        k.op(k.dve, lambda e, c=c, i=i, h=h, egl=egl: e.scalar_tensor_tensor(
                            out=St[:, h, :], in0=St[:, h, :], scalar=egl[:, c:c + 1], in1=dS_ps(i, c),
                            op0=ALU.mult, op1=ALU.add),
                            reads=[b_S[h], Begl, b_dS[i][c]], writes=[b_S[h]])
                def head_norm(i, h):
                    st = sets[i]
                    sqo, Bsqo = st["sqo"]
                    r2, Br2 = st["r2"]
                    tmp, Btmp = st["tmp"]
                    k.op(k.act, lambda e, sqo=sqo, i=i: e.activation(out=sqo, in_=hi(i), func=AF.Square),
                         reads=[pB[i]], writes=[Bsqo])
                    yield
                    k.op(k.pe, lambda e, sqo=sqo, i=i: e.matmul(hi(2 + i), lhsT=G.ones_bf[:], rhs=sqo, start=True, stop=True),
                         reads=[Bsqo, G.b_const], writes=bank(2 + i))
                    yield
                    k.op(k.act, lambda e, r2=r2, i=i: e.activation(out=r2[:], in_=hi(2 + i), func=AF.Ln,
                                                                   bias=cst[:, 0:1], scale=1.0 / 128),
                         reads=[pB[2 + i], b_cst], writes=[Br2])
                    yield
                    k.op(k.act, lambda e, r2=r2: e.activation(out=r2[:], in_=r2[:], func=AF.Exp, scale=-0.5), reads=[Br2], writes=[Br2])
                    yield
                    k.op(k.dve, lambda e, tmp=tmp, r2=r2, i=i: e.scalar_tensor_tensor(
                        out=tmp[:], in0=hi(i), scalar=ogain, in1=r2[:], op0=ALU.mult, op1=ALU.mult),
                        reads=[pB[i], Br2, b_vec], writes=[Btmp])
                    yield
                    k.op(k.pool, lambda e, tmp=tmp, h=h: e.tensor_tensor(out=oTn[:, h, :], in0=tmp[:], in1=sgate[:, h, :], op=ALU.mult),
                         reads=[Btmp, b_sg[h]], writes=[b_oTn[h]])
                    yield

                interleave([head_norm(i_, h_) for i_, h_ in enumerate(hh)])
            for oc in range(8):
                pi = oc % 2
                for h in range(8):
                    k.op(k.pe, lambda e, oc=oc, h=h, pi=pi: e.matmul(
                        lo(pi), lhsT=wout.view[:, h, oc * 128:(oc + 1) * 128], rhs=oTn[:, h, :],
                        start=(h == 0), stop=(h == 7)), reads=[wout.buf(h, oc * 128), b_oTn[h]], writes=bank(pi))
                k.op(k.dve, lambda e, oc=oc, pi=pi: e.tensor_tensor(out=xo[:, oc, :], in0=lo(pi), in1=xo[:, oc, :],
                                                                   op=ALU.add), reads=[pA[pi], b_xo], writes=[b_xo])
            k.op(k.sp, lambda e, c0=c0: e.dma_start(out=dst[:, :, c0:c0 + TH], in_=xo[:]), reads=[b_xo], writes=[sb_dst[ti]],
                 dma=True)


PHASE_FN = {"F1": phase_F1, "F2": phase_F2, "M1": phase_M1, "M2": phase_M2, "M3": phase_M3, "H1": phase_H1}


def fm(v, n):
    return np.ascontiguousarray(np.asarray(v, np.float32).reshape(n, 128).T)


def host_consts(kinds):
    c = {"c_ones": np.ones((128, 128), np.float32), "c_ident": np.eye(128, dtype=np.float32)}
    if "moba" in kinds:
        rot = np.zeros((128, 128), np.float32)
        for m_ in range(64):
            rot[m_ + 64, m_] = -1.0
            rot[m_, m_ + 64] = 1.0
        c["c_rot"] = rot
        inv = (1.0 / (np.float32(10000.0) ** (np.arange(0, 128, 2, dtype=np.float32) / np.float32(128)))).astype(np.float32)
        ang = (np.arange(S, dtype=np.float32)[:, None] * inv[None, :]).astype(np.float32)
        ang = np.concatenate([ang, ang], axis=-1)
        c["c_cos"] = np.ascontiguousarray(np.cos(ang).astype(np.float32).T)
        c["c_sin"] = np.ascontiguousarray(np.sin(ang).astype(np.float32).T)
        past = np.full((32, 16), -1e30, np.float32)
        for i in range(32):
            past[i, :i // 2] = 0.0
        c["c_past"] = np.ascontiguousarray(np.broadcast_to(past.reshape(1, 512), (128, 512)))
        causal = np.full((128, 2, 256), NEG, np.float32)
        for kt in range(2):
            for p in range(128):
                causal[p, kt, kt * 128 + p:] = 0.0
        c["c_causal"] = causal.reshape(128, 512)
        selrow = np.zeros((128, 16, 128), np.float32)
        for n_ in range(16):
            selrow[n_, n_, :] = 1.0
        c["c_selrow"] = selrow.reshape(128, 2048)
    if "hgrn" in kinds:
        tri = np.zeros((128, 128), np.float32)
        for s_ in range(128):
            for t_ in range(128):
                if s_ // 64 == t_ // 64 and s_ <= t_:
                    tri[s_, t_] = 1.0
        c["c_tri"] = tri
        sm = np.ones((128, T), np.float32)
        sm[:, ::64] = 0.0
        c["c_scan"] = sm
    return c


def stage_inputs(inputs, stages):
    m = {}
    for kind, l in stages:
        if kind == "ffn":
            m[f"ffn_w_up_{l}"] = np.ascontiguousarray(inputs["ffn_w_up"][l])
            m[f"ffn_w_dn_{l}"] = np.ascontiguousarray(inputs["ffn_w_down"][l])
            cw = inputs["ffn_conv_w"][l]
            m[f"ffn_vec_{l}"] = np.ascontiguousarray(np.concatenate(
                [fm(inputs["ffn_norm"][l], 8), fm(cw[0], 24), fm(cw[1], 24), fm(cw[2], 24),
                 fm(inputs["ffn_conv_b"][l], 24)], axis=1))
        elif kind == "hgrn":
            sl = l // 2
            m[f"hgrn_w_in_{l}"] = np.ascontiguousarray(inputs["hgrn_w_in"][sl])
            m[f"hgrn_w_out_{l}"] = np.ascontiguousarray(inputs["hgrn_w_out"][sl])
            m[f"hgrn_vec_{l}"] = np.ascontiguousarray(np.concatenate(
                [fm(inputs["attn_norm"][l], 8), fm(inputs["hgrn_lb"][0], 8), fm(inputs["hgrn_lb"][1], 8),
                 fm(inputs["hgrn_out_norm"][sl], 1)], axis=1))
        elif kind == "moba":
            sl = l // 2
            m[f"moba_w_qkv_{l}"] = np.ascontiguousarray(inputs["moba_w_qkv"][sl])
            m[f"moba_w_out_{l}"] = np.ascontiguousarray(inputs["moba_w_out"][sl])
            m[f"moba_vec_{l}"] = np.ascontiguousarray(np.concatenate(
                [fm(inputs["attn_norm"][l], 8), fm(inputs["moba_q_norm"][sl], 1), fm(inputs["moba_k_norm"][sl], 1)], axis=1))
    return m


def x_to_dev(xb):
    return np.ascontiguousarray(xb.T.reshape(8, 128, S).transpose(1, 0, 2))


def x_from_dev(y):
    return np.ascontiguousarray(y.transpose(1, 0, 2).reshape(D, S).T)


FUSED = True
LAYER_STAGES = [[("hgrn", 0), ("ffn", 0)], [("moba", 1), ("ffn", 1)], [("hgrn", 2), ("ffn", 2)], [("moba", 3), ("ffn", 3)]]


def kernel(**inputs):
    inputs = {k_: np.asarray(v) for k_, v in inputs.items()}
    x = inputs["x"].astype(np.float32)
    nb = x.shape[0]
    groups = [sum(LAYER_STAGES, [])] if FUSED else LAYER_STAGES
    xs = [x_to_dev(x[b]) for b in range(nb)]
    for stages in groups:
        P = build_program(stages)
        shared = dict(host_consts({kd for kd, _ in stages}))
        shared.update(stage_inputs(inputs, stages))
        shared = {k_: v for k_, v in shared.items() if k_ in P.ext_in}
        missing = set(P.ext_in) - set(shared) - {"xin"}
        assert not missing, missing
        in_maps = [dict(shared, xin=xs[b]) for b in range(nb)]
        res = run_bass_kernel_spmd(P.nc, in_maps, core_ids=list(range(nb)))
        xs = [np.asarray(res.results[b]["yout"], dtype=np.float32) for b in range(nb)]
    return np.stack([x_from_dev(xs[b]) for b in range(nb)]).astype(np.float32)
```

```python
import contextlib
import numpy as np
import concourse.bass as bass
import concourse.mybir as mybir
from concourse.bass_utils import run_bass_kernel_spmd

F32 = mybir.dt.float32
BF16 = mybir.dt.bfloat16
AF = mybir.ActivationFunctionType
ALU = mybir.AluOpType
AX = mybir.AxisListType

D = 1024
S = 4096
T = 512
NT = S // T
DEPTH = 4
FF = 3072
EPS = 1e-6
NEG = -30000.0


class Sem:
    __slots__ = ("h", "v")

    def __init__(self, h):
        self.h = h
        self.v = 0


class Buf:
    __slots__ = ("name", "w", "r")

    def __init__(self, name=""):
        self.name = name
        self.w = None
        self.r = {}


class Eng:
    def __init__(self, k, name, h, is_pe=False):
        self.k = k
        self.name = name
        self.h = h
        self.is_pe = is_pe
        self.sem = k.new_sem(name)
        self.dma_sems = []
        self.dma_i = 0
        self.waited = {}
        self.n_ops = 0
        self.n_waits = 0

    def next_dma_sem(self):
        if not self.dma_sems:
            n = 32 if self.name == "pool" else 16
            self.dma_sems = [self.k.new_sem(f"{self.name}_dma{i}") for i in range(n)]
        s = self.dma_sems[self.dma_i % len(self.dma_sems)]
        self.dma_i += 1
        return s


class K:
    def __init__(self, nc, es):
        self.nc = nc
        self.es = es
        self.sems = []
        self.pe = Eng(self, "pe", nc.tensor, is_pe=True)
        self.act = Eng(self, "act", nc.scalar)
        self.dve = Eng(self, "dve", nc.vector)
        self.pool = Eng(self, "pool", nc.gpsimd)
        self.sp = Eng(self, "sp", nc.sync)
        self.engs = [self.pe, self.act, self.dve, self.pool, self.sp]

    def new_sem(self, name):
        h = self.es.enter_context(self.nc.semaphore(f"s_{name}_{len(self.sems)}"))
        s = Sem(h)
        self.sems.append(s)
        return s

    def op(self, eng, fn, reads=(), writes=(), dma=False, after=None):
        deps = {}
        if after:
            for s, v in after.items():
                if deps.get(s, 0) < v:
                    deps[s] = v
        for b in reads:
            if b.w is not None and deps.get(b.w[0], 0) < b.w[1]:
                deps[b.w[0]] = b.w[1]
        for b in writes:
            if b.w is not None and deps.get(b.w[0], 0) < b.w[1]:
                deps[b.w[0]] = b.w[1]
            for s, v in b.r.items():
                if deps.get(s, 0) < v:
                    deps[s] = v
        for s, v in deps.items():
            if eng.is_pe and s is eng.sem:
                continue
            if eng.waited.get(s, 0) >= v:
                continue
            eng.h.wait_ge(s.h, v)
            eng.waited[s] = v
            eng.n_waits += 1
        if dma:
            ds = eng.next_dma_sem()
            if ds.v > 0 and eng.waited.get(ds, 0) < ds.v:
                eng.h.wait_ge(ds.h, ds.v)
                eng.waited[ds] = ds.v
                eng.n_waits += 1
        ins = fn(eng.h)
        eng.n_ops += 1
        if dma:
            s = ds
            s.v += 16
            ins.then_inc(s.h, 16)
        else:
            if eng.sem.v >= 30000:
                eng.sem = self.new_sem(eng.name)
            s = eng.sem
            s.v += 1
            ins.then_inc(s.h, 1)
        ev = (s, s.v)
        for b in reads:
            if b.r.get(s, 0) < s.v:
                b.r[s] = s.v
        for b in writes:
            b.w = ev
            b.r = {}
        return ev

    def snapshot(self):
        return {s: s.v for s in self.sems if s.v > 0}

    def barrier(self, snap, engines=None):
        for e in (engines or self.engs):
            for s, v in snap.items():
                if e.waited.get(s, 0) >= v:
                    continue
                e.h.wait_ge(s.h, v)
                e.waited[s] = v
                e.n_waits += 1


class Prog:
    def __init__(self, stages):
        self.stages = stages
        self.nc = bass.Bass("TRN2", target_bir_lowering=False)
        self.ext_in = {}

    def dram_in(self, name, shape, dt=F32):
        t = self.nc.dram_tensor(name, list(shape), dt, kind="ExternalInput")
        self.ext_in[name] = tuple(shape)
        return t.ap()

    def dram_tmp(self, name, shape, dt):
        kind = "ExternalOutput" if DEBUG_SCRATCH else "Internal"
        return self.nc.dram_tensor(name, list(shape), dt, kind=kind).ap()


DEBUG_SCRATCH = False
DEBUG_ONLY = None
SUBPHASES = {"ffn": ["F1", "F2"], "moba": ["M1", "M2", "M3"], "hgrn": ["H1"]}
NEEDS = {"F1": ("A", "F1", 0, 0, 0),
         "M1": ("A", "w_qkv", D, 3 * D, 1024), "H1": ("A", "w_in", D, 4 * D, 2048),
         "M2": ("A", None, 0, 0, 0)}


def build_program(stages):
    P = Prog(stages)
    nc = P.nc
    with contextlib.ExitStack() as es:
        k = K(nc, es)
        G = _Globals(P, k, es)
        n = len(stages)
        subs = []
        for i, (kind, layer) in enumerate(stages):
            io = dict(src=G.xin if i == 0 else G.xres, dst=G.yout if i == n - 1 else G.xres,
                      sb_src=G.xin_bufs if i == 0 else G.xres_bufs,
                      sb_dst=G.yout_bufs if i == n - 1 else G.xres_bufs)
            for sp in SUBPHASES[kind]:
                subs.append((sp, kind, layer, io))
        loaded = {}
        last_user = {"A": -1, "B": -1}
        regs = {"A": G.regA, "B": G.regB}
        for i, (sp, kind, layer, io) in enumerate(subs):
            snap = phase_begin(G)
            for r in ("A",):
                for j in range(i, len(subs)):
                    nd = NEEDS.get(subs[j][0])
                    if nd is not None and nd[0] == r:
                        if j not in loaded and last_user[r] < i:
                            W = G.w[(subs[j][1], subs[j][2])]
                            if nd[1] is None:
                                if j != i:
                                    break
                                loaded[j] = None
                            elif nd[1] == "F1":
                                lay = subs[j][2]
                                loaded[j] = [wup_third(G, lay, 1, G.regA, 0, snap), wup_third(G, lay, 2, G.regA, 16384, snap)]
                            else:
                                loaded[j] = WRegion(G, regs[r], snap, W[nd[1]], nd[2], nd[3], blk=nd[4], name=nd[1])
                            last_user[r] = j
                        break
            wr = loaded.get(i)
            if sp in ("M1", "H1") or (sp == "F1" and ("ffn", layer) not in G.wup0):
                for j in range(i, len(subs)):
                    if subs[j][0] == "F1":
                        lay = subs[j][2]
                        if ("ffn", lay) not in G.wup0:
                            G.wup0[("ffn", lay)] = wup_third(G, lay, 0, G.regB, 0, snap)
                        break
            if DEBUG_ONLY is None or sp in DEBUG_ONLY:
                PHASE_FN[sp](G, layer, wr, **io)
        k.barrier(k.snapshot(), engines=[k.sp])
        P.stats = {e.name: (e.n_ops, e.n_waits) for e in k.engs}
        P.nsems = len(k.sems)
    return P


class _Globals:
    def uniq(self, name):
        self._uid = getattr(self, "_uid", 0) + 1
        return f"{name}_u{self._uid}"

    def __init__(self, P, k, es):
        self.P = P
        self.k = k
        self.es = es
        nc = P.nc
        self.nc = nc
        stages = P.stages
        self.xin = P.dram_in("xin", [128, 8, S])
        self.yout = nc.dram_tensor("yout", [128, 8, S], F32, kind="ExternalOutput").ap()
        self.xres = P.dram_tmp("xres", [128, 8, S], F32)
        self.xin_bufs = [Buf(f"xin{t}") for t in range(16)]
        self.yout_bufs = [Buf(f"yout{t}") for t in range(16)]
        self.xres_bufs = [Buf(f"xres{t}") for t in range(16)]
        kinds = {kd for kd, _ in stages}
        self.c_ones = P.dram_in("c_ones", [128, 128])
        self.c_ident = P.dram_in("c_ident", [128, 128])
        sb = lambda name, shape, dt: es.enter_context(nc.sbuf_tensor(name, list(shape), dt))
        self.sb = sb
        self.ones_bf = sb("ones_bf", [128, 128], BF16)
        self.ident_bf = sb("ident_bf", [128, 128], BF16)
        self.ident_f = sb("ident_f", [128, 128], F32)
        self.b_const = Buf("consts")
        k.op(k.pool, lambda e: e.dma_start(out=self.ones_bf[:], in_=self.c_ones[:, :]), writes=[self.b_const], dma=True)
        k.op(k.pool, lambda e: e.dma_start(out=self.ident_bf[:], in_=self.c_ident[:, :]), writes=[self.b_const], dma=True)
        k.op(k.sp, lambda e: e.dma_start(out=self.ident_f[:], in_=self.c_ident[:, :]), writes=[self.b_const], dma=True)
        self.regA = sb("regA", [128, 49152], BF16)
        self.regB = sb("regB", [128, 24576], BF16)
        self.wup0 = {}
        self.wdn = {}
        self.ps = [es.enter_context(nc.psum_tensor(f"psb{i}", [128, 512], F32)) for i in range(8)]
        self.psb = [Buf(f"psb{i}") for i in range(8)]
        self.w = {}
        for kind, l in stages:
            if kind == "ffn":
                self.w[("ffn", l)] = dict(
                    w_up=P.dram_in(f"ffn_w_up_{l}", [D, 2 * FF]),
                    w_dn=P.dram_in(f"ffn_w_dn_{l}", [FF, D]),
                    vec=P.dram_in(f"ffn_vec_{l}", [128, 8 + 24 * 4]),
                )
            elif kind == "moba":
                self.w[("moba", l)] = dict(
                    w_qkv=P.dram_in(f"moba_w_qkv_{l}", [D, 3 * D]),
                    w_out=P.dram_in(f"moba_w_out_{l}", [D, D]),
                    vec=P.dram_in(f"moba_vec_{l}", [128, 8 + 2]),
                )
            elif kind == "hgrn":
                self.w[("hgrn", l)] = dict(
                    w_in=P.dram_in(f"hgrn_w_in_{l}", [D, 4 * D]),
                    w_out=P.dram_in(f"hgrn_w_out_{l}", [D, D]),
                    vec=P.dram_in(f"hgrn_vec_{l}", [128, 8 + 16 + 1]),
                )
        if "ffn" in kinds:
            self.gT = P.dram_tmp("gT", [128, 24, S], BF16)
            self.gT_bufs = [[Buf(f"gT{t}_{g}") for g in range(6)] for t in range(NT)]
        if "moba" in kinds:
            self.c_rot = P.dram_in("c_rot", [128, 128])
            self.c_cos = P.dram_in("c_cos", [128, S])
            self.c_sin = P.dram_in("c_sin", [128, S])
            self.c_past = P.dram_in("c_past", [128, 32 * 16])
            self.c_causal = P.dram_in("c_causal", [128, 2 * 256])
            self.c_selrow = P.dram_in("c_selrow", [128, 16 * 128])
            self.qT = P.dram_tmp("qT", [8, 128, S], BF16)
            self.kT = P.dram_tmp("kT", [8, 128, S], BF16)
            self.vtok = P.dram_tmp("vtok", [128, 32, D], BF16)
            self.oT = P.dram_tmp("oT", [128, 8, S], BF16)
            self.q_bufs = [[Buf(f"q{h}_{t}") for t in range(NT)] for h in range(8)]
            self.k_bufs = [[Buf(f"k{h}_{t}") for t in range(NT)] for h in range(8)]
            self.v_bufs = [Buf(f"v{t}") for t in range(NT)]
            self.o_bufs = [Buf(f"o{h}") for h in range(8)]
        if "hgrn" in kinds:
            self.c_tri = P.dram_in("c_tri", [128, 128])
            self.c_scan = P.dram_in("c_scan", [128, T])


class WRegion:
    def __init__(self, G, reg, free_after, w_ap, kdim, ncols, col0=0, blk=2048, name="w", segs=None, reg_off=0):
        k = G.k
        if segs is None:
            segs = [(col0, ncols)]
        ncols = sum(n for _, n in segs)
        self.kc = kdim // 128
        self.ncols = ncols
        self.blk = min(blk, min(n for _, n in segs))
        self.view = reg[:, reg_off:reg_off + self.kc * ncols].rearrange("p (kc n) -> p kc n", n=ncols)
        self.bufs = {}
        wv = w_ap.rearrange("(kc p) n -> p kc n", p=128)
        for kc in range(self.kc):
            d0 = 0
            for (c0, n) in segs:
                for j in range(n // self.blk):
                    b = Buf(f"{name}_{kc}_{d0 // self.blk}")
                    self.bufs[(kc, d0 // self.blk)] = b
                    k.op(k.pool,
                         lambda e, kc=kc, d0=d0, s0=c0 + j * self.blk: e.dma_start(
                             out=self.view[:, kc, d0:d0 + self.blk], in_=wv[:, kc, s0:s0 + self.blk]),
                         writes=[b], dma=True, after=free_after)
                    d0 += self.blk

    def buf(self, kc, n0):
        return self.bufs[(kc, n0 // self.blk)]


def wup_third(G, layer, g, reg, reg_off, free_after):
    W = G.w[("ffn", layer)]
    return WRegion(G, reg, free_after, W["w_up"], D, 2048, blk=1024, name=f"wup{g}",
                   segs=[(g * 1024, 1024), (FF + g * 1024, 1024)], reg_off=reg_off)


def rmsnorm_tile(G, L, xt, b_xt, gain, hT, b_hT, ps_i, nfeat_chunks=8, ps_ap=None, ps_buf=None):
    k = G.k
    ps, pb = (G.ps[ps_i], G.psb[ps_i]) if ps_ap is None else (ps_ap, ps_buf)
    for c in range(nfeat_chunks):
        sq, b_sq = L["sq"][c % 2]
        k.op(k.act, lambda e, c=c, sq=sq: e.activation(out=sq[:], in_=xt[:, c, :], func=AF.Square),
             reads=[b_xt], writes=[b_sq])
        k.op(k.pe, lambda e, c=c, sq=sq: e.matmul(ps if ps_ap is not None else ps[:], lhsT=G.ones_bf[:], rhs=sq[:], start=(c == 0),
                                                  stop=(c == nfeat_chunks - 1)),
             reads=[b_sq, G.b_const], writes=(pb if isinstance(pb, list) else [pb]))
    rs, b_rs = L["rstd"]
    k.op(k.act, lambda e: e.activation(out=rs[:], in_=(ps if ps_ap is not None else ps[:]), func=AF.Ln, scale=1.0 / (128 * nfeat_chunks),
                                       bias=L["eps"][:, 0:1]),
         reads=(pb if isinstance(pb, list) else [pb]) + [L["b_eps"]], writes=[b_rs])
    k.op(k.act, lambda e: e.activation(out=rs[:], in_=rs[:], func=AF.Exp, scale=-0.5), reads=[b_rs], writes=[b_rs])
    for c in range(nfeat_chunks):
        k.op(k.dve, lambda e, c=c: e.scalar_tensor_tensor(out=hT[:, c, :], in0=xt[:, c, :], scalar=gain[:, c:c + 1],
                                                          in1=rs[:], op0=ALU.mult, op1=ALU.mult),
             reads=[b_xt, b_rs, L["b_vec"]], writes=[b_hT])


def phase_begin(G):
    snap = G.k.snapshot()
    G.k.barrier(snap)
    return snap


def phase_F1(G, layer, wups, src, dst, sb_src, sb_dst):
    k, nc = G.k, G.nc
    W = G.w[("ffn", layer)]
    with contextlib.ExitStack() as es:
        sb = lambda name, shape, dt: es.enter_context(nc.sbuf_tensor(G.uniq(name), list(shape), dt))
        vec = sb("f_vec", [128, 8 + 96], F32)
        b_vec = Buf("f_vec")
        k.op(k.sp, lambda e: e.dma_start(out=vec[:], in_=W["vec"][:, :]), writes=[b_vec], dma=True)
        eps = sb("f_eps", [128, 1], F32)
        b_eps = Buf("f_eps")
        k.op(k.pool, lambda e: e.memset(eps[:], EPS), writes=[b_eps])
        gain = vec[:, 0:8]
        cw = vec[:, 8:104].rearrange("p (j f) -> p j f", f=24)
        xt = sb("f_xt", [128, 8, T], F32)
        b_xt = Buf("f_xt")
        hTs = [(sb(f"f_hT{i}", [128, 8, T], BF16), Buf(f"f_hT{i}")) for i in range(2)]
        L = dict(sq=[(sb(f"f_sq{i}", [128, T], BF16), Buf(f"f_sq{i}")) for i in range(2)],
                 rstd=(sb("f_rstd", [128, T], F32), Buf("f_rstd")), eps=eps, b_eps=b_eps, b_vec=b_vec)
        abufs = [(sb(f"f_ab{i}", [128, T + 2], F32), Buf(f"f_ab{i}")) for i in range(2)]
        tbufs = [(sb(f"f_t{i}", [128, T], F32), Buf(f"f_t{i}")) for i in range(2)]
        gbufs = [(sb(f"f_g{i}", [128, 4, T], BF16), Buf(f"f_g{i}")) for i in range(2)]
        carry = sb("f_carry", [128, 24, 2], F32)
        b_carry = [Buf(f"f_carry{f}") for f in range(24)]
        k.op(k.pool, lambda e: e.memset(carry[:], 0.0), writes=b_carry)

        thirds = [G.wup0[("ffn", layer)], wups[0], wups[1]]
        k.op(k.sp, lambda e: e.dma_start(out=xt[:], in_=src[:, :, 0:T]), reads=sb_src[0:2], writes=[b_xt], dma=True)
        def emit_norm(idx):
            hT_, b_hT_ = hTs[idx % 2]
            rmsnorm_tile(G, L, xt, b_xt, gain, hT_, b_hT_, ps_i=0)
            if idx + 1 < 3 * NT:
                tn = (idx + 1) % NT
                k.op(k.sp, lambda e, tn=tn: e.dma_start(out=xt[:], in_=src[:, :, tn * T:(tn + 1) * T]),
                     reads=sb_src[2 * tn:2 * tn + 2], writes=[b_xt], dma=True)

        it = 0
        emit_norm(0)
        for fcg in range(3):
          wup = thirds[fcg]
          for ti in range(NT):
            hT, b_hT = hTs[it % 2]
            it += 1
            def fc_chain(f):
                fc = fcg * 8 + f
                pa_i, pu_i = 1 + 2 * (fc % 3), 2 + 2 * (fc % 3)
                pa, pu = G.ps[pa_i], G.ps[pu_i]
                for kc in range(8):
                    k.op(k.pe, lambda e, kc=kc, f=f, pa=pa: e.matmul(
                        pa[:], lhsT=wup.view[:, kc, f * 128:(f + 1) * 128], rhs=hT[:, kc, :],
                        start=(kc == 0), stop=(kc == 7)),
                        reads=[wup.buf(kc, f * 128), b_hT], writes=[G.psb[pa_i]])
                for kc in range(8):
                    k.op(k.pe, lambda e, kc=kc, f=f, pu=pu: e.matmul(
                        pu[:], lhsT=wup.view[:, kc, 1024 + f * 128:1024 + (f + 1) * 128], rhs=hT[:, kc, :],
                        start=(kc == 0), stop=(kc == 7)),
                        reads=[wup.buf(kc, 1024 + f * 128), b_hT], writes=[G.psb[pu_i]])
                ab, b_ab = abufs[fc % 2]
                tb, b_tb = tbufs[fc % 2]
                gb, b_gb = gbufs[(fc // 4) % 2]
                k.op(k.dve, lambda e, fc=fc, ab=ab: e.tensor_copy(out=ab[:, 0:2], in_=carry[:, fc, :]),
                     reads=[b_carry[fc]], writes=[b_ab])
                yield
                k.op(k.act, lambda e, ab=ab, pa=pa: e.activation(out=ab[:, 2:T + 2], in_=pa[:], func=AF.Copy),
                     reads=[G.psb[pa_i]], writes=[b_ab])
                yield
                k.op(k.act, lambda e, fc=fc, tb=tb, pa=pa: e.activation(out=tb[:], in_=pa[:], func=AF.Identity,
                                                                       bias=cw[:, 3, fc:fc + 1], scale=cw[:, 2, fc:fc + 1]),
                     reads=[G.psb[pa_i], b_vec], writes=[b_tb])
                yield
                k.op(k.dve, lambda e, fc=fc, tb=tb, ab=ab: e.scalar_tensor_tensor(
                    out=tb[:], in0=ab[:, 1:T + 1], scalar=cw[:, 1, fc:fc + 1], in1=tb[:], op0=ALU.mult, op1=ALU.add),
                    reads=[b_ab, b_tb, b_vec], writes=[b_tb])
                yield
                k.op(k.dve, lambda e, fc=fc, tb=tb, ab=ab: e.scalar_tensor_tensor(
                    out=tb[:], in0=ab[:, 0:T], scalar=cw[:, 0, fc:fc + 1], in1=tb[:], op0=ALU.mult, op1=ALU.add),
                    reads=[b_ab, b_tb, b_vec], writes=[b_tb])
                yield
                k.op(k.act, lambda e, tb=tb: e.activation(out=tb[:], in_=tb[:], func=AF.Silu), reads=[b_tb], writes=[b_tb])
                yield
                k.op(k.dve, lambda e, fc=fc, tb=tb, gb=gb, pu=pu: e.tensor_tensor(
                    out=gb[:, fc % 4, :], in0=tb[:], in1=pu[:], op=ALU.mult),
                    reads=[b_tb, G.psb[pu_i]], writes=[b_gb])
                yield
                k.op(k.dve, lambda e, fc=fc, ab=ab: e.tensor_copy(out=carry[:, fc, :], in_=ab[:, T:T + 2]),
                     reads=[b_ab], writes=[b_carry[fc]])
                yield
                if fc % 4 == 3:
                    f0 = fc - 3
                    k.op(k.sp, lambda e, f0=f0, gb=gb, ti=ti: e.dma_start(
                        out=G.gT[:, f0:f0 + 4, ti * T:(ti + 1) * T], in_=gb[:]),
                        reads=[b_gb], writes=[G.gT_bufs[ti][fc // 4]], dma=True)

            for f0 in range(0, 8, 2):
                interleave([fc_chain(f0), fc_chain(f0 + 1)])
                if f0 == 2 and it < 3 * NT:
                    emit_norm(it)
          if fcg == 0:
            G.wdn[layer] = WRegion(G, G.regB, k.snapshot(), W["w_dn"], FF, D, blk=1024, name="wdn")


def phase_F2(G, layer, wr, src, dst, sb_src, sb_dst):
    k, nc = G.k, G.nc
    wdn = G.wdn[layer]
    with contextlib.ExitStack() as es:
        sb = lambda name, shape, dt: es.enter_context(nc.sbuf_tensor(G.uniq(name), list(shape), dt))
        xts = [(sb(f"g_xt{i}", [128, 8, T], F32), Buf(f"g_xt{i}")) for i in range(2)]
        gts = [(sb(f"g_gt{i}", [128, 12, T], BF16), Buf(f"g_gt{i}")) for i in range(2)]
        for ti in range(NT):
            xt, b_xt = xts[ti % 2]
            k.op(k.sp, lambda e, ti=ti, xt=xt: e.dma_start(out=xt[:], in_=src[:, :, ti * T:(ti + 1) * T]),
                 reads=sb_src[2 * ti:2 * ti + 2], writes=[b_xt], dma=True)
            for half in range(2):
                gt, b_gt = gts[half]
                k.op(k.act, lambda e, ti=ti, gt=gt, half=half: e.dma_start(
                    out=gt[:], in_=G.gT[:, half * 12:(half + 1) * 12, ti * T:(ti + 1) * T]),
                    reads=G.gT_bufs[ti][half * 3:(half + 1) * 3], writes=[b_gt], dma=True)
                for oc in range(8):
                    for f in range(12):
                        fc = half * 12 + f
                        k.op(k.pe, lambda e, oc=oc, fc=fc, f=f, gt=gt: e.matmul(
                            G.ps[oc][:], lhsT=wdn.view[:, fc, oc * 128:(oc + 1) * 128], rhs=gt[:, f, :],
                            start=(fc == 0), stop=(fc == 23)),
                            reads=[wdn.buf(fc, oc * 128), b_gt], writes=[G.psb[oc]])
            for oc in range(8):
                k.op(k.dve, lambda e, oc=oc, xt=xt: e.tensor_tensor(out=xt[:, oc, :], in0=G.ps[oc][:], in1=xt[:, oc, :],
                                                                   op=ALU.add),
                     reads=[G.psb[oc], b_xt], writes=[b_xt])
            k.op(k.sp, lambda e, ti=ti, xt=xt: e.dma_start(out=dst[:, :, ti * T:(ti + 1) * T], in_=xt[:]),
                 reads=[b_xt], writes=sb_dst[2 * ti:2 * ti + 2], dma=True)


def interleave(gens):
    gens = list(gens)
    while gens:
        for g in list(gens):
            try:
                next(g)
            except StopIteration:
                gens.remove(g)


def carve(reg, off, shape):
    n = int(np.prod(shape[1:]))
    v = reg[:, off:off + n]
    if len(shape) == 3:
        v = v.rearrange("p (a b) -> p a b", b=shape[2])
    return v, off + n


def phase_M1(G, layer, wqkv, src, dst, sb_src, sb_dst):
    k, nc = G.k, G.nc
    W = G.w[("moba", layer)]
    with contextlib.ExitStack() as es:
        sb = lambda name, shape, dt: es.enter_context(nc.sbuf_tensor(G.uniq(name), list(shape), dt))
        vec = sb("m_vec", [128, 10], F32)
        b_vec = Buf("m_vec")
        k.op(k.sp, lambda e: e.dma_start(out=vec[:], in_=W["vec"][:, :]), writes=[b_vec], dma=True)
        qkg = sb("m_qkg", [128, 2], F32)
        k.op(k.dve, lambda e: e.tensor_scalar(out=qkg[:, 0:1], in0=vec[:, 8:9], scalar1=128.0 ** -0.5, scalar2=None,
                                              op0=ALU.mult), reads=[b_vec], writes=[b_vec])
        k.op(k.dve, lambda e: e.tensor_copy(out=qkg[:, 1:2], in_=vec[:, 9:10]), reads=[b_vec], writes=[b_vec])
        eps = sb("m_eps", [128, 1], F32)
        b_eps = Buf("m_eps")
        k.op(k.pool, lambda e: e.memset(eps[:], EPS), writes=[b_eps])
        gain = vec[:, 0:8]
        xt = sb("m_xt", [128, 8, T], F32)
        b_xt = Buf("m_xt")
        cs = sb("m_cs", [128, 2, T], F32)
        b_cs = Buf("m_cs")
        off = 8 * 3 * D
        hTs = []
        for i in range(2):
            v, off = carve(G.regA, off, [128, 8, T])
            hTs.append((v, Buf(f"m_hT{i}")))
        vt, off = carve(G.regA, off, [128, 4, D])
        b_vt = Buf("m_vt")
        rot_bf, off = carve(G.regA, off, [128, 128])
        b_rot = Buf("m_rot")
        k.op(k.pool, lambda e: e.dma_start(out=rot_bf, in_=G.c_rot[:, :]), writes=[b_rot], dma=True)
        two = lambda nm: None
        sqs, sqh, qnb, qfs = [], [], [], []
        NS = 4
        for i in range(2):
            v, off = carve(G.regA, off, [128, T]); sqs.append((v, Buf(f"m_sq{i}")))
        for i in range(NS):
            v, off = carve(G.regA, off, [128, T]); sqh.append((v, Buf(f"m_sqh{i}")))
            v, off = carve(G.regA, off, [128, T]); qnb.append((v, Buf(f"m_qnb{i}")))
            v, off = carve(G.regA, off, [128, T]); qfs.append((v, Buf(f"m_qf{i}")))
        assert off <= 49152
        L = dict(sq=sqs, rstd=(sb("m_rstd", [128, T], F32), Buf("m_rstd")), eps=eps, b_eps=b_eps, b_vec=b_vec)
        r2s = [(sb(f"m_r2{i}", [128, T], F32), Buf(f"m_r2{i}")) for i in range(NS)]
        qns = [(sb(f"m_qn{i}", [128, T], F32), Buf(f"m_qn{i}")) for i in range(NS)]
        t1s = [(sb(f"m_t1{i}", [128, T], F32), Buf(f"m_t1{i}")) for i in range(NS)]
        t2s = [(sb(f"m_t2{i}", [128, T], F32), Buf(f"m_t2{i}")) for i in range(NS)]

        k.op(k.sp, lambda e: e.dma_start(out=xt[:], in_=src[:, :, 0:T]), reads=sb_src[0:2], writes=[b_xt], dma=True)
        cnt = 0
        dbg = "vq"

        def emit_norm(idx):
            hT_, b_hT_ = hTs[idx % 2]
            rmsnorm_tile(G, L, xt, b_xt, gain, hT_, b_hT_, ps_i=0)
            if idx + 1 < NT:
                k.op(k.sp, lambda e: e.dma_start(out=xt[:], in_=src[:, :, (idx + 1) * T:(idx + 2) * T]),
                     reads=sb_src[2 * idx + 2:2 * idx + 4], writes=[b_xt], dma=True)

        emit_norm(0)
        for ti in range(NT):
            hT, b_hT = hTs[ti % 2]
            k.op(k.sp, lambda e, ti=ti: e.dma_start(out=cs[:, 0, :], in_=G.c_cos[:, ti * T:(ti + 1) * T]),
                 writes=[b_cs], dma=True)
            k.op(k.sp, lambda e, ti=ti: e.dma_start(out=cs[:, 1, :], in_=G.c_sin[:, ti * T:(ti + 1) * T]),
                 writes=[b_cs], dma=True)
            for sub in range(4 if "v" in dbg else 0):
                for half in range(2):
                    pi = 1 + (sub * 2 + half) % 2
                    for kc in range(8):
                        k.op(k.pe, lambda e, kc=kc, sub=sub, half=half, pi=pi: e.matmul(
                            G.ps[pi][:], lhsT=hT[:, kc, sub * 128:(sub + 1) * 128],
                            rhs=wqkv.view[:, kc, 2 * D + half * 512:2 * D + (half + 1) * 512],
                            start=(kc == 0), stop=(kc == 7)),
                            reads=[b_hT, wqkv.buf(kc, 2 * D + half * 512)], writes=[G.psb[pi]])
                    k.op(k.act, lambda e, sub=sub, half=half, pi=pi: e.activation(
                        out=vt[:, sub, half * 512:(half + 1) * 512], in_=G.ps[pi][:], func=AF.Copy),
                        reads=[G.psb[pi]], writes=[b_vt])
            if "v" in dbg:
                k.op(k.sp, lambda e, ti=ti: e.dma_start(out=G.vtok[:, ti * 4:(ti + 1) * 4, :], in_=vt),
                     reads=[b_vt], writes=[G.v_bufs[ti]], dma=True)
            def qk_chain(which, h, sl, g):
                col0 = which * D + h * 128
                pi = 1 + (g % 2) * 2 + sl
                pss = 5 if sl == 0 else 0
                pr = 6 + sl
                sl = (g % 2) * 2 + sl
                sq2, b_sq2 = sqh[sl]
                r2, b_r2 = r2s[sl]
                qn, b_qn = qns[sl]
                qb, b_qb = qnb[sl]
                t1, b_t1 = t1s[sl]
                t2, b_t2 = t2s[sl]
                qf, b_qf = qfs[sl]
                for kc in range(8):
                    k.op(k.pe, lambda e: e.matmul(
                        G.ps[pi][:], lhsT=wqkv.view[:, kc, col0:col0 + 128], rhs=hT[:, kc, :],
                        start=(kc == 0), stop=(kc == 7)),
                        reads=[b_hT, wqkv.buf(kc, col0)], writes=[G.psb[pi]])
                yield
                k.op(k.act, lambda e: e.activation(out=sq2, in_=G.ps[pi][:], func=AF.Square),
                     reads=[G.psb[pi]], writes=[b_sq2])
                yield
                k.op(k.pe, lambda e: e.matmul(G.ps[pss][:], lhsT=G.ones_bf[:], rhs=sq2, start=True, stop=True),
                     reads=[b_sq2, G.b_const], writes=[G.psb[pss]])
                yield
                k.op(k.act, lambda e: e.activation(out=r2[:], in_=G.ps[pss][:], func=AF.Ln, bias=eps[:, 0:1],
                                                   scale=1.0 / 128), reads=[G.psb[pss], b_eps], writes=[b_r2])
                yield
                k.op(k.act, lambda e: e.activation(out=r2[:], in_=r2[:], func=AF.Exp, scale=-0.5),
                     reads=[b_r2], writes=[b_r2])
                yield
                k.op(k.dve, lambda e: e.scalar_tensor_tensor(
                    out=qn[:], in0=G.ps[pi][:], scalar=qkg[:, which:which + 1], in1=r2[:], op0=ALU.mult, op1=ALU.mult),
                    reads=[G.psb[pi], b_r2, b_vec], writes=[b_qn])
                yield
                k.op(k.act, lambda e: e.activation(out=qb, in_=qn[:], func=AF.Copy), reads=[b_qn], writes=[b_qb])
                yield
                k.op(k.pe, lambda e: e.matmul(G.ps[pr][:], lhsT=rot_bf, rhs=qb, start=True, stop=True),
                     reads=[b_qb, b_rot], writes=[G.psb[pr]])
                yield
                k.op(k.dve, lambda e: e.tensor_tensor(out=t1[:], in0=qn[:], in1=cs[:, 0, :], op=ALU.mult),
                     reads=[b_qn, b_cs], writes=[b_t1])
                yield
                k.op(k.dve, lambda e: e.tensor_tensor(out=t2[:], in0=G.ps[pr][:], in1=cs[:, 1, :], op=ALU.mult),
                     reads=[G.psb[pr], b_cs], writes=[b_t2])
                yield
                k.op(k.dve, lambda e: e.tensor_tensor(out=qf, in0=t1[:], in1=t2[:], op=ALU.add),
                     reads=[b_t1, b_t2], writes=[b_qf])
                yield
                dT = G.qT if which == 0 else G.kT
                db = G.q_bufs if which == 0 else G.k_bufs
                k.op(k.sp, lambda e: e.dma_start(out=dT[h, :, ti * T:(ti + 1) * T], in_=qf),
                     reads=[b_qf], writes=[db[h][ti]], dma=True)
                yield

            chains = [(w_, h_) for w_ in range(2) for h_ in range(8)]
            for g in range(8):
                interleave([qk_chain(w_, h_, sl, g) for sl, (w_, h_) in enumerate(chains[2 * g:2 * g + 2])])
                if g == 5 and ti + 1 < NT:
                    emit_norm(ti + 1)


def phase_M2(G, layer, wr, src, dst, sb_src, sb_dst):
    k, nc = G.k, G.nc
    with contextlib.ExitStack() as es:
        sb = lambda name, shape, dt: es.enter_context(nc.sbuf_tensor(G.uniq(name), list(shape), dt))
        off = 0
        qT, off = carve(G.regA, off, [128, S]); b_q = Buf("a_q")
        kT, off = carve(G.regA, off, [128, S]); b_k = Buf("a_k")
        vt, off = carve(G.regA, off, [128, 32, 128]); b_v = Buf("a_v")
        oTh, off = carve(G.regA, off, [128, S]); b_o = Buf("a_o")
        biasT, off = carve(G.regA, off, [128, S]); b_bT = Buf("a_bT")
        causal, off = carve(G.regA, off, [128, 512]); b_cz = Buf("a_causal")
        selrow, off = carve(G.regA, off, [128, 2048]); b_sr = Buf("a_selrow")
        km_bf, off = carve(G.regA, off, [128, 16]); b_kmb = Buf("a_kmb")
        PTs = []
        for i in range(4):
            v, off = carve(G.regA, off, [128, 256]); PTs.append((v, Buf(f"a_PT{i}")))
        assert off <= 49152
        past = sb("a_past", [128, 512], F32); b_past = Buf("a_past")
        km = sb("a_km", [128, 16], F32); b_km = Buf("a_km")
        gm = sb("a_gm", [128, 512], F32); b_gm = Buf("a_gm")
        top8 = sb("a_top8", [128, 32, 8], F32); b_top8 = Buf("a_top8")
        thr = sb("a_thr", [128, 32], F32); b_thr = Buf("a_thr")
        sel = sb("a_sel", [128, 512], F32); b_sel = Buf("a_sel")
        rdens = [(sb(f"a_rden{i}", [128, 256], F32), Buf(f"a_rden{i}")) for i in range(2)]
        k.op(k.sp, lambda e: e.dma_start(out=past[:], in_=G.c_past[:, :]), writes=[b_past], dma=True)
        k.op(k.pool, lambda e: e.dma_start(out=causal, in_=G.c_causal[:, :]), writes=[b_cz], dma=True)
        k.op(k.pool, lambda e: e.dma_start(out=selrow, in_=G.c_selrow[:, :]), writes=[b_sr], dma=True)
        k.op(k.pool, lambda e: e.memset(biasT, 0.0), writes=[b_bT])
        scnt = 0
        for h in range(8):
            k.op(k.sp, lambda e, h=h: e.dma_start(out=qT, in_=G.qT[h, :, :]), reads=G.q_bufs[h], writes=[b_q], dma=True)
            k.op(k.sp, lambda e, h=h: e.dma_start(out=kT, in_=G.kT[h, :, :]), reads=G.k_bufs[h], writes=[b_k], dma=True)
            k.op(k.sp, lambda e, h=h: e.dma_start(out=vt, in_=G.vtok[:, :, h * 128:(h + 1) * 128]),
                 reads=G.v_bufs, writes=[b_v], dma=True)
            k.op(k.dve, lambda e: e.tensor_reduce(out=km[:], in_=kT.rearrange("p (n j) -> p n j", j=256), axis=AX.X,
                                                  op=ALU.add), reads=[b_k], writes=[b_km])
            k.op(k.dve, lambda e: e.tensor_scalar(out=km_bf, in0=km[:], scalar1=1.0 / 256, scalar2=None, op0=ALU.mult),
                 reads=[b_km], writes=[b_kmb])
            for i in range(32):
                k.op(k.pe, lambda e, i=i: e.matmul(G.ps[0][:, i * 16:(i + 1) * 16], lhsT=qT[:, i * 128:(i + 1) * 128],
                                                   rhs=km_bf, start=True, stop=True),
                     reads=[b_q, b_kmb], writes=[G.psb[0]])
            k.op(k.dve, lambda e: e.tensor_tensor(out=gm[:], in0=G.ps[0][:], in1=past[:], op=ALU.add),
                 reads=[G.psb[0], b_past], writes=[b_gm])
            for i in range(32):
                k.op(k.dve, lambda e, i=i: e.max(out=top8[:, i, :], in_=gm[:, i * 16:(i + 1) * 16]),
                     reads=[b_gm], writes=[b_top8])
            k.op(k.dve, lambda e: e.tensor_scalar(out=thr[:], in0=top8[:, :, 2], scalar1=-1e29, scalar2=None, op0=ALU.max),
                 reads=[b_top8], writes=[b_thr])
            k.op(k.dve, lambda e: e.tensor_tensor(
                out=sel[:].rearrange("p (i n) -> p i n", n=16), in0=gm[:].rearrange("p (i n) -> p i n", n=16),
                in1=thr[:].unsqueeze(2).to_broadcast([128, 32, 16]), op=ALU.is_ge),
                reads=[b_gm, b_thr], writes=[b_sel])
            k.op(k.dve, lambda e: e.tensor_scalar(out=sel[:], in0=sel[:], scalar1=-1.0, scalar2=-NEG, op0=ALU.add,
                                                  op1=ALU.mult), reads=[b_sel], writes=[b_sel])
            for g in range(8):
                pi = g % 2
                for i4 in range(4):
                    i = g * 4 + i4
                    k.op(k.pe, lambda e, i=i, i4=i4, pi=pi: e.transpose(
                        out=G.ps[pi][0:16, i4 * 128:(i4 + 1) * 128], in_=sel[:, i * 16:(i + 1) * 16], identity=G.ident_f[:]),
                        reads=[b_sel, G.b_const], writes=[G.psb[pi]])
                k.op(k.act, lambda e, g=g, pi=pi: e.activation(out=biasT[0:16, g * 512:(g + 1) * 512],
                                                                in_=G.ps[pi][0:16, :], func=AF.Copy),
                     reads=[G.psb[pi]], writes=[b_bT])
            items = [(j, n, kt) for j in range(16) for n in range(j + 1) for kt in range(2)]
            LA = 3

            def s_stage(ii):
                j, n, kt = items[ii]
                qs = slice(j * 256, (j + 1) * 256)
                kc0 = n * 256 + kt * 128
                pi = 2 + ii % 4
                k.op(k.pe, lambda e: e.matmul(
                    G.ps[pi][:, 0:256], lhsT=kT[:, kc0:kc0 + 128], rhs=qT[:, qs], start=True, stop=False),
                    reads=[b_k, b_q], writes=[G.psb[pi]])
                if n < j:
                    k.op(k.pe, lambda e: e.matmul(
                        G.ps[pi][:, 0:256], lhsT=selrow[:, n * 128:(n + 1) * 128], rhs=biasT[:, qs],
                        start=False, stop=True), reads=[b_sr, b_bT], writes=[G.psb[pi]])
                else:
                    k.op(k.pe, lambda e: e.matmul(
                        G.ps[pi][:, 0:256], lhsT=G.ident_bf[:], rhs=causal[:, kt * 256:(kt + 1) * 256],
                        start=False, stop=True), reads=[b_cz, G.b_const], writes=[G.psb[pi]])

            def p_stage(ii):
                j, n, kt = items[ii]
                qs = slice(j * 256, (j + 1) * 256)
                po, pd = (6, 7) if j % 2 == 0 else (0, 1)
                pi = 2 + ii % 4
                PT, b_PT = PTs[ii % 4]
                first = (n == 0 and kt == 0)
                last = (n == j and kt == 1)
                k.op(k.act, lambda e: e.activation(out=PT, in_=G.ps[pi][:, 0:256], func=AF.Exp),
                     reads=[G.psb[pi]], writes=[b_PT])
                k.op(k.pe, lambda e: e.matmul(
                    G.ps[po][:, 0:256], lhsT=vt[:, n * 2 + kt, :], rhs=PT, start=first, stop=last),
                    reads=[b_v, b_PT], writes=[G.psb[po]])
                k.op(k.pe, lambda e: e.matmul(
                    G.ps[pd][:, 0:256], lhsT=G.ones_bf[:], rhs=PT, start=first, stop=last),
                    reads=[G.b_const, b_PT], writes=[G.psb[pd]])
                if last:
                    rd, b_rd = rdens[j % 2]
                    k.op(k.dve, lambda e: e.reciprocal(out=rd[:], in_=G.ps[pd][:, 0:256]),
                         reads=[G.psb[pd]], writes=[b_rd])
                    k.op(k.dve, lambda e: e.tensor_tensor(out=oTh[:, qs], in0=G.ps[po][:, 0:256], in1=rd[:], op=ALU.mult),
                         reads=[G.psb[po], b_rd], writes=[b_o])

            for ii in range(len(items) + LA):
                if ii < len(items):
                    s_stage(ii)
                if ii >= LA:
                    p_stage(ii - LA)
            k.op(k.sp, lambda e, h=h: e.dma_start(out=G.oT[:, h, :], in_=oTh), reads=[b_o], writes=[G.o_bufs[h]], dma=True)


def phase_M3(G, layer, wr, src, dst, sb_src, sb_dst):
    k, nc = G.k, G.nc
    W = G.w[("moba", layer)]
    with contextlib.ExitStack() as es:
        sb = lambda name, shape, dt: es.enter_context(nc.sbuf_tensor(G.uniq(name), list(shape), dt))
        regC = sb("regC", [128, 8 * D], BF16)
        wout = WRegion(G, regC, None, W["w_out"], D, D, blk=1024, name="wout")
        xts = [(sb(f"o_xt{i}", [128, 8, T], F32), Buf(f"o_xt{i}")) for i in range(1)]
        ots = [(sb(f"o_ot{i}", [128, 8, T], BF16), Buf(f"o_ot{i}")) for i in range(2)]
        for ti in range(NT):
            xt, b_xt = xts[0]
            ot, b_ot = ots[ti % 2]
            k.op(k.sp, lambda e, ti=ti, xt=xt: e.dma_start(out=xt[:], in_=src[:, :, ti * T:(ti + 1) * T]),
                 reads=sb_src[2 * ti:2 * ti + 2], writes=[b_xt], dma=True)
            k.op(k.act, lambda e, ti=ti, ot=ot: e.dma_start(out=ot[:], in_=G.oT[:, :, ti * T:(ti + 1) * T]),
                 reads=G.o_bufs, writes=[b_ot], dma=True)
            for oc in range(8):
                for h in range(8):
                    k.op(k.pe, lambda e, oc=oc, h=h, ot=ot: e.matmul(
                        G.ps[oc][:], lhsT=wout.view[:, h, oc * 128:(oc + 1) * 128], rhs=ot[:, h, :],
                        start=(h == 0), stop=(h == 7)),
                        reads=[wout.buf(h, oc * 128), b_ot], writes=[G.psb[oc]])
                k.op(k.dve, lambda e, oc=oc, xt=xt: e.tensor_tensor(out=xt[:, oc, :], in0=G.ps[oc][:], in1=xt[:, oc, :],
                                                                   op=ALU.add),
                     reads=[G.psb[oc], b_xt], writes=[b_xt])
            k.op(k.sp, lambda e, ti=ti, xt=xt: e.dma_start(out=dst[:, :, ti * T:(ti + 1) * T], in_=xt[:]),
                 reads=[b_xt], writes=sb_dst[2 * ti:2 * ti + 2], dma=True)


TH = 256
NTH = S // TH


def phase_H1(G, layer, win, src, dst, sb_src, sb_dst):
    k, nc = G.k, G.nc
    W = G.w[("hgrn", layer)]
    slot = layer // 2
    with contextlib.ExitStack() as es:
        sb = lambda name, shape, dt: es.enter_context(nc.sbuf_tensor(G.uniq(name), list(shape), dt))
        regC = sb("regC", [128, 8 * D], BF16)
        wout = WRegion(G, regC, None, W["w_out"], D, D, blk=1024, name="hwout")
        vec = sb("h_vec", [128, 25], F32); b_vec = Buf("h_vec")
        k.op(k.sp, lambda e: e.dma_start(out=vec[:], in_=W["vec"][:, :]), writes=[b_vec], dma=True)
        gain = vec[:, 0:8]
        ogain = vec[:, 24:25]
        cst = sb("h_cst", [128, 2], F32); b_cst = Buf("h_cst")
        k.op(k.pool, lambda e: e.memset(cst[:, 0:1], EPS), writes=[b_cst])
        k.op(k.pool, lambda e: e.memset(cst[:, 1:2], 1.0), writes=[b_cst])
        eps = cst
        lbv = sb("h_lb", [128, 3, 8], F32); b_lb = Buf("h_lb")
        if slot == 0:
            k.op(k.pool, lambda e: e.memset(lbv[:, 0, :], 0.0), writes=[b_lb])
        else:
            k.op(k.dve, lambda e: e.tensor_tensor(out=lbv[:, 0, :], in0=vec[:, 8:16], in1=vec[:, 16:24], op=ALU.subtract),
                 reads=[b_vec], writes=[b_lb])
            k.op(k.act, lambda e: e.activation(out=lbv[:, 0, :], in_=lbv[:, 0, :], func=AF.Exp), reads=[b_lb], writes=[b_lb])
            k.op(k.dve, lambda e: e.tensor_scalar(out=lbv[:, 0, :], in0=lbv[:, 0, :], scalar1=1.0, scalar2=None, op0=ALU.add),
                 reads=[b_lb], writes=[b_lb])
            k.op(k.dve, lambda e: e.reciprocal(out=lbv[:, 0, :], in_=lbv[:, 0, :]), reads=[b_lb], writes=[b_lb])
        k.op(k.dve, lambda e: e.tensor_scalar(out=lbv[:, 1, :], in0=lbv[:, 0, :], scalar1=-1.0, scalar2=1.0, op0=ALU.mult,
                                              op1=ALU.add), reads=[b_lb], writes=[b_lb])
        k.op(k.dve, lambda e: e.tensor_scalar(out=lbv[:, 2, :], in0=lbv[:, 1, :], scalar1=-1.0, scalar2=None, op0=ALU.mult),
             reads=[b_lb], writes=[b_lb])
        tri = sb("h_tri", [128, 128], F32); b_tri = Buf("h_tri")
        k.op(k.sp, lambda e: e.dma_start(out=tri[:], in_=G.c_tri[:, :]), writes=[b_tri], dma=True)
        smask = sb("h_smask", [128, TH], F32); b_sm = Buf("h_smask")
        k.op(k.sp, lambda e: e.dma_start(out=smask[:], in_=G.c_scan[:, 0:TH]), writes=[b_sm], dma=True)
        xt = sb("h_xt", [128, 8, TH], F32); b_xt = Buf("h_xt")
        xo = sb("h_xo", [128, 8, TH], F32); b_xo = Buf("h_xo")
        St = sb("h_S", [128, 8, 128], F32); b_S = [Buf(f"h_S{h}") for h in range(8)]
        k.op(k.pool, lambda e: e.memset(St[:], 0.0), writes=b_S)
        rstd = (sb("h_rstd", [128, TH], F32), Buf("h_rstd"))
        sets = []
        for i in range(2):
            d_ = dict(
                b=[(sb(f"h_b{i}_{j}", [128, TH], F32), Buf(f"h_b{i}_{j}")) for j in range(4)],
                qin=(sb(f"h_qin{i}", [128, TH], F32), Buf(f"h_qin{i}")),
                egl=(sb(f"h_egl{i}", [128, 4], F32), Buf(f"h_egl{i}")),
                r2=(sb(f"h_r2{i}", [128, TH], F32), Buf(f"h_r2{i}")),
                tmp=(sb(f"h_tmp{i}", [128, TH], F32), Buf(f"h_tmp{i}")),
            )
            sets.append(d_)
        off = 8 * 4 * D
        hTs = []
        for i in range(2):
            v, off = carve(G.regA, off, [128, 8, TH]); hTs.append((v, Buf(f"h_hT{i}")))
        vtok, off = carve(G.regA, off, [128, 2, D]); b_vtok = Buf("h_vtok")
        sgate, off = carve(G.regA, off, [128, 8, TH]); b_sg = [Buf(f"h_sg{h}") for h in range(8)]
        oTn, off = carve(G.regA, off, [128, 8, TH]); b_oTn = [Buf(f"h_oTn{h}") for h in range(8)]
        sqs = []
        for i in range(2):
            v, off = carve(G.regA, off, [128, TH]); sqs.append((v, Buf(f"h_sq{i}")))
        for i in range(2):
            d_ = sets[i]
            v, off = carve(G.regA, off, [128, TH]); d_["qrel"] = (v, Buf(f"h_qrel{i}"))
            v, off = carve(G.regA, off, [128, TH]); d_["krel"] = (v, Buf(f"h_krel{i}"))
            v, off = carve(G.regA, off, [128, 2, 128]); d_["attT"] = (v, Buf(f"h_attT{i}"))
            v, off = carve(G.regA, off, [128, 4, 128]); d_["kz"] = (v, Buf(f"h_kz{i}"))
            v, off = carve(G.regA, off, [128, TH]); d_["sqo"] = (v, Buf(f"h_sqo{i}"))
            k.op(k.pool, lambda e, v=d_["kz"][0]: e.memset(v, 0.0), writes=[d_["kz"][1]])
        assert off <= 49152, off
        L = dict(sq=sqs, rstd=rstd, eps=eps, b_eps=b_cst, b_vec=b_vec)
        pA = [Buf(f"h_pA{b}") for b in range(8)]
        pB = [Buf(f"h_pB{b}") for b in range(8)]
        lo = lambda b: G.ps[b][:, 0:TH]
        hi = lambda b: G.ps[b][:, TH:2 * TH]
        b_dS = [[Buf(f"h_dS{i}_{c}") for c in range(4)] for i in range(2)]
        dS_ps = lambda i, c: G.ps[7 - i][:, c * 128:(c + 1) * 128]
        bank = lambda b: [pA[b], pB[b]] if b < 6 else b_dS[7 - b]
        SCL = 128.0 ** -0.5

        k.op(k.sp, lambda e: e.dma_start(out=xt[:], in_=src[:, :, 0:TH]), reads=[sb_src[0]], writes=[b_xt], dma=True)

        def emit_norm(idx):
            hT_, b_hT_ = hTs[idx % 2]
            rmsnorm_tile(G, L, xt, b_xt, gain, hT_, b_hT_, ps_i=0, ps_ap=lo(0), ps_buf=bank(0))
            if idx + 1 < NTH:
                k.op(k.sp, lambda e: e.dma_start(out=xt[:], in_=src[:, :, (idx + 1) * TH:(idx + 2) * TH]),
                     reads=[sb_src[idx + 1]], writes=[b_xt], dma=True)

        emit_norm(0)
        for ti in range(NTH):
            c0 = ti * TH
            hT, b_hT = hTs[ti % 2]
            k.op(k.sp, lambda e, c0=c0: e.dma_start(out=xo[:], in_=src[:, :, c0:c0 + TH]), reads=[sb_src[ti]],
                 writes=[b_xo], dma=True)
            for sub in range(2):
                for half in range(2):
                    pi = 1 + (sub * 2 + half) % 2
                    for kc in range(8):
                        k.op(k.pe, lambda e, kc=kc, sub=sub, half=half, pi=pi: e.matmul(
                            G.ps[pi][:], lhsT=hT[:, kc, sub * 128:(sub + 1) * 128],
                            rhs=win.view[:, kc, 2 * D + half * 512:2 * D + (half + 1) * 512],
                            start=(kc == 0), stop=(kc == 7)),
                            reads=[b_hT, win.buf(kc, 2 * D + half * 512)], writes=bank(pi))
                    k.op(k.act, lambda e, sub=sub, half=half, pi=pi: e.activation(
                        out=vtok[:, sub, half * 512:(half + 1) * 512], in_=G.ps[pi][:], func=AF.Copy),
                        reads=[pA[pi], pB[pi]], writes=[b_vtok])
            for h in range(8):
                pi = 3 + h % 2
                for kc in range(8):
                    k.op(k.pe, lambda e, kc=kc, h=h, pi=pi: e.matmul(
                        lo(pi), lhsT=win.view[:, kc, 3 * D + h * 128:3 * D + (h + 1) * 128], rhs=hT[:, kc, :],
                        start=(kc == 0), stop=(kc == 7)),
                        reads=[b_hT, win.buf(kc, 3 * D + h * 128)], writes=bank(pi))
                k.op(k.act, lambda e, h=h, pi=pi: e.activation(out=sgate[:, h, :], in_=lo(pi), func=AF.Silu),
                     reads=[pA[pi]], writes=[b_sg[h]])
            for hp in range(4):
                hh = (2 * hp, 2 * hp + 1)
                def head_elem(i, h):
                    st = sets[i]
                    pq, pz = 0 + i, 2 + i
                    (b1, B1), (b2, B2), (b3, B3), (b4, B4) = st["b"]
                    for kc in range(8):
                        k.op(k.pe, lambda e, kc=kc, h=h, pq=pq: e.matmul(
                            lo(pq), lhsT=win.view[:, kc, h * 128:(h + 1) * 128], rhs=hT[:, kc, :],
                            start=(kc == 0), stop=(kc == 7)), reads=[b_hT, win.buf(kc, h * 128)], writes=bank(pq))
                    for kc in range(8):
                        k.op(k.pe, lambda e, kc=kc, h=h, pz=pz: e.matmul(
                            lo(pz), lhsT=win.view[:, kc, D + h * 128:D + (h + 1) * 128], rhs=hT[:, kc, :],
                            start=(kc == 0), stop=(kc == 7)), reads=[b_hT, win.buf(kc, D + h * 128)], writes=bank(pz))
                    k.op(k.act, lambda e, b1=b1, pz=pz: e.activation(out=b1[:], in_=lo(pz), func=AF.Exp, scale=-1.0),
                         reads=[pA[pz]], writes=[B1])
                    yield
                    k.op(k.dve, lambda e, b1=b1: e.tensor_scalar(out=b1[:], in0=b1[:], scalar1=1.0, scalar2=None, op0=ALU.add),
                         reads=[B1], writes=[B1])
                    yield
                    k.op(k.dve, lambda e, b1=b1: e.reciprocal(out=b1[:], in_=b1[:]), reads=[B1], writes=[B1])
                    yield
                    k.op(k.act, lambda e, b1=b1, b2=b2, h=h: e.activation(out=b2[:], in_=b1[:], func=AF.Ln, bias=lbv[:, 0, h:h + 1],
                                                                        scale=lbv[:, 1, h:h + 1]),
                         reads=[B1, b_lb], writes=[B2])
                    yield
                    k.op(k.dve, lambda e, b1=b1, h=h: e.tensor_scalar(out=b1[:], in0=b1[:], scalar1=lbv[:, 2, h:h + 1],
                                                                     scalar2=lbv[:, 1, h:h + 1], op0=ALU.mult, op1=ALU.add),
                         reads=[B1, b_lb], writes=[B1])
                    yield
                    k.op(k.dve, lambda e, b2=b2, b3=b3: e.tensor_tensor_scan(out=b3[:], data0=smask[:], data1=b2[:], initial=0.0,
                                                                            op0=ALU.mult, op1=ALU.add),
                         reads=[B2, b_sm], writes=[B3])
                    yield
                    G3 = b3[:].rearrange("p (c t) -> p c t", t=64)
                    k.op(k.dve, lambda e, b2=b2, G3=G3: e.tensor_tensor(
                        out=b2[:].rearrange("p (c t) -> p c t", t=64), in0=G3, in1=G3[:, :, 31:32].to_broadcast([128, 4, 64]),
                        op=ALU.subtract), reads=[B3], writes=[B2])
                    yield
                    k.op(k.act, lambda e, b2=b2, b4=b4: e.activation(out=b4[:], in_=b2[:], func=AF.Exp), reads=[B2], writes=[B4])
                    yield
                    qrel, Bqrel = st["qrel"]
                    k.op(k.dve, lambda e, qrel=qrel, b4=b4, pq=pq: e.scalar_tensor_tensor(
                        out=qrel, in0=lo(pq), scalar=SCL, in1=b4[:], op0=ALU.mult, op1=ALU.mult),
                        reads=[pA[pq], B4], writes=[Bqrel])
                    yield
                    k.op(k.act, lambda e, b2=b2: e.activation(out=b2[:], in_=b2[:], func=AF.Exp, scale=-1.0), reads=[B2], writes=[B2])
                    yield
                    krel, Bkrel = st["krel"]
                    k.op(k.dve, lambda e, krel=krel, b1=b1, b2=b2: e.tensor_tensor(out=krel, in0=b1[:], in1=b2[:], op=ALU.mult),
                         reads=[B1, B2], writes=[Bkrel])
                    yield
                    k.op(k.act, lambda e, b3=b3, b4=b4: e.activation(out=b4[:], in_=b3[:], func=AF.Exp), reads=[B3], writes=[B4])
                    yield
                    qin, Bqin = st["qin"]
                    k.op(k.dve, lambda e, qin=qin, b4=b4, pq=pq: e.scalar_tensor_tensor(
                        out=qin[:], in0=lo(pq), scalar=SCL, in1=b4[:], op0=ALU.mult, op1=ALU.mult),
                        reads=[pA[pq], B4], writes=[Bqin])
                    yield
                    k.op(k.dve, lambda e, b2=b2, G3=G3: e.tensor_tensor(
                        out=b2[:].rearrange("p (c t) -> p c t", t=64), in0=G3, in1=G3[:, :, 63:64].to_broadcast([128, 4, 64]),
                        op=ALU.subtract), reads=[B3], writes=[B2])
                    yield
                    k.op(k.act, lambda e, b2=b2: e.activation(out=b2[:], in_=b2[:], func=AF.Exp, scale=-1.0), reads=[B2], writes=[B2])
                    yield
                    k.op(k.dve, lambda e, b1=b1, b2=b2, b4=b4: e.tensor_tensor(out=b4[:], in0=b1[:], in1=b2[:], op=ALU.mult),
                         reads=[B1, B2], writes=[B4])
                    yield
                    egl, Begl = st["egl"]
                    k.op(k.act, lambda e, egl=egl, G3=G3: e.activation(out=egl[:], in_=G3[:, :, 63], func=AF.Exp),
                         reads=[B3], writes=[Begl])
                    yield
                    kTp = lo(5) if i == 0 else hi(5)
                    BkT = pA[5] if i == 0 else pB[5]
                    for pr in range(2):
                        k.op(k.pe, lambda e, pr=pr, b4=b4, kTp=kTp: e.transpose(
                            out=kTp[:, pr * 128:(pr + 1) * 128], in_=b4[:, pr * 128:(pr + 1) * 128],
                            identity=G.ident_f[:]), reads=[B4, G.b_const], writes=bank(5))
                    kz, Bkz = st["kz"]
                    kzv = kz.rearrange("p (pr cc) d -> p pr cc d", cc=2)
                    k.op(k.act, lambda e, kzv=kzv, kTp=kTp: e.activation(
                        out=kzv[0:64, :, 0, :], in_=kTp[0:64, :].rearrange("p (pr d) -> p pr d", d=128),
                        func=AF.Copy), reads=[BkT], writes=[Bkz])
                    yield
                    k.op(k.act, lambda e, kzv=kzv, kTp=kTp: e.activation(
                        out=kzv[64:128, :, 1, :], in_=kTp[64:128, :].rearrange("p (pr d) -> p pr d", d=128),
                        func=AF.Copy), reads=[BkT], writes=[Bkz])
                    yield
                    aTp = lo(4) if i == 0 else hi(4)
                    BaT = pA[4] if i == 0 else pB[4]
                    for pr in range(2):
                        k.op(k.pe, lambda e, pr=pr, aTp=aTp, krel=krel, qrel=qrel: e.matmul(
                            aTp[:, pr * 128:(pr + 1) * 128], lhsT=krel[:, pr * 128:(pr + 1) * 128],
                            rhs=qrel[:, pr * 128:(pr + 1) * 128], start=True, stop=True),
                            reads=[Bkrel, Bqrel], writes=bank(4))
                    attT, BattT = st["attT"]
                    k.op(k.dve, lambda e, attT=attT, aTp=aTp: e.tensor_tensor(
                        out=attT, in0=aTp.rearrange("p (pr t) -> p pr t", t=128),
                        in1=tri[:].unsqueeze(1).to_broadcast([128, 2, 128]), op=ALU.mult),
                        reads=[BaT, b_tri], writes=[BattT])
                    yield
                    for c in range(4):
                        k.op(k.pe, lambda e, c=c, i=i, kz=kz, h=h: e.matmul(
                            dS_ps(i, c), lhsT=kz[:, c, :], rhs=vtok[:, c // 2, h * 128:(h + 1) * 128], start=True, stop=True),
                            reads=[Bkz, b_vtok], writes=bank(7 - i))

                interleave([head_elem(i_, h_) for i_, h_ in enumerate(hh)])
                for c in range(4):
                    for i, h in enumerate(hh):
                        st = sets[i]
                        qin, Bqin = st["qin"]
                        attT, BattT = st["attT"]
                        egl, Begl = st["egl"]
                        pr, cc = c // 2, c % 2
                        k.op(k.pe, lambda e, c=c, i=i, h=h, qin=qin: e.matmul(
                            hi(i)[:, c * 64:(c + 1) * 64], lhsT=St[:, h, :], rhs=qin[:, c * 64:(c + 1) * 64],
                            start=True, stop=False), reads=[b_S[h], Bqin], writes=bank(i))
                        k.op(k.pe, lambda e, c=c, i=i, h=h, attT=attT, pr=pr, cc=cc: e.matmul(
                            hi(i)[:, c * 64:(c + 1) * 64], lhsT=vtok[:, pr, h * 128:(h + 1) * 128],
                            rhs=attT[:, pr, cc * 64:(cc + 1) * 64], start=False, stop=True),
                            reads=[b_vtok, BattT], writes=bank(i))
                        k.op(k.dve, lambda e, c=c, i=i, h=h, egl=egl: e.scalar_tensor_tensor(
                            out=St[:, h, :], in0=St[:, h, :], scalar=egl[:, c:c + 1], in1=dS_ps(i, c),
                            op0=ALU.mult, op1=ALU.add),
                            reads=[b_S[h], Begl, b_dS[i][c]], writes=[b_S[h]])
                def head_norm(i, h):
                    st = sets[i]
                    sqo, Bsqo = st["sqo"]
                    r2, Br2 = st["r2"]
                    tmp, Btmp = st["tmp"]
                    k.op(k.act, lambda e, sqo=sqo, i=i: e.activation(out=sqo, in_=hi(i), func=AF.Square),
                         reads=[pB[i]], writes=[Bsqo])
                    yield
                    k.op(k.pe, lambda e, sqo=sqo, i=i: e.matmul(hi(2 + i), lhsT=G.ones_bf[:], rhs=sqo, start=True, stop=True),
                         reads=[Bsqo, G.b_const], writes=bank(2 + i))
                    yield
                    k.op(k.act, lambda e, r2=r2, i=i: e.activation(out=r2[:], in_=hi(2 + i), func=AF.Ln,
                                                                   bias=cst[:, 0:1], scale=1.0 / 128),
                         reads=[pB[2 + i], b_cst], writes=[Br2])
                    yield
                    k.op(k.act, lambda e, r2=r2: e.activation(out=r2[:], in_=r2[:], func=AF.Exp, scale=-0.5), reads=[Br2], writes=[Br2])
                    yield
                    k.op(k.dve, lambda e, tmp=tmp, r2=r2, i=i: e.scalar_tensor_tensor(
                        out=tmp[:], in0=hi(i), scalar=ogain, in1=r2[:], op0=ALU.mult, op1=ALU.mult),
                        reads=[pB[i], Br2, b_vec], writes=[Btmp])
                    yield
                    k.op(k.dve, lambda e, tmp=tmp, h=h: e.tensor_tensor(out=oTn[:, h, :], in0=tmp[:], in1=sgate[:, h, :], op=ALU.mult),
                         reads=[Btmp, b_sg[h]], writes=[b_oTn[h]])
                    yield

                interleave([head_norm(i_, h_) for i_, h_ in enumerate(hh)])
                if hp == 2 and ti + 1 < NTH:
                    emit_norm(ti + 1)
            for oc in range(8):
                pi = oc % 2
                for h in range(8):
                    k.op(k.pe, lambda e, oc=oc, h=h, pi=pi: e.matmul(
                        lo(pi), lhsT=wout.view[:, h, oc * 128:(oc + 1) * 128], rhs=oTn[:, h, :],
                        start=(h == 0), stop=(h == 7)), reads=[wout.buf(h, oc * 128), b_oTn[h]], writes=bank(pi))
                k.op(k.dve, lambda e, oc=oc, pi=pi: e.tensor_tensor(out=xo[:, oc, :], in0=lo(pi), in1=xo[:, oc, :],
                                                                   op=ALU.add), reads=[pA[pi], b_xo], writes=[b_xo])
            k.op(k.sp, lambda e, c0=c0: e.dma_start(out=dst[:, :, c0:c0 + TH], in_=xo[:]), reads=[b_xo], writes=[sb_dst[ti]],
                 dma=True)


PHASE_FN = {"F1": phase_F1, "F2": phase_F2, "M1": phase_M1, "M2": phase_M2, "M3": phase_M3, "H1": phase_H1}


def fm(v, n):
    return np.ascontiguousarray(np.asarray(v, np.float32).reshape(n, 128).T)


def host_consts(kinds):
    c = {"c_ones": np.ones((128, 128), np.float32), "c_ident": np.eye(128, dtype=np.float32)}
    if "moba" in kinds:
        rot = np.zeros((128, 128), np.float32)
        for m_ in range(64):
            rot[m_ + 64, m_] = -1.0
            rot[m_, m_ + 64] = 1.0
        c["c_rot"] = rot
        inv = (1.0 / (np.float32(10000.0) ** (np.arange(0, 128, 2, dtype=np.float32) / np.float32(128)))).astype(np.float32)
        ang = (np.arange(S, dtype=np.float32)[:, None] * inv[None, :]).astype(np.float32)
        ang = np.concatenate([ang, ang], axis=-1)
        c["c_cos"] = np.ascontiguousarray(np.cos(ang).astype(np.float32).T)
        c["c_sin"] = np.ascontiguousarray(np.sin(ang).astype(np.float32).T)
        past = np.full((32, 16), -1e30, np.float32)
        for i in range(32):
            past[i, :i // 2] = 0.0
        c["c_past"] = np.ascontiguousarray(np.broadcast_to(past.reshape(1, 512), (128, 512)))
        causal = np.full((128, 2, 256), NEG, np.float32)
        for kt in range(2):
            for p in range(128):
                causal[p, kt, kt * 128 + p:] = 0.0
        c["c_causal"] = causal.reshape(128, 512)
        selrow = np.zeros((128, 16, 128), np.float32)
        for n_ in range(16):
            selrow[n_, n_, :] = 1.0
        c["c_selrow"] = selrow.reshape(128, 2048)
    if "hgrn" in kinds:
        tri = np.zeros((128, 128), np.float32)
        for s_ in range(128):
            for t_ in range(128):
                if s_ // 64 == t_ // 64 and s_ <= t_:
                    tri[s_, t_] = 1.0
        c["c_tri"] = tri
        sm = np.ones((128, T), np.float32)
        sm[:, ::64] = 0.0
        c["c_scan"] = sm
    return c


def stage_inputs(inputs, stages):
    m = {}
    for kind, l in stages:
        if kind == "ffn":
            m[f"ffn_w_up_{l}"] = np.ascontiguousarray(inputs["ffn_w_up"][l])
            m[f"ffn_w_dn_{l}"] = np.ascontiguousarray(inputs["ffn_w_down"][l])
            cw = inputs["ffn_conv_w"][l]
            m[f"ffn_vec_{l}"] = np.ascontiguousarray(np.concatenate(
                [fm(inputs["ffn_norm"][l], 8), fm(cw[0], 24), fm(cw[1], 24), fm(cw[2], 24),
                 fm(inputs["ffn_conv_b"][l], 24)], axis=1))
        elif kind == "hgrn":
            sl = l // 2
            m[f"hgrn_w_in_{l}"] = np.ascontiguousarray(inputs["hgrn_w_in"][sl])
            m[f"hgrn_w_out_{l}"] = np.ascontiguousarray(inputs["hgrn_w_out"][sl])
            m[f"hgrn_vec_{l}"] = np.ascontiguousarray(np.concatenate(
                [fm(inputs["attn_norm"][l], 8), fm(inputs["hgrn_lb"][0], 8), fm(inputs["hgrn_lb"][1], 8),
                 fm(inputs["hgrn_out_norm"][sl], 1)], axis=1))
        elif kind == "moba":
            sl = l // 2
            m[f"moba_w_qkv_{l}"] = np.ascontiguousarray(inputs["moba_w_qkv"][sl])
            m[f"moba_w_out_{l}"] = np.ascontiguousarray(inputs["moba_w_out"][sl])
            m[f"moba_vec_{l}"] = np.ascontiguousarray(np.concatenate(
                [fm(inputs["attn_norm"][l], 8), fm(inputs["moba_q_norm"][sl], 1), fm(inputs["moba_k_norm"][sl], 1)], axis=1))
    return m


def x_to_dev(xb):
    return np.ascontiguousarray(xb.T.reshape(8, 128, S).transpose(1, 0, 2))


def x_from_dev(y):
    return np.ascontiguousarray(y.transpose(1, 0, 2).reshape(D, S).T)


FUSED = True
LAYER_STAGES = [[("hgrn", 0), ("ffn", 0)], [("moba", 1), ("ffn", 1)], [("hgrn", 2), ("ffn", 2)], [("moba", 3), ("ffn", 3)]]


def kernel(**inputs):
    inputs = {k_: np.asarray(v) for k_, v in inputs.items()}
    x = inputs["x"].astype(np.float32)
    nb = x.shape[0]
    groups = [sum(LAYER_STAGES, [])] if FUSED else LAYER_STAGES
    xs = [x_to_dev(x[b]) for b in range(nb)]
    for stages in groups:
        P = build_program(stages)
        shared = dict(host_consts({kd for kd, _ in stages}))
        shared.update(stage_inputs(inputs, stages))
        shared = {k_: v for k_, v in shared.items() if k_ in P.ext_in}
        missing = set(P.ext_in) - set(shared) - {"xin"}
        assert not missing, missing
        in_maps = [dict(shared, xin=xs[b]) for b in range(nb)]
        res = run_bass_kernel_spmd(P.nc, in_maps, core_ids=list(range(nb)))
        xs = [np.asarray(res.results[b]["yout"], dtype=np.float32) for b in range(nb)]
    return np.stack([x_from_dev(xs[b]) for b in range(nb)]).astype(np.float32)
```

```python
import contextlib
import numpy as np
import concourse.bass as bass
import concourse.mybir as mybir
from concourse.bass_utils import run_bass_kernel_spmd

F32 = mybir.dt.float32
BF16 = mybir.dt.bfloat16
AF = mybir.ActivationFunctionType
ALU = mybir.AluOpType
AX = mybir.AxisListType

D = 1024
S = 4096
T = 512
NT = S // T
DEPTH = 4
FF = 3072
EPS = 1e-6
NEG = -30000.0


class Sem:
    __slots__ = ("h", "v")

    def __init__(self, h):
        self.h = h
        self.v = 0


class Buf:
    __slots__ = ("name", "w", "r")

    def __init__(self, name=""):
        self.name = name
        self.w = None
        self.r = {}


class Eng:
    def __init__(self, k, name, h, is_pe=False):
        self.k = k
        self.name = name
        self.h = h
        self.is_pe = is_pe
        self.sem = k.new_sem(name)
        self.dma_sems = []
        self.dma_i = 0
        self.waited = {}
        self.n_ops = 0
        self.n_waits = 0

    def next_dma_sem(self):
        if not self.dma_sems:
            n = 32 if self.name == "pool" else 16
            self.dma_sems = [self.k.new_sem(f"{self.name}_dma{i}") for i in range(n)]
        s = self.dma_sems[self.dma_i % len(self.dma_sems)]
        self.dma_i += 1
        return s


class K:
    def __init__(self, nc, es):
        self.nc = nc
        self.es = es
        self.sems = []
        self.pe = Eng(self, "pe", nc.tensor, is_pe=True)
        self.act = Eng(self, "act", nc.scalar)
        self.dve = Eng(self, "dve", nc.vector)
        self.pool = Eng(self, "pool", nc.gpsimd)
        self.sp = Eng(self, "sp", nc.sync)
        self.engs = [self.pe, self.act, self.dve, self.pool, self.sp]

    def new_sem(self, name):
        h = self.es.enter_context(self.nc.semaphore(f"s_{name}_{len(self.sems)}"))
        s = Sem(h)
        self.sems.append(s)
        return s

    def op(self, eng, fn, reads=(), writes=(), dma=False, after=None):
        deps = {}
        if after:
            for s, v in after.items():
                if deps.get(s, 0) < v:
                    deps[s] = v
        for b in reads:
            if b.w is not None and deps.get(b.w[0], 0) < b.w[1]:
                deps[b.w[0]] = b.w[1]
        for b in writes:
            if b.w is not None and deps.get(b.w[0], 0) < b.w[1]:
                deps[b.w[0]] = b.w[1]
            for s, v in b.r.items():
                if deps.get(s, 0) < v:
                    deps[s] = v
        for s, v in deps.items():
            if eng.is_pe and s is eng.sem:
                continue
            if eng.waited.get(s, 0) >= v:
                continue
            eng.h.wait_ge(s.h, v)
            eng.waited[s] = v
            eng.n_waits += 1
        if dma:
            ds = eng.next_dma_sem()
            if ds.v > 0 and eng.waited.get(ds, 0) < ds.v:
                eng.h.wait_ge(ds.h, ds.v)
                eng.waited[ds] = ds.v
                eng.n_waits += 1
        ins = fn(eng.h)
        eng.n_ops += 1
        if dma:
            s = ds
            s.v += 16
            ins.then_inc(s.h, 16)
        else:
            if eng.sem.v >= 30000:
                eng.sem = self.new_sem(eng.name)
            s = eng.sem
            s.v += 1
            ins.then_inc(s.h, 1)
        ev = (s, s.v)
        for b in reads:
            if b.r.get(s, 0) < s.v:
                b.r[s] = s.v
        for b in writes:
            b.w = ev
            b.r = {}
        return ev

    def snapshot(self):
        return {s: s.v for s in self.sems if s.v > 0}

    def barrier(self, snap, engines=None):
        for e in (engines or self.engs):
            for s, v in snap.items():
                if e.waited.get(s, 0) >= v:
                    continue
                e.h.wait_ge(s.h, v)
                e.waited[s] = v
                e.n_waits += 1


class Prog:
    def __init__(self, stages):
        self.stages = stages
        self.nc = bass.Bass("TRN2", target_bir_lowering=False)
        self.ext_in = {}

    def dram_in(self, name, shape, dt=F32):
        t = self.nc.dram_tensor(name, list(shape), dt, kind="ExternalInput")
        self.ext_in[name] = tuple(shape)
        return t.ap()

    def dram_tmp(self, name, shape, dt):
        kind = "ExternalOutput" if DEBUG_SCRATCH else "Internal"
        return self.nc.dram_tensor(name, list(shape), dt, kind=kind).ap()


DEBUG_SCRATCH = False
DEBUG_ONLY = None
SUBPHASES = {"ffn": ["F1", "F2"], "moba": ["M1", "M2", "M3"], "hgrn": ["H1"]}
NEEDS = {"F1": ("A", "F1", 0, 0, 0),
         "M1": ("A", "w_qkv", D, 3 * D, 1024), "H1": ("A", "w_in", D, 4 * D, 2048),
         "M2": ("A", None, 0, 0, 0)}


def build_program(stages):
    P = Prog(stages)
    nc = P.nc
    with contextlib.ExitStack() as es:
        k = K(nc, es)
        G = _Globals(P, k, es)
        n = len(stages)
        subs = []
        for i, (kind, layer) in enumerate(stages):
            io = dict(src=G.xin if i == 0 else G.xres, dst=G.yout if i == n - 1 else G.xres,
                      sb_src=G.xin_bufs if i == 0 else G.xres_bufs,
                      sb_dst=G.yout_bufs if i == n - 1 else G.xres_bufs)
            for sp in SUBPHASES[kind]:
                subs.append((sp, kind, layer, io))
        loaded = {}
        last_user = {"A": -1, "B": -1}
        regs = {"A": G.regA, "B": G.regB}
        for i, (sp, kind, layer, io) in enumerate(subs):
            snap = phase_begin(G)
            for r in ("A",):
                for j in range(i, len(subs)):
                    nd = NEEDS.get(subs[j][0])
                    if nd is not None and nd[0] == r:
                        if j not in loaded and last_user[r] < i:
                            W = G.w[(subs[j][1], subs[j][2])]
                            if nd[1] is None:
                                if j != i:
                                    break
                                loaded[j] = None
                            elif nd[1] == "F1":
                                lay = subs[j][2]
                                loaded[j] = [wup_third(G, lay, 1, G.regA, 0, snap), wup_third(G, lay, 2, G.regA, 16384, snap)]
                            else:
                                loaded[j] = WRegion(G, regs[r], snap, W[nd[1]], nd[2], nd[3], blk=nd[4], name=nd[1])
                            last_user[r] = j
                        break
            wr = loaded.get(i)
            if sp in ("M1", "H1") or (sp == "F1" and ("ffn", layer) not in G.wup0):
                for j in range(i, len(subs)):
                    if subs[j][0] == "F1":
                        lay = subs[j][2]
                        if ("ffn", lay) not in G.wup0:
                            G.wup0[("ffn", lay)] = wup_third(G, lay, 0, G.regB, 0, snap)
                        break
            if DEBUG_ONLY is None or sp in DEBUG_ONLY:
                PHASE_FN[sp](G, layer, wr, **io)
        k.barrier(k.snapshot(), engines=[k.sp])
        P.stats = {e.name: (e.n_ops, e.n_waits) for e in k.engs}
        P.nsems = len(k.sems)
    return P


class _Globals:
    def uniq(self, name):
        self._uid = getattr(self, "_uid", 0) + 1
        return f"{name}_u{self._uid}"

    def __init__(self, P, k, es):
        self.P = P
        self.k = k
        self.es = es
        nc = P.nc
        self.nc = nc
        stages = P.stages
        self.xin = P.dram_in("xin", [128, 8, S])
        self.yout = nc.dram_tensor("yout", [128, 8, S], F32, kind="ExternalOutput").ap()
        self.xres = P.dram_tmp("xres", [128, 8, S], F32)
        self.xin_bufs = [Buf(f"xin{t}") for t in range(16)]
        self.yout_bufs = [Buf(f"yout{t}") for t in range(16)]
        self.xres_bufs = [Buf(f"xres{t}") for t in range(16)]
        kinds = {kd for kd, _ in stages}
        self.c_ones = P.dram_in("c_ones", [128, 128])
        self.c_ident = P.dram_in("c_ident", [128, 128])
        sb = lambda name, shape, dt: es.enter_context(nc.sbuf_tensor(name, list(shape), dt))
        self.sb = sb
        self.ones_bf = sb("ones_bf", [128, 128], BF16)
        self.ident_bf = sb("ident_bf", [128, 128], BF16)
        self.ident_f = sb("ident_f", [128, 128], F32)
        self.b_const = Buf("consts")
        k.op(k.pool, lambda e: e.dma_start(out=self.ones_bf[:], in_=self.c_ones[:, :]), writes=[self.b_const], dma=True)
        k.op(k.pool, lambda e: e.dma_start(out=self.ident_bf[:], in_=self.c_ident[:, :]), writes=[self.b_const], dma=True)
        k.op(k.sp, lambda e: e.dma_start(out=self.ident_f[:], in_=self.c_ident[:, :]), writes=[self.b_const], dma=True)
        self.regA = sb("regA", [128, 49152], BF16)
        self.regB = sb("regB", [128, 24576], BF16)
        self.wup0 = {}
        self.wdn = {}
        self.ps = [es.enter_context(nc.psum_tensor(f"psb{i}", [128, 512], F32)) for i in range(8)]
        self.psb = [Buf(f"psb{i}") for i in range(8)]
        self.w = {}
        for kind, l in stages:
            if kind == "ffn":
                self.w[("ffn", l)] = dict(
                    w_up=P.dram_in(f"ffn_w_up_{l}", [D, 2 * FF]),
                    w_dn=P.dram_in(f"ffn_w_dn_{l}", [FF, D]),
                    vec=P.dram_in(f"ffn_vec_{l}", [128, 8 + 24 * 4]),
                )
            elif kind == "moba":
                self.w[("moba", l)] = dict(
                    w_qkv=P.dram_in(f"moba_w_qkv_{l}", [D, 3 * D]),
                    w_out=P.dram_in(f"moba_w_out_{l}", [D, D]),
                    vec=P.dram_in(f"moba_vec_{l}", [128, 8 + 2]),
                )
            elif kind == "hgrn":
                self.w[("hgrn", l)] = dict(
                    w_in=P.dram_in(f"hgrn_w_in_{l}", [D, 4 * D]),
                    w_out=P.dram_in(f"hgrn_w_out_{l}", [D, D]),
                    vec=P.dram_in(f"hgrn_vec_{l}", [128, 8 + 16 + 1]),
                )
        if "ffn" in kinds:
            self.gT = P.dram_tmp("gT", [128, 24, S], BF16)
            self.gT_bufs = [[Buf(f"gT{t}_{g}") for g in range(6)] for t in range(NT)]
        if "moba" in kinds:
            self.c_rot = P.dram_in("c_rot", [128, 128])
            self.c_cos = P.dram_in("c_cos", [128, S])
            self.c_sin = P.dram_in("c_sin", [128, S])
            self.c_past = P.dram_in("c_past", [128, 32 * 16])
            self.c_causal = P.dram_in("c_causal", [128, 2 * 256])
            self.c_selrow = P.dram_in("c_selrow", [128, 16 * 128])
            self.qT = P.dram_tmp("qT", [8, 128, S], BF16)
            self.kT = P.dram_tmp("kT", [8, 128, S], BF16)
            self.vtok = P.dram_tmp("vtok", [128, 32, D], BF16)
            self.oT = P.dram_tmp("oT", [128, 8, S], BF16)
            self.q_bufs = [[Buf(f"q{h}_{t}") for t in range(NT)] for h in range(8)]
            self.k_bufs = [[Buf(f"k{h}_{t}") for t in range(NT)] for h in range(8)]
            self.v_bufs = [Buf(f"v{t}") for t in range(NT)]
            self.o_bufs = [Buf(f"o{h}") for h in range(8)]
        if "hgrn" in kinds:
            self.c_tri = P.dram_in("c_tri", [128, 128])
            self.c_scan = P.dram_in("c_scan", [128, T])


class WRegion:
    def __init__(self, G, reg, free_after, w_ap, kdim, ncols, col0=0, blk=2048, name="w", segs=None, reg_off=0):
        k = G.k
        if segs is None:
            segs = [(col0, ncols)]
        ncols = sum(n for _, n in segs)
        self.kc = kdim // 128
        self.ncols = ncols
        self.blk = min(blk, min(n for _, n in segs))
        self.view = reg[:, reg_off:reg_off + self.kc * ncols].rearrange("p (kc n) -> p kc n", n=ncols)
        self.bufs = {}
        wv = w_ap.rearrange("(kc p) n -> p kc n", p=128)
        for kc in range(self.kc):
            d0 = 0
            for (c0, n) in segs:
                for j in range(n // self.blk):
                    b = Buf(f"{name}_{kc}_{d0 // self.blk}")
                    self.bufs[(kc, d0 // self.blk)] = b
                    k.op(k.pool,
                         lambda e, kc=kc, d0=d0, s0=c0 + j * self.blk: e.dma_start(
                             out=self.view[:, kc, d0:d0 + self.blk], in_=wv[:, kc, s0:s0 + self.blk]),
                         writes=[b], dma=True, after=free_after)
                    d0 += self.blk

    def buf(self, kc, n0):
        return self.bufs[(kc, n0 // self.blk)]


def wup_third(G, layer, g, reg, reg_off, free_after):
    W = G.w[("ffn", layer)]
    return WRegion(G, reg, free_after, W["w_up"], D, 2048, blk=1024, name=f"wup{g}",
                   segs=[(g * 1024, 1024), (FF + g * 1024, 1024)], reg_off=reg_off)


def rmsnorm_tile(G, L, xt, b_xt, gain, hT, b_hT, ps_i, nfeat_chunks=8, ps_ap=None, ps_buf=None):
    k = G.k
    ps, pb = (G.ps[ps_i], G.psb[ps_i]) if ps_ap is None else (ps_ap, ps_buf)
    for c in range(nfeat_chunks):
        sq, b_sq = L["sq"][c % 2]
        k.op(k.act, lambda e, c=c, sq=sq: e.activation(out=sq[:], in_=xt[:, c, :], func=AF.Square),
             reads=[b_xt], writes=[b_sq])
        k.op(k.pe, lambda e, c=c, sq=sq: e.matmul(ps if ps_ap is not None else ps[:], lhsT=G.ones_bf[:], rhs=sq[:], start=(c == 0),
                                                  stop=(c == nfeat_chunks - 1)),
             reads=[b_sq, G.b_const], writes=(pb if isinstance(pb, list) else [pb]))
    rs, b_rs = L["rstd"]
    k.op(k.act, lambda e: e.activation(out=rs[:], in_=(ps if ps_ap is not None else ps[:]), func=AF.Ln, scale=1.0 / (128 * nfeat_chunks),
                                       bias=L["eps"][:, 0:1]),
         reads=(pb if isinstance(pb, list) else [pb]) + [L["b_eps"]], writes=[b_rs])
    k.op(k.act, lambda e: e.activation(out=rs[:], in_=rs[:], func=AF.Exp, scale=-0.5), reads=[b_rs], writes=[b_rs])
    for c in range(nfeat_chunks):
        k.op(k.dve, lambda e, c=c: e.scalar_tensor_tensor(out=hT[:, c, :], in0=xt[:, c, :], scalar=gain[:, c:c + 1],
                                                          in1=rs[:], op0=ALU.mult, op1=ALU.mult),
             reads=[b_xt, b_rs, L["b_vec"]], writes=[b_hT])


def phase_begin(G):
    snap = G.k.snapshot()
    G.k.barrier(snap)
    return snap


def phase_F1(G, layer, wups, src, dst, sb_src, sb_dst):
    k, nc = G.k, G.nc
    W = G.w[("ffn", layer)]
    with contextlib.ExitStack() as es:
        sb = lambda name, shape, dt: es.enter_context(nc.sbuf_tensor(G.uniq(name), list(shape), dt))
        vec = sb("f_vec", [128, 8 + 96], F32)
        b_vec = Buf("f_vec")
        k.op(k.sp, lambda e: e.dma_start(out=vec[:], in_=W["vec"][:, :]), writes=[b_vec], dma=True)
        eps = sb("f_eps", [128, 1], F32)
        b_eps = Buf("f_eps")
        k.op(k.pool, lambda e: e.memset(eps[:], EPS), writes=[b_eps])
        gain = vec[:, 0:8]
        cw = vec[:, 8:104].rearrange("p (j f) -> p j f", f=24)
        xt = sb("f_xt", [128, 8, T], F32)
        b_xt = Buf("f_xt")
        hTs = [(sb(f"f_hT{i}", [128, 8, T], BF16), Buf(f"f_hT{i}")) for i in range(2)]
        L = dict(sq=[(sb(f"f_sq{i}", [128, T], BF16), Buf(f"f_sq{i}")) for i in range(2)],
                 rstd=(sb("f_rstd", [128, T], F32), Buf("f_rstd")), eps=eps, b_eps=b_eps, b_vec=b_vec)
        abufs = [(sb(f"f_ab{i}", [128, T + 2], F32), Buf(f"f_ab{i}")) for i in range(4)]
        tbufs = [(sb(f"f_t{i}", [128, T], F32), Buf(f"f_t{i}")) for i in range(4)]
        gbufs = [(sb(f"f_g{i}", [128, 4, T], BF16), Buf(f"f_g{i}")) for i in range(2)]
        carry = sb("f_carry", [128, 24, 2], F32)
        b_carry = [Buf(f"f_carry{f}") for f in range(24)]
        k.op(k.pool, lambda e: e.memset(carry[:], 0.0), writes=b_carry)

        thirds = [G.wup0[("ffn", layer)], wups[0], wups[1]]
        k.op(k.sp, lambda e: e.dma_start(out=xt[:], in_=src[:, :, 0:T]), reads=sb_src[0:2], writes=[b_xt], dma=True)
        def emit_norm(idx):
            hT_, b_hT_ = hTs[idx % 2]
            rmsnorm_tile(G, L, xt, b_xt, gain, hT_, b_hT_, ps_i=0)
            if idx + 1 < 3 * NT:
                tn = (idx + 1) % NT
                k.op(k.sp, lambda e, tn=tn: e.dma_start(out=xt[:], in_=src[:, :, tn * T:(tn + 1) * T]),
                     reads=sb_src[2 * tn:2 * tn + 2], writes=[b_xt], dma=True)

        it = 0
        emit_norm(0)
        for fcg in range(3):
          wup = thirds[fcg]
          for ti in range(NT):
            hT, b_hT = hTs[it % 2]
            it += 1
            def fc_chain(f):
                fc = fcg * 8 + f
                pa_i, pu_i = [(1, 2), (3, 4), (5, 6), (7, 0)][fc % 4]
                pa, pu = G.ps[pa_i], G.ps[pu_i]
                for kc in range(8):
                    k.op(k.pe, lambda e, kc=kc, f=f, pa=pa: e.matmul(
                        pa[:], lhsT=wup.view[:, kc, f * 128:(f + 1) * 128], rhs=hT[:, kc, :],
                        start=(kc == 0), stop=(kc == 7)),
                        reads=[wup.buf(kc, f * 128), b_hT], writes=[G.psb[pa_i]])
                for kc in range(8):
                    k.op(k.pe, lambda e, kc=kc, f=f, pu=pu: e.matmul(
                        pu[:], lhsT=wup.view[:, kc, 1024 + f * 128:1024 + (f + 1) * 128], rhs=hT[:, kc, :],
                        start=(kc == 0), stop=(kc == 7)),
                        reads=[wup.buf(kc, 1024 + f * 128), b_hT], writes=[G.psb[pu_i]])
                ab, b_ab = abufs[fc % 4]
                tb, b_tb = tbufs[fc % 4]
                gb, b_gb = gbufs[(fc // 4) % 2]
                k.op(k.dve, lambda e, fc=fc, ab=ab: e.tensor_copy(out=ab[:, 0:2], in_=carry[:, fc, :]),
                     reads=[b_carry[fc]], writes=[b_ab])
                yield
                k.op(k.act, lambda e, ab=ab, pa=pa: e.activation(out=ab[:, 2:T + 2], in_=pa[:], func=AF.Copy),
                     reads=[G.psb[pa_i]], writes=[b_ab])
                yield
                k.op(k.act, lambda e, fc=fc, tb=tb, pa=pa: e.activation(out=tb[:], in_=pa[:], func=AF.Identity,
                                                                       bias=cw[:, 3, fc:fc + 1], scale=cw[:, 2, fc:fc + 1]),
                     reads=[G.psb[pa_i], b_vec], writes=[b_tb])
                yield
                k.op(k.dve, lambda e, fc=fc, tb=tb, ab=ab: e.scalar_tensor_tensor(
                    out=tb[:], in0=ab[:, 1:T + 1], scalar=cw[:, 1, fc:fc + 1], in1=tb[:], op0=ALU.mult, op1=ALU.add),
                    reads=[b_ab, b_tb, b_vec], writes=[b_tb])
                yield
                k.op(k.dve, lambda e, fc=fc, tb=tb, ab=ab: e.scalar_tensor_tensor(
                    out=tb[:], in0=ab[:, 0:T], scalar=cw[:, 0, fc:fc + 1], in1=tb[:], op0=ALU.mult, op1=ALU.add),
                    reads=[b_ab, b_tb, b_vec], writes=[b_tb])
                yield
                k.op(k.act, lambda e, tb=tb: e.activation(out=tb[:], in_=tb[:], func=AF.Silu), reads=[b_tb], writes=[b_tb])
                yield
                k.op(k.dve, lambda e, fc=fc, tb=tb, gb=gb, pu=pu: e.tensor_tensor(
                    out=gb[:, fc % 4, :], in0=tb[:], in1=pu[:], op=ALU.mult),
                    reads=[b_tb, G.psb[pu_i]], writes=[b_gb])
                yield
                k.op(k.dve, lambda e, fc=fc, ab=ab: e.tensor_copy(out=carry[:, fc, :], in_=ab[:, T:T + 2]),
                     reads=[b_ab], writes=[b_carry[fc]])
                yield
                if fc % 4 == 3:
                    f0 = fc - 3
                    k.op(k.sp, lambda e, f0=f0, gb=gb, ti=ti: e.dma_start(
                        out=G.gT[:, f0:f0 + 4, ti * T:(ti + 1) * T], in_=gb[:]),
                        reads=[b_gb], writes=[G.gT_bufs[ti][fc // 4]], dma=True)

            for f0 in range(0, 8, 2):
                interleave([fc_chain(f0), fc_chain(f0 + 1)])
                if f0 == 2 and it < 3 * NT:
                    emit_norm(it)
          if fcg == 0:
            G.wdn[layer] = WRegion(G, G.regB, k.snapshot(), W["w_dn"], FF, D, blk=1024, name="wdn")


def phase_F2(G, layer, wr, src, dst, sb_src, sb_dst):
    k, nc = G.k, G.nc
    wdn = G.wdn[layer]
    with contextlib.ExitStack() as es:
        sb = lambda name, shape, dt: es.enter_context(nc.sbuf_tensor(G.uniq(name), list(shape), dt))
        xts = [(sb(f"g_xt{i}", [128, 8, T], F32), Buf(f"g_xt{i}")) for i in range(2)]
        gts = [(sb(f"g_gt{i}", [128, 12, T], BF16), Buf(f"g_gt{i}")) for i in range(2)]
        for ti in range(NT):
            xt, b_xt = xts[ti % 2]
            k.op(k.sp, lambda e, ti=ti, xt=xt: e.dma_start(out=xt[:], in_=src[:, :, ti * T:(ti + 1) * T]),
                 reads=sb_src[2 * ti:2 * ti + 2], writes=[b_xt], dma=True)
            for half in range(2):
                gt, b_gt = gts[half]
                k.op(k.act, lambda e, ti=ti, gt=gt, half=half: e.dma_start(
                    out=gt[:], in_=G.gT[:, half * 12:(half + 1) * 12, ti * T:(ti + 1) * T]),
                    reads=G.gT_bufs[ti][half * 3:(half + 1) * 3], writes=[b_gt], dma=True)
                for oc in range(8):
                    for f in range(12):
                        fc = half * 12 + f
                        k.op(k.pe, lambda e, oc=oc, fc=fc, f=f, gt=gt: e.matmul(
                            G.ps[oc][:], lhsT=wdn.view[:, fc, oc * 128:(oc + 1) * 128], rhs=gt[:, f, :],
                            start=(fc == 0), stop=(fc == 23)),
                            reads=[wdn.buf(fc, oc * 128), b_gt], writes=[G.psb[oc]])
            for oc in range(8):
                k.op(k.dve, lambda e, oc=oc, xt=xt: e.tensor_tensor(out=xt[:, oc, :], in0=G.ps[oc][:], in1=xt[:, oc, :],
                                                                   op=ALU.add),
                     reads=[G.psb[oc], b_xt], writes=[b_xt])
            k.op(k.sp, lambda e, ti=ti, xt=xt: e.dma_start(out=dst[:, :, ti * T:(ti + 1) * T], in_=xt[:]),
                 reads=[b_xt], writes=sb_dst[2 * ti:2 * ti + 2], dma=True)


def interleave(gens):
    gens = list(gens)
    while gens:
        for g in list(gens):
            try:
                next(g)
            except StopIteration:
                gens.remove(g)


def carve(reg, off, shape):
    n = int(np.prod(shape[1:]))
    v = reg[:, off:off + n]
    if len(shape) == 3:
        v = v.rearrange("p (a b) -> p a b", b=shape[2])
    return v, off + n


def phase_M1(G, layer, wqkv, src, dst, sb_src, sb_dst):
    k, nc = G.k, G.nc
    W = G.w[("moba", layer)]
    with contextlib.ExitStack() as es:
        sb = lambda name, shape, dt: es.enter_context(nc.sbuf_tensor(G.uniq(name), list(shape), dt))
        vec = sb("m_vec", [128, 10], F32)
        b_vec = Buf("m_vec")
        k.op(k.sp, lambda e: e.dma_start(out=vec[:], in_=W["vec"][:, :]), writes=[b_vec], dma=True)
        qkg = sb("m_qkg", [128, 2], F32)
        k.op(k.dve, lambda e: e.tensor_scalar(out=qkg[:, 0:1], in0=vec[:, 8:9], scalar1=128.0 ** -0.5, scalar2=None,
                                              op0=ALU.mult), reads=[b_vec], writes=[b_vec])
        k.op(k.dve, lambda e: e.tensor_copy(out=qkg[:, 1:2], in_=vec[:, 9:10]), reads=[b_vec], writes=[b_vec])
        eps = sb("m_eps", [128, 1], F32)
        b_eps = Buf("m_eps")
        k.op(k.pool, lambda e: e.memset(eps[:], EPS), writes=[b_eps])
        gain = vec[:, 0:8]
        xt = sb("m_xt", [128, 8, T], F32)
        b_xt = Buf("m_xt")
        cs = sb("m_cs", [128, 2, T], F32)
        b_cs = Buf("m_cs")
        off = 8 * 3 * D
        hTs = []
        for i in range(2):
            v, off = carve(G.regA, off, [128, 8, T])
            hTs.append((v, Buf(f"m_hT{i}")))
        vt, off = carve(G.regA, off, [128, 4, D])
        b_vt = Buf("m_vt")
        rot_bf, off = carve(G.regA, off, [128, 128])
        b_rot = Buf("m_rot")
        k.op(k.pool, lambda e: e.dma_start(out=rot_bf, in_=G.c_rot[:, :]), writes=[b_rot], dma=True)
        two = lambda nm: None
        sqs, sqh, qnb, qfs = [], [], [], []
        NS = 4
        for i in range(2):
            v, off = carve(G.regA, off, [128, T]); sqs.append((v, Buf(f"m_sq{i}")))
        for i in range(NS):
            v, off = carve(G.regA, off, [128, T]); sqh.append((v, Buf(f"m_sqh{i}")))
            v, off = carve(G.regA, off, [128, T]); qnb.append((v, Buf(f"m_qnb{i}")))
            v, off = carve(G.regA, off, [128, T]); qfs.append((v, Buf(f"m_qf{i}")))
        assert off <= 49152
        L = dict(sq=sqs, rstd=(sb("m_rstd", [128, T], F32), Buf("m_rstd")), eps=eps, b_eps=b_eps, b_vec=b_vec)
        r2s = [(sb(f"m_r2{i}", [128, T], F32), Buf(f"m_r2{i}")) for i in range(NS)]
        qns = [(sb(f"m_qn{i}", [128, T], F32), Buf(f"m_qn{i}")) for i in range(NS)]
        t1s = [(sb(f"m_t1{i}", [128, T], F32), Buf(f"m_t1{i}")) for i in range(NS)]
        t2s = [(sb(f"m_t2{i}", [128, T], F32), Buf(f"m_t2{i}")) for i in range(NS)]

        k.op(k.sp, lambda e: e.dma_start(out=xt[:], in_=src[:, :, 0:T]), reads=sb_src[0:2], writes=[b_xt], dma=True)
        cnt = 0
        dbg = "vq"

        def emit_norm(idx):
            hT_, b_hT_ = hTs[idx % 2]
            rmsnorm_tile(G, L, xt, b_xt, gain, hT_, b_hT_, ps_i=0)
            if idx + 1 < NT:
                k.op(k.sp, lambda e: e.dma_start(out=xt[:], in_=src[:, :, (idx + 1) * T:(idx + 2) * T]),
                     reads=sb_src[2 * idx + 2:2 * idx + 4], writes=[b_xt], dma=True)

        emit_norm(0)
        for ti in range(NT):
            hT, b_hT = hTs[ti % 2]
            k.op(k.sp, lambda e, ti=ti: e.dma_start(out=cs[:, 0, :], in_=G.c_cos[:, ti * T:(ti + 1) * T]),
                 writes=[b_cs], dma=True)
            k.op(k.sp, lambda e, ti=ti: e.dma_start(out=cs[:, 1, :], in_=G.c_sin[:, ti * T:(ti + 1) * T]),
                 writes=[b_cs], dma=True)
            for sub in range(4 if "v" in dbg else 0):
                for half in range(2):
                    pi = 1 + (sub * 2 + half) % 2
                    for kc in range(8):
                        k.op(k.pe, lambda e, kc=kc, sub=sub, half=half, pi=pi: e.matmul(
                            G.ps[pi][:], lhsT=hT[:, kc, sub * 128:(sub + 1) * 128],
                            rhs=wqkv.view[:, kc, 2 * D + half * 512:2 * D + (half + 1) * 512],
                            start=(kc == 0), stop=(kc == 7)),
                            reads=[b_hT, wqkv.buf(kc, 2 * D + half * 512)], writes=[G.psb[pi]])
                    k.op(k.act, lambda e, sub=sub, half=half, pi=pi: e.activation(
                        out=vt[:, sub, half * 512:(half + 1) * 512], in_=G.ps[pi][:], func=AF.Copy),
                        reads=[G.psb[pi]], writes=[b_vt])
            if "v" in dbg:
                k.op(k.sp, lambda e, ti=ti: e.dma_start(out=G.vtok[:, ti * 4:(ti + 1) * 4, :], in_=vt),
                     reads=[b_vt], writes=[G.v_bufs[ti]], dma=True)
            def qk_chain(which, h, sl, g):
                col0 = which * D + h * 128
                pi = 1 + (g % 2) * 2 + sl
                pss = 5 if sl == 0 else 0
                pr = 6 + sl
                sl = (g % 2) * 2 + sl
                sq2, b_sq2 = sqh[sl]
                r2, b_r2 = r2s[sl]
                qn, b_qn = qns[sl]
                qb, b_qb = qnb[sl]
                t1, b_t1 = t1s[sl]
                t2, b_t2 = t2s[sl]
                qf, b_qf = qfs[sl]
                for kc in range(8):
                    k.op(k.pe, lambda e: e.matmul(
                        G.ps[pi][:], lhsT=wqkv.view[:, kc, col0:col0 + 128], rhs=hT[:, kc, :],
                        start=(kc == 0), stop=(kc == 7)),
                        reads=[b_hT, wqkv.buf(kc, col0)], writes=[G.psb[pi]])
                yield
                k.op(k.act, lambda e: e.activation(out=sq2, in_=G.ps[pi][:], func=AF.Square),
                     reads=[G.psb[pi]], writes=[b_sq2])
                yield
                k.op(k.pe, lambda e: e.matmul(G.ps[pss][:], lhsT=G.ones_bf[:], rhs=sq2, start=True, stop=True),
                     reads=[b_sq2, G.b_const], writes=[G.psb[pss]])
                yield
                k.op(k.act, lambda e: e.activation(out=r2[:], in_=G.ps[pss][:], func=AF.Ln, bias=eps[:, 0:1],
                                                   scale=1.0 / 128), reads=[G.psb[pss], b_eps], writes=[b_r2])
                yield
                k.op(k.act, lambda e: e.activation(out=r2[:], in_=r2[:], func=AF.Exp, scale=-0.5),
                     reads=[b_r2], writes=[b_r2])
                yield
                k.op(k.dve, lambda e: e.scalar_tensor_tensor(
                    out=qn[:], in0=G.ps[pi][:], scalar=qkg[:, which:which + 1], in1=r2[:], op0=ALU.mult, op1=ALU.mult),
                    reads=[G.psb[pi], b_r2, b_vec], writes=[b_qn])
                yield
                k.op(k.act, lambda e: e.activation(out=qb, in_=qn[:], func=AF.Copy), reads=[b_qn], writes=[b_qb])
                yield
                k.op(k.pe, lambda e: e.matmul(G.ps[pr][:], lhsT=rot_bf, rhs=qb, start=True, stop=True),
                     reads=[b_qb, b_rot], writes=[G.psb[pr]])
                yield
                k.op(k.dve, lambda e: e.tensor_tensor(out=t1[:], in0=qn[:], in1=cs[:, 0, :], op=ALU.mult),
                     reads=[b_qn, b_cs], writes=[b_t1])
                yield
                k.op(k.dve, lambda e: e.tensor_tensor(out=t2[:], in0=G.ps[pr][:], in1=cs[:, 1, :], op=ALU.mult),
                     reads=[G.psb[pr], b_cs], writes=[b_t2])
                yield
                k.op(k.dve, lambda e: e.tensor_tensor(out=qf, in0=t1[:], in1=t2[:], op=ALU.add),
                     reads=[b_t1, b_t2], writes=[b_qf])
                yield
                dT = G.qT if which == 0 else G.kT
                db = G.q_bufs if which == 0 else G.k_bufs
                k.op(k.sp, lambda e: e.dma_start(out=dT[h, :, ti * T:(ti + 1) * T], in_=qf),
                     reads=[b_qf], writes=[db[h][ti]], dma=True)
                yield

            chains = [(w_, h_) for w_ in range(2) for h_ in range(8)]
            for g in range(8):
                interleave([qk_chain(w_, h_, sl, g) for sl, (w_, h_) in enumerate(chains[2 * g:2 * g + 2])])
                if g == 5 and ti + 1 < NT:
                    emit_norm(ti + 1)


def phase_M2(G, layer, wr, src, dst, sb_src, sb_dst):
    k, nc = G.k, G.nc
    with contextlib.ExitStack() as es:
        sb = lambda name, shape, dt: es.enter_context(nc.sbuf_tensor(G.uniq(name), list(shape), dt))
        off = 0
        qT, off = carve(G.regA, off, [128, S]); b_q = Buf("a_q")
        kT, off = carve(G.regA, off, [128, S]); b_k = Buf("a_k")
        vt, off = carve(G.regA, off, [128, 32, 128]); b_v = Buf("a_v")
        oTh, off = carve(G.regA, off, [128, S]); b_o = Buf("a_o")
        biasT, off = carve(G.regA, off, [128, S]); b_bT = Buf("a_bT")
        causal, off = carve(G.regA, off, [128, 512]); b_cz = Buf("a_causal")
        selrow, off = carve(G.regA, off, [128, 2048]); b_sr = Buf("a_selrow")
        km_bf, off = carve(G.regA, off, [128, 16]); b_kmb = Buf("a_kmb")
        PTs = []
        for i in range(4):
            v, off = carve(G.regA, off, [128, 256]); PTs.append((v, Buf(f"a_PT{i}")))
        assert off <= 49152
        past = sb("a_past", [128, 512], F32); b_past = Buf("a_past")
        km = sb("a_km", [128, 16], F32); b_km = Buf("a_km")
        gm = sb("a_gm", [128, 512], F32); b_gm = Buf("a_gm")
        top8 = sb("a_top8", [128, 32, 8], F32); b_top8 = Buf("a_top8")
        thr = sb("a_thr", [128, 32], F32); b_thr = Buf("a_thr")
        sel = sb("a_sel", [128, 512], F32); b_sel = Buf("a_sel")
        rdens = [(sb(f"a_rden{i}", [128, 256], F32), Buf(f"a_rden{i}")) for i in range(2)]
        k.op(k.sp, lambda e: e.dma_start(out=past[:], in_=G.c_past[:, :]), writes=[b_past], dma=True)
        k.op(k.pool, lambda e: e.dma_start(out=causal, in_=G.c_causal[:, :]), writes=[b_cz], dma=True)
        k.op(k.pool, lambda e: e.dma_start(out=selrow, in_=G.c_selrow[:, :]), writes=[b_sr], dma=True)
        k.op(k.pool, lambda e: e.memset(biasT, 0.0), writes=[b_bT])
        scnt = 0
        for h in range(8):
            k.op(k.sp, lambda e, h=h: e.dma_start(out=qT, in_=G.qT[h, :, :]), reads=G.q_bufs[h], writes=[b_q], dma=True)
            k.op(k.sp, lambda e, h=h: e.dma_start(out=kT, in_=G.kT[h, :, :]), reads=G.k_bufs[h], writes=[b_k], dma=True)
            k.op(k.sp, lambda e, h=h: e.dma_start(out=vt, in_=G.vtok[:, :, h * 128:(h + 1) * 128]),
                 reads=G.v_bufs, writes=[b_v], dma=True)
            k.op(k.dve, lambda e: e.tensor_reduce(out=km[:], in_=kT.rearrange("p (n j) -> p n j", j=256), axis=AX.X,
                                                  op=ALU.add), reads=[b_k], writes=[b_km])
            k.op(k.dve, lambda e: e.tensor_scalar(out=km_bf, in0=km[:], scalar1=1.0 / 256, scalar2=None, op0=ALU.mult),
                 reads=[b_km], writes=[b_kmb])
            for i in range(32):
                k.op(k.pe, lambda e, i=i: e.matmul(G.ps[0][:, i * 16:(i + 1) * 16], lhsT=qT[:, i * 128:(i + 1) * 128],
                                                   rhs=km_bf, start=True, stop=True),
                     reads=[b_q, b_kmb], writes=[G.psb[0]])
            k.op(k.dve, lambda e: e.tensor_tensor(out=gm[:], in0=G.ps[0][:], in1=past[:], op=ALU.add),
                 reads=[G.psb[0], b_past], writes=[b_gm])
            for i in range(32):
                k.op(k.dve, lambda e, i=i: e.max(out=top8[:, i, :], in_=gm[:, i * 16:(i + 1) * 16]),
                     reads=[b_gm], writes=[b_top8])
            k.op(k.dve, lambda e: e.tensor_scalar(out=thr[:], in0=top8[:, :, 2], scalar1=-1e29, scalar2=None, op0=ALU.max),
                 reads=[b_top8], writes=[b_thr])
            k.op(k.dve, lambda e: e.tensor_tensor(
                out=sel[:].rearrange("p (i n) -> p i n", n=16), in0=gm[:].rearrange("p (i n) -> p i n", n=16),
                in1=thr[:].unsqueeze(2).to_broadcast([128, 32, 16]), op=ALU.is_ge),
                reads=[b_gm, b_thr], writes=[b_sel])
            k.op(k.dve, lambda e: e.tensor_scalar(out=sel[:], in0=sel[:], scalar1=-1.0, scalar2=-NEG, op0=ALU.add,
                                                  op1=ALU.mult), reads=[b_sel], writes=[b_sel])
            for g in range(8):
                pi = g % 2
                for i4 in range(4):
                    i = g * 4 + i4
                    k.op(k.pe, lambda e, i=i, i4=i4, pi=pi: e.transpose(
                        out=G.ps[pi][0:16, i4 * 128:(i4 + 1) * 128], in_=sel[:, i * 16:(i + 1) * 16], identity=G.ident_f[:]),
                        reads=[b_sel, G.b_const], writes=[G.psb[pi]])
                k.op(k.act, lambda e, g=g, pi=pi: e.activation(out=biasT[0:16, g * 512:(g + 1) * 512],
                                                                in_=G.ps[pi][0:16, :], func=AF.Copy),
                     reads=[G.psb[pi]], writes=[b_bT])
            items = [(j, n, kt) for j in range(16) for n in range(j + 1) for kt in range(2)]
            LA = 3

            def s_stage(ii):
                j, n, kt = items[ii]
                qs = slice(j * 256, (j + 1) * 256)
                kc0 = n * 256 + kt * 128
                pi = 2 + ii % 4
                k.op(k.pe, lambda e: e.matmul(
                    G.ps[pi][:, 0:256], lhsT=kT[:, kc0:kc0 + 128], rhs=qT[:, qs], start=True, stop=False),
                    reads=[b_k, b_q], writes=[G.psb[pi]])
                if n < j:
                    k.op(k.pe, lambda e: e.matmul(
                        G.ps[pi][:, 0:256], lhsT=selrow[:, n * 128:(n + 1) * 128], rhs=biasT[:, qs],
                        start=False, stop=True), reads=[b_sr, b_bT], writes=[G.psb[pi]])
                else:
                    k.op(k.pe, lambda e: e.matmul(
                        G.ps[pi][:, 0:256], lhsT=G.ident_bf[:], rhs=causal[:, kt * 256:(kt + 1) * 256],
                        start=False, stop=True), reads=[b_cz, G.b_const], writes=[G.psb[pi]])

            def p_stage(ii):
                j, n, kt = items[ii]
                qs = slice(j * 256, (j + 1) * 256)
                po, pd = (6, 7) if j % 2 == 0 else (0, 1)
                pi = 2 + ii % 4
                PT, b_PT = PTs[ii % 4]
                first = (n == 0 and kt == 0)
                last = (n == j and kt == 1)
                k.op(k.act, lambda e: e.activation(out=PT, in_=G.ps[pi][:, 0:256], func=AF.Exp),
                     reads=[G.psb[pi]], writes=[b_PT])
                k.op(k.pe, lambda e: e.matmul(
                    G.ps[po][:, 0:256], lhsT=vt[:, n * 2 + kt, :], rhs=PT, start=first, stop=last),
                    reads=[b_v, b_PT], writes=[G.psb[po]])
                k.op(k.pe, lambda e: e.matmul(
                    G.ps[pd][:, 0:256], lhsT=G.ones_bf[:], rhs=PT, start=first, stop=last),
                    reads=[G.b_const, b_PT], writes=[G.psb[pd]])
                if last:
                    rd, b_rd = rdens[j % 2]
                    k.op(k.dve, lambda e: e.reciprocal(out=rd[:], in_=G.ps[pd][:, 0:256]),
                         reads=[G.psb[pd]], writes=[b_rd])
                    k.op(k.dve, lambda e: e.tensor_tensor(out=oTh[:, qs], in0=G.ps[po][:, 0:256], in1=rd[:], op=ALU.mult),
                         reads=[G.psb[po], b_rd], writes=[b_o])

            for ii in range(len(items) + LA):
                if ii < len(items):
                    s_stage(ii)
                if ii >= LA:
                    p_stage(ii - LA)
            k.op(k.sp, lambda e, h=h: e.dma_start(out=G.oT[:, h, :], in_=oTh), reads=[b_o], writes=[G.o_bufs[h]], dma=True)


def phase_M3(G, layer, wr, src, dst, sb_src, sb_dst):
    k, nc = G.k, G.nc
    W = G.w[("moba", layer)]
    with contextlib.ExitStack() as es:
        sb = lambda name, shape, dt: es.enter_context(nc.sbuf_tensor(G.uniq(name), list(shape), dt))
        regC = sb("regC", [128, 8 * D], BF16)
        wout = WRegion(G, regC, None, W["w_out"], D, D, blk=1024, name="wout")
        xts = [(sb(f"o_xt{i}", [128, 8, T], F32), Buf(f"o_xt{i}")) for i in range(1)]
        ots = [(sb(f"o_ot{i}", [128, 8, T], BF16), Buf(f"o_ot{i}")) for i in range(2)]
        for ti in range(NT):
            xt, b_xt = xts[0]
            ot, b_ot = ots[ti % 2]
            k.op(k.sp, lambda e, ti=ti, xt=xt: e.dma_start(out=xt[:], in_=src[:, :, ti * T:(ti + 1) * T]),
                 reads=sb_src[2 * ti:2 * ti + 2], writes=[b_xt], dma=True)
            k.op(k.act, lambda e, ti=ti, ot=ot: e.dma_start(out=ot[:], in_=G.oT[:, :, ti * T:(ti + 1) * T]),
                 reads=G.o_bufs, writes=[b_ot], dma=True)
            for oc in range(8):
                for h in range(8):
                    k.op(k.pe, lambda e, oc=oc, h=h, ot=ot: e.matmul(
                        G.ps[oc][:], lhsT=wout.view[:, h, oc * 128:(oc + 1) * 128], rhs=ot[:, h, :],
                        start=(h == 0), stop=(h == 7)),
                        reads=[wout.buf(h, oc * 128), b_ot], writes=[G.psb[oc]])
                k.op(k.dve, lambda e, oc=oc, xt=xt: e.tensor_tensor(out=xt[:, oc, :], in0=G.ps[oc][:], in1=xt[:, oc, :],
                                                                   op=ALU.add),
                     reads=[G.psb[oc], b_xt], writes=[b_xt])
            k.op(k.sp, lambda e, ti=ti, xt=xt: e.dma_start(out=dst[:, :, ti * T:(ti + 1) * T], in_=xt[:]),
                 reads=[b_xt], writes=sb_dst[2 * ti:2 * ti + 2], dma=True)


TH = 256
NTH = S // TH


def phase_H1(G, layer, win, src, dst, sb_src, sb_dst):
    k, nc = G.k, G.nc
    W = G.w[("hgrn", layer)]
    slot = layer // 2
    with contextlib.ExitStack() as es:
        sb = lambda name, shape, dt: es.enter_context(nc.sbuf_tensor(G.uniq(name), list(shape), dt))
        regC = sb("regC", [128, 8 * D], BF16)
        wout = WRegion(G, regC, None, W["w_out"], D, D, blk=1024, name="hwout")
        vec = sb("h_vec", [128, 25], F32); b_vec = Buf("h_vec")
        k.op(k.sp, lambda e: e.dma_start(out=vec[:], in_=W["vec"][:, :]), writes=[b_vec], dma=True)
        gain = vec[:, 0:8]
        ogain = vec[:, 24:25]
        cst = sb("h_cst", [128, 2], F32); b_cst = Buf("h_cst")
        k.op(k.pool, lambda e: e.memset(cst[:, 0:1], EPS), writes=[b_cst])
        k.op(k.pool, lambda e: e.memset(cst[:, 1:2], 1.0), writes=[b_cst])
        eps = cst
        lbv = sb("h_lb", [128, 3, 8], F32); b_lb = Buf("h_lb")
        if slot == 0:
            k.op(k.pool, lambda e: e.memset(lbv[:, 0, :], 0.0), writes=[b_lb])
        else:
            k.op(k.dve, lambda e: e.tensor_tensor(out=lbv[:, 0, :], in0=vec[:, 8:16], in1=vec[:, 16:24], op=ALU.subtract),
                 reads=[b_vec], writes=[b_lb])
            k.op(k.act, lambda e: e.activation(out=lbv[:, 0, :], in_=lbv[:, 0, :], func=AF.Exp), reads=[b_lb], writes=[b_lb])
            k.op(k.dve, lambda e: e.tensor_scalar(out=lbv[:, 0, :], in0=lbv[:, 0, :], scalar1=1.0, scalar2=None, op0=ALU.add),
                 reads=[b_lb], writes=[b_lb])
            k.op(k.dve, lambda e: e.reciprocal(out=lbv[:, 0, :], in_=lbv[:, 0, :]), reads=[b_lb], writes=[b_lb])
        k.op(k.dve, lambda e: e.tensor_scalar(out=lbv[:, 1, :], in0=lbv[:, 0, :], scalar1=-1.0, scalar2=1.0, op0=ALU.mult,
                                              op1=ALU.add), reads=[b_lb], writes=[b_lb])
        k.op(k.dve, lambda e: e.tensor_scalar(out=lbv[:, 2, :], in0=lbv[:, 1, :], scalar1=-1.0, scalar2=None, op0=ALU.mult),
             reads=[b_lb], writes=[b_lb])
        tri = sb("h_tri", [128, 128], F32); b_tri = Buf("h_tri")
        k.op(k.sp, lambda e: e.dma_start(out=tri[:], in_=G.c_tri[:, :]), writes=[b_tri], dma=True)
        smask = sb("h_smask", [128, TH], F32); b_sm = Buf("h_smask")
        k.op(k.sp, lambda e: e.dma_start(out=smask[:], in_=G.c_scan[:, 0:TH]), writes=[b_sm], dma=True)
        xt = sb("h_xt", [128, 8, TH], F32); b_xt = Buf("h_xt")
        xo = sb("h_xo", [128, 8, TH], F32); b_xo = Buf("h_xo")
        St = sb("h_S", [128, 8, 128], F32); b_S = [Buf(f"h_S{h}") for h in range(8)]
        k.op(k.pool, lambda e: e.memset(St[:], 0.0), writes=b_S)
        rstd = (sb("h_rstd", [128, TH], F32), Buf("h_rstd"))
        sets = []
        for i in range(2):
            d_ = dict(
                b=[(sb(f"h_b{i}_{j}", [128, TH], F32), Buf(f"h_b{i}_{j}")) for j in range(4)],
                qin=(sb(f"h_qin{i}", [128, TH], F32), Buf(f"h_qin{i}")),
                egl=(sb(f"h_egl{i}", [128, 4], F32), Buf(f"h_egl{i}")),
                r2=(sb(f"h_r2{i}", [128, TH], F32), Buf(f"h_r2{i}")),
                tmp=(sb(f"h_tmp{i}", [128, TH], F32), Buf(f"h_tmp{i}")),
            )
            sets.append(d_)
        off = 8 * 4 * D
        hTs = []
        for i in range(2):
            v, off = carve(G.regA, off, [128, 8, TH]); hTs.append((v, Buf(f"h_hT{i}")))
        vtok, off = carve(G.regA, off, [128, 2, D]); b_vtok = Buf("h_vtok")
        sgate, off = carve(G.regA, off, [128, 8, TH]); b_sg = [Buf(f"h_sg{h}") for h in range(8)]
        oTn, off = carve(G.regA, off, [128, 8, TH]); b_oTn = [Buf(f"h_oTn{h}") for h in range(8)]
        sqs = []
        for i in range(2):
            v, off = carve(G.regA, off, [128, TH]); sqs.append((v, Buf(f"h_sq{i}")))
        for i in range(2):
            d_ = sets[i]
            v, off = carve(G.regA, off, [128, TH]); d_["qrel"] = (v, Buf(f"h_qrel{i}"))
            v, off = carve(G.regA, off, [128, TH]); d_["krel"] = (v, Buf(f"h_krel{i}"))
            v, off = carve(G.regA, off, [128, 2, 128]); d_["attT"] = (v, Buf(f"h_attT{i}"))
            v, off = carve(G.regA, off, [128, 4, 128]); d_["kz"] = (v, Buf(f"h_kz{i}"))
            v, off = carve(G.regA, off, [128, TH]); d_["sqo"] = (v, Buf(f"h_sqo{i}"))
            k.op(k.pool, lambda e, v=d_["kz"][0]: e.memset(v, 0.0), writes=[d_["kz"][1]])
        assert off <= 49152, off
        L = dict(sq=sqs, rstd=rstd, eps=eps, b_eps=b_cst, b_vec=b_vec)
        pA = [Buf(f"h_pA{b}") for b in range(8)]
        pB = [Buf(f"h_pB{b}") for b in range(8)]
        lo = lambda b: G.ps[b][:, 0:TH]
        hi = lambda b: G.ps[b][:, TH:2 * TH]
        b_dS = [[Buf(f"h_dS{i}_{c}") for c in range(4)] for i in range(2)]
        dS_ps = lambda i, c: G.ps[7 - i][:, c * 128:(c + 1) * 128]
        bank = lambda b: [pA[b], pB[b]] if b < 6 else b_dS[7 - b]
        SCL = 128.0 ** -0.5

        k.op(k.sp, lambda e: e.dma_start(out=xt[:], in_=src[:, :, 0:TH]), reads=[sb_src[0]], writes=[b_xt], dma=True)

        def emit_norm(idx):
            hT_, b_hT_ = hTs[idx % 2]
            rmsnorm_tile(G, L, xt, b_xt, gain, hT_, b_hT_, ps_i=0, ps_ap=lo(0), ps_buf=bank(0))
            if idx + 1 < NTH:
                k.op(k.sp, lambda e: e.dma_start(out=xt[:], in_=src[:, :, (idx + 1) * TH:(idx + 2) * TH]),
                     reads=[sb_src[idx + 1]], writes=[b_xt], dma=True)

        emit_norm(0)
        for ti in range(NTH):
            c0 = ti * TH
            hT, b_hT = hTs[ti % 2]
            k.op(k.sp, lambda e, c0=c0: e.dma_start(out=xo[:], in_=src[:, :, c0:c0 + TH]), reads=[sb_src[ti]],
                 writes=[b_xo], dma=True)
            for sub in range(2):
                for half in range(2):
                    pi = 1 + (sub * 2 + half) % 2
                    for kc in range(8):
                        k.op(k.pe, lambda e, kc=kc, sub=sub, half=half, pi=pi: e.matmul(
                            G.ps[pi][:], lhsT=hT[:, kc, sub * 128:(sub + 1) * 128],
                            rhs=win.view[:, kc, 2 * D + half * 512:2 * D + (half + 1) * 512],
                            start=(kc == 0), stop=(kc == 7)),
                            reads=[b_hT, win.buf(kc, 2 * D + half * 512)], writes=bank(pi))
                    k.op(k.act, lambda e, sub=sub, half=half, pi=pi: e.activation(
                        out=vtok[:, sub, half * 512:(half + 1) * 512], in_=G.ps[pi][:], func=AF.Copy),
                        reads=[pA[pi], pB[pi]], writes=[b_vtok])
            for h in range(8):
                pi = 3 + h % 2
                for kc in range(8):
                    k.op(k.pe, lambda e, kc=kc, h=h, pi=pi: e.matmul(
                        lo(pi), lhsT=win.view[:, kc, 3 * D + h * 128:3 * D + (h + 1) * 128], rhs=hT[:, kc, :],
                        start=(kc == 0), stop=(kc == 7)),
                        reads=[b_hT, win.buf(kc, 3 * D + h * 128)], writes=bank(pi))
                k.op(k.act, lambda e, h=h, pi=pi: e.activation(out=sgate[:, h, :], in_=lo(pi), func=AF.Silu),
                     reads=[pA[pi]], writes=[b_sg[h]])
            for hp in range(4):
                hh = (2 * hp, 2 * hp + 1)
                def head_elem(i, h):
                    st = sets[i]
                    pq, pz = 0 + i, 2 + i
                    (b1, B1), (b2, B2), (b3, B3), (b4, B4) = st["b"]
                    for kc in range(8):
                        k.op(k.pe, lambda e, kc=kc, h=h, pq=pq: e.matmul(
                            lo(pq), lhsT=win.view[:, kc, h * 128:(h + 1) * 128], rhs=hT[:, kc, :],
                            start=(kc == 0), stop=(kc == 7)), reads=[b_hT, win.buf(kc, h * 128)], writes=bank(pq))
                    for kc in range(8):
                        k.op(k.pe, lambda e, kc=kc, h=h, pz=pz: e.matmul(
                            lo(pz), lhsT=win.view[:, kc, D + h * 128:D + (h + 1) * 128], rhs=hT[:, kc, :],
                            start=(kc == 0), stop=(kc == 7)), reads=[b_hT, win.buf(kc, D + h * 128)], writes=bank(pz))
                    k.op(k.act, lambda e, b1=b1, pz=pz: e.activation(out=b1[:], in_=lo(pz), func=AF.Exp, scale=-1.0),
                         reads=[pA[pz]], writes=[B1])
                    yield
                    k.op(k.dve, lambda e, b1=b1: e.tensor_scalar(out=b1[:], in0=b1[:], scalar1=1.0, scalar2=None, op0=ALU.add),
                         reads=[B1], writes=[B1])
                    yield
                    k.op(k.dve, lambda e, b1=b1: e.reciprocal(out=b1[:], in_=b1[:]), reads=[B1], writes=[B1])
                    yield
                    k.op(k.act, lambda e, b1=b1, b2=b2, h=h: e.activation(out=b2[:], in_=b1[:], func=AF.Ln, bias=lbv[:, 0, h:h + 1],
                                                                        scale=lbv[:, 1, h:h + 1]),
                         reads=[B1, b_lb], writes=[B2])
                    yield
                    k.op(k.dve, lambda e, b1=b1, h=h: e.tensor_scalar(out=b1[:], in0=b1[:], scalar1=lbv[:, 2, h:h + 1],
                                                                     scalar2=lbv[:, 1, h:h + 1], op0=ALU.mult, op1=ALU.add),
                         reads=[B1, b_lb], writes=[B1])
                    yield
                    k.op(k.dve, lambda e, b2=b2, b3=b3: e.tensor_tensor_scan(out=b3[:], data0=smask[:], data1=b2[:], initial=0.0,
                                                                            op0=ALU.mult, op1=ALU.add),
                         reads=[B2, b_sm], writes=[B3])
                    yield
                    G3 = b3[:].rearrange("p (c t) -> p c t", t=64)
                    k.op(k.dve, lambda e, b2=b2, G3=G3: e.tensor_tensor(
                        out=b2[:].rearrange("p (c t) -> p c t", t=64), in0=G3, in1=G3[:, :, 31:32].to_broadcast([128, 4, 64]),
                        op=ALU.subtract), reads=[B3], writes=[B2])
                    yield
                    k.op(k.act, lambda e, b2=b2, b4=b4: e.activation(out=b4[:], in_=b2[:], func=AF.Exp), reads=[B2], writes=[B4])
                    yield
                    qrel, Bqrel = st["qrel"]
                    k.op(k.dve, lambda e, qrel=qrel, b4=b4, pq=pq: e.scalar_tensor_tensor(
                        out=qrel, in0=lo(pq), scalar=SCL, in1=b4[:], op0=ALU.mult, op1=ALU.mult),
                        reads=[pA[pq], B4], writes=[Bqrel])
                    yield
                    k.op(k.act, lambda e, b2=b2: e.activation(out=b2[:], in_=b2[:], func=AF.Exp, scale=-1.0), reads=[B2], writes=[B2])
                    yield
                    krel, Bkrel = st["krel"]
                    k.op(k.dve, lambda e, krel=krel, b1=b1, b2=b2: e.tensor_tensor(out=krel, in0=b1[:], in1=b2[:], op=ALU.mult),
                         reads=[B1, B2], writes=[Bkrel])
                    yield
                    k.op(k.act, lambda e, b3=b3, b4=b4: e.activation(out=b4[:], in_=b3[:], func=AF.Exp), reads=[B3], writes=[B4])
                    yield
                    qin, Bqin = st["qin"]
                    k.op(k.dve, lambda e, qin=qin, b4=b4, pq=pq: e.scalar_tensor_tensor(
                        out=qin[:], in0=lo(pq), scalar=SCL, in1=b4[:], op0=ALU.mult, op1=ALU.mult),
                        reads=[pA[pq], B4], writes=[Bqin])
                    yield
                    k.op(k.dve, lambda e, b2=b2, G3=G3: e.tensor_tensor(
                        out=b2[:].rearrange("p (c t) -> p c t", t=64), in0=G3, in1=G3[:, :, 63:64].to_broadcast([128, 4, 64]),
                        op=ALU.subtract), reads=[B3], writes=[B2])
                    yield
                    k.op(k.act, lambda e, b2=b2: e.activation(out=b2[:], in_=b2[:], func=AF.Exp, scale=-1.0), reads=[B2], writes=[B2])
                    yield
                    k.op(k.dve, lambda e, b1=b1, b2=b2, b4=b4: e.tensor_tensor(out=b4[:], in0=b1[:], in1=b2[:], op=ALU.mult),
                         reads=[B1, B2], writes=[B4])
                    yield
                    egl, Begl = st["egl"]
                    k.op(k.act, lambda e, egl=egl, G3=G3: e.activation(out=egl[:], in_=G3[:, :, 63], func=AF.Exp),
                         reads=[B3], writes=[Begl])
                    yield
                    kTp = lo(5) if i == 0 else hi(5)
                    BkT = pA[5] if i == 0 else pB[5]
                    for pr in range(2):
                        k.op(k.pe, lambda e, pr=pr, b4=b4, kTp=kTp: e.transpose(
                            out=kTp[:, pr * 128:(pr + 1) * 128], in_=b4[:, pr * 128:(pr + 1) * 128],
                            identity=G.ident_f[:]), reads=[B4, G.b_const], writes=bank(5))
                    kz, Bkz = st["kz"]
                    kzv = kz.rearrange("p (pr cc) d -> p pr cc d", cc=2)
                    k.op(k.act, lambda e, kzv=kzv, kTp=kTp: e.activation(
                        out=kzv[0:64, :, 0, :], in_=kTp[0:64, :].rearrange("p (pr d) -> p pr d", d=128),
                        func=AF.Copy), reads=[BkT], writes=[Bkz])
                    yield
                    k.op(k.act, lambda e, kzv=kzv, kTp=kTp: e.activation(
                        out=kzv[64:128, :, 1, :], in_=kTp[64:128, :].rearrange("p (pr d) -> p pr d", d=128),
                        func=AF.Copy), reads=[BkT], writes=[Bkz])
                    yield
                    aTp = lo(4) if i == 0 else hi(4)
                    BaT = pA[4] if i == 0 else pB[4]
                    for pr in range(2):
                        k.op(k.pe, lambda e, pr=pr, aTp=aTp, krel=krel, qrel=qrel: e.matmul(
                            aTp[:, pr * 128:(pr + 1) * 128], lhsT=krel[:, pr * 128:(pr + 1) * 128],
                            rhs=qrel[:, pr * 128:(pr + 1) * 128], start=True, stop=True),
                            reads=[Bkrel, Bqrel], writes=bank(4))
                    attT, BattT = st["attT"]
                    k.op(k.dve, lambda e, attT=attT, aTp=aTp: e.tensor_tensor(
                        out=attT, in0=aTp.rearrange("p (pr t) -> p pr t", t=128),
                        in1=tri[:].unsqueeze(1).to_broadcast([128, 2, 128]), op=ALU.mult),
                        reads=[BaT, b_tri], writes=[BattT])
                    yield
                    for c in range(4):
                        k.op(k.pe, lambda e, c=c, i=i, kz=kz, h=h: e.matmul(
                            dS_ps(i, c), lhsT=kz[:, c, :], rhs=vtok[:, c // 2, h * 128:(h + 1) * 128], start=True, stop=True),
                            reads=[Bkz, b_vtok], writes=bank(7 - i))

                interleave([head_elem(i_, h_) for i_, h_ in enumerate(hh)])
                for c in range(4):
                    for i, h in enumerate(hh):
                        st = sets[i]
                        qin, Bqin = st["qin"]
                        attT, BattT = st["attT"]
                        egl, Begl = st["egl"]
                        pr, cc = c // 2, c % 2
                        k.op(k.pe, lambda e, c=c, i=i, h=h, qin=qin: e.matmul(
                            hi(i)[:, c * 64:(c + 1) * 64], lhsT=St[:, h, :], rhs=qin[:, c * 64:(c + 1) * 64],
                            start=True, stop=False), reads=[b_S[h], Bqin], writes=bank(i))
                        k.op(k.pe, lambda e, c=c, i=i, h=h, attT=attT, pr=pr, cc=cc: e.matmul(
                            hi(i)[:, c * 64:(c + 1) * 64], lhsT=vtok[:, pr, h * 128:(h + 1) * 128],
                            rhs=attT[:, pr, cc * 64:(cc + 1) * 64], start=False, stop=True),
                            reads=[b_vtok, BattT], writes=bank(i))
                        k.op(k.dve, lambda e, c=c, i=i, h=h, egl=egl: e.scalar_tensor_tensor(
                            out=St[:, h, :], in0=St[:, h, :], scalar=egl[:, c:c + 1], in1=dS_ps(i, c),
                            op0=ALU.mult, op1=ALU.add),
                            reads=[b_S[h], Begl, b_dS[i][c]], writes=[b_S[h]])
                def head_norm(i, h):
                    st = sets[i]
                    sqo, Bsqo = st["sqo"]
                    r2, Br2 = st["r2"]
                    tmp, Btmp = st["tmp"]
                    k.op(k.act, lambda e, sqo=sqo, i=i: e.activation(out=sqo, in_=hi(i), func=AF.Square),
                         reads=[pB[i]], writes=[Bsqo])
                    yield
                    k.op(k.pe, lambda e, sqo=sqo, i=i: e.matmul(hi(2 + i), lhsT=G.ones_bf[:], rhs=sqo, start=True, stop=True),
                         reads=[Bsqo, G.b_const], writes=bank(2 + i))
                    yield
                    k.op(k.act, lambda e, r2=r2, i=i: e.activation(out=r2[:], in_=hi(2 + i), func=AF.Ln,
                                                                   bias=cst[:, 0:1], scale=1.0 / 128),
                         reads=[pB[2 + i], b_cst], writes=[Br2])
                    yield
                    k.op(k.act, lambda e, r2=r2: e.activation(out=r2[:], in_=r2[:], func=AF.Exp, scale=-0.5), reads=[Br2], writes=[Br2])
                    yield
                    k.op(k.dve, lambda e, tmp=tmp, r2=r2, i=i: e.scalar_tensor_tensor(
                        out=tmp[:], in0=hi(i), scalar=ogain, in1=r2[:], op0=ALU.mult, op1=ALU.mult),
                        reads=[pB[i], Br2, b_vec], writes=[Btmp])
                    yield
                    k.op(k.dve, lambda e, tmp=tmp, h=h: e.tensor_tensor(out=oTn[:, h, :], in0=tmp[:], in1=sgate[:, h, :], op=ALU.mult),
                         reads=[Btmp, b_sg[h]], writes=[b_oTn[h]])
                    yield

                interleave([head_norm(i_, h_) for i_, h_ in enumerate(hh)])
                if hp == 2 and ti + 1 < NTH:
                    emit_norm(ti + 1)
            for oc in range(8):
                pi = oc % 2
                for h in range(8):
                    k.op(k.pe, lambda e, oc=oc, h=h, pi=pi: e.matmul(
                        lo(pi), lhsT=wout.view[:, h, oc * 128:(oc + 1) * 128], rhs=oTn[:, h, :],
                        start=(h == 0), stop=(h == 7)), reads=[wout.buf(h, oc * 128), b_oTn[h]], writes=bank(pi))
                k.op(k.dve, lambda e, oc=oc, pi=pi: e.tensor_tensor(out=xo[:, oc, :], in0=lo(pi), in1=xo[:, oc, :],
                                                                   op=ALU.add), reads=[pA[pi], b_xo], writes=[b_xo])
            k.op(k.sp, lambda e, c0=c0: e.dma_start(out=dst[:, :, c0:c0 + TH], in_=xo[:]), reads=[b_xo], writes=[sb_dst[ti]],
                 dma=True)


PHASE_FN = {"F1": phase_F1, "F2": phase_F2, "M1": phase_M1, "M2": phase_M2, "M3": phase_M3, "H1": phase_H1}


def fm(v, n):
    return np.ascontiguousarray(np.asarray(v, np.float32).reshape(n, 128).T)


def host_consts(kinds):
    c = {"c_ones": np.ones((128, 128), np.float32), "c_ident": np.eye(128, dtype=np.float32)}
    if "moba" in kinds:
        rot = np.zeros((128, 128), np.float32)
        for m_ in range(64):
            rot[m_ + 64, m_] = -1.0
            rot[m_, m_ + 64] = 1.0
        c["c_rot"] = rot
        inv = (1.0 / (np.float32(10000.0) ** (np.arange(0, 128, 2, dtype=np.float32) / np.float32(128)))).astype(np.float32)
        ang = (np.arange(S, dtype=np.float32)[:, None] * inv[None, :]).astype(np.float32)
        ang = np.concatenate([ang, ang], axis=-1)
        c["c_cos"] = np.ascontiguousarray(np.cos(ang).astype(np.float32).T)
        c["c_sin"] = np.ascontiguousarray(np.sin(ang).astype(np.float32).T)
        past = np.full((32, 16), -1e30, np.float32)
        for i in range(32):
            past[i, :i // 2] = 0.0
        c["c_past"] = np.ascontiguousarray(np.broadcast_to(past.reshape(1, 512), (128, 512)))
        causal = np.full((128, 2, 256), NEG, np.float32)
        for kt in range(2):
            for p in range(128):
                causal[p, kt, kt * 128 + p:] = 0.0
        c["c_causal"] = causal.reshape(128, 512)
        selrow = np.zeros((128, 16, 128), np.float32)
        for n_ in range(16):
            selrow[n_, n_, :] = 1.0
        c["c_selrow"] = selrow.reshape(128, 2048)
    if "hgrn" in kinds:
        tri = np.zeros((128, 128), np.float32)
        for s_ in range(128):
            for t_ in range(128):
                if s_ // 64 == t_ // 64 and s_ <= t_:
                    tri[s_, t_] = 1.0
        c["c_tri"] = tri
        sm = np.ones((128, T), np.float32)
        sm[:, ::64] = 0.0
        c["c_scan"] = sm
    return c


def stage_inputs(inputs, stages):
    m = {}
    for kind, l in stages:
        if kind == "ffn":
            m[f"ffn_w_up_{l}"] = np.ascontiguousarray(inputs["ffn_w_up"][l])
            m[f"ffn_w_dn_{l}"] = np.ascontiguousarray(inputs["ffn_w_down"][l])
            cw = inputs["ffn_conv_w"][l]
            m[f"ffn_vec_{l}"] = np.ascontiguousarray(np.concatenate(
                [fm(inputs["ffn_norm"][l], 8), fm(cw[0], 24), fm(cw[1], 24), fm(cw[2], 24),
                 fm(inputs["ffn_conv_b"][l], 24)], axis=1))
        elif kind == "hgrn":
            sl = l // 2
            m[f"hgrn_w_in_{l}"] = np.ascontiguousarray(inputs["hgrn_w_in"][sl])
            m[f"hgrn_w_out_{l}"] = np.ascontiguousarray(inputs["hgrn_w_out"][sl])
            m[f"hgrn_vec_{l}"] = np.ascontiguousarray(np.concatenate(
                [fm(inputs["attn_norm"][l], 8), fm(inputs["hgrn_lb"][0], 8), fm(inputs["hgrn_lb"][1], 8),
                 fm(inputs["hgrn_out_norm"][sl], 1)], axis=1))
        elif kind == "moba":
            sl = l // 2
            m[f"moba_w_qkv_{l}"] = np.ascontiguousarray(inputs["moba_w_qkv"][sl])
            m[f"moba_w_out_{l}"] = np.ascontiguousarray(inputs["moba_w_out"][sl])
            m[f"moba_vec_{l}"] = np.ascontiguousarray(np.concatenate(
                [fm(inputs["attn_norm"][l], 8), fm(inputs["moba_q_norm"][sl], 1), fm(inputs["moba_k_norm"][sl], 1)], axis=1))
    return m


def x_to_dev(xb):
    return np.ascontiguousarray(xb.T.reshape(8, 128, S).transpose(1, 0, 2))


def x_from_dev(y):
    return np.ascontiguousarray(y.transpose(1, 0, 2).reshape(D, S).T)


FUSED = True
LAYER_STAGES = [[("hgrn", 0), ("ffn", 0)], [("moba", 1), ("ffn", 1)], [("hgrn", 2), ("ffn", 2)], [("moba", 3), ("ffn", 3)]]


def kernel(**inputs):
    inputs = {k_: np.asarray(v) for k_, v in inputs.items()}
    x = inputs["x"].astype(np.float32)
    nb = x.shape[0]
    groups = [sum(LAYER_STAGES, [])] if FUSED else LAYER_STAGES
    xs = [x_to_dev(x[b]) for b in range(nb)]
    for stages in groups:
        P = build_program(stages)
        shared = dict(host_consts({kd for kd, _ in stages}))
        shared.update(stage_inputs(inputs, stages))
        shared = {k_: v for k_, v in shared.items() if k_ in P.ext_in}
        missing = set(P.ext_in) - set(shared) - {"xin"}
        assert not missing, missing
        in_maps = [dict(shared, xin=xs[b]) for b in range(nb)]
        res = run_bass_kernel_spmd(P.nc, in_maps, core_ids=list(range(nb)))
        xs = [np.asarray(res.results[b]["yout"], dtype=np.float32) for b in range(nb)]
    return np.stack([x_from_dev(xs[b]) for b in range(nb)]).astype(np.float32)
```
